# Optimizing a Trainium2 kernel written in Bass

```python
import math
import numpy as np
import jax
import jax.numpy as jnp
from jax import lax

D_MODEL = 2048
BATCH = 8
SEQ = 2048
DEPTH = 2

GRID_W = 64
CTX_LEN = 256

NA_HEADS = 16
NA_HEAD_DIM = 64
NA_W = NA_HEADS * NA_HEAD_DIM
NA_WIN_H = 8
NA_WIN_W = 16
NA_COL_BLOCK = 16
NA_COL_HALO = 32

RW_HEADS = 16
RW_HEAD = 64
RW_W = RW_HEADS * RW_HEAD
RW_DECAY_LORA = 64
RW_AAA_LORA = 64
RW_GATE_LORA = 160
RW_SHIFT_W = 3 * RW_W + 2 * RW_DECAY_LORA + 2 * RW_AAA_LORA + RW_GATE_LORA
GN_EPS = 64e-5

MLA_HEADS = 16
MLA_NOPE = 64
MLA_ROPE = 32
MLA_V = 64
MLA_QK = MLA_NOPE + MLA_ROPE
MLA_Q_LORA = 512
MLA_KV_LORA = 256
MLA_Q_BLOCK = 128
ROPE_THETA = 10000.0

N_BRANCH = 3
BRANCH_W = 1024
IN_SPLIT = (3 * NA_W, RW_SHIFT_W, MLA_Q_LORA, MLA_KV_LORA, MLA_ROPE, N_BRANCH * D_MODEL)
IN_W = 3 * NA_W + RW_SHIFT_W + MLA_Q_LORA + MLA_KV_LORA + MLA_ROPE + N_BRANCH * D_MODEL
RW_SPLIT = (RW_W, RW_W, RW_W, 2 * RW_DECAY_LORA, 2 * RW_AAA_LORA, RW_GATE_LORA)

N_EXPERTS = 16
N_GROUPS = 4
EXPERTS_PER_GROUP = N_EXPERTS // N_GROUPS
TOP_K = 2
D_EXPERT = 1024
MOE_BLOCK = 256

NORM_EPS = 1e-6
NEG_INF = -1e30

kernel_name = "hybrid_na_rwkv7_mla_grouped_moe_dit"


def _split(x, sizes):
    idx = np.cumsum(sizes)[:-1].tolist()
    return jnp.split(x, idx, axis=-1)


def rms_norm(x, g):
    xf = x.astype(jnp.float32)
    y = xf * lax.rsqrt(jnp.mean(xf * xf, axis=-1, keepdims=True) + NORM_EPS)
    return (y * g.astype(jnp.float32)).astype(x.dtype)


def axial_rope(x, pos):
    half = MLA_ROPE // 2
    nf = half // 2
    inv = ROPE_THETA ** (-jnp.arange(nf, dtype=jnp.float32) / nf)
    shape = (pos.shape[0],) + (1,) * (x.ndim - 3) + (nf,)
    xf = x.astype(jnp.float32)

    def rot(v, p):
        ang = (p.astype(jnp.float32)[:, None] * inv).reshape(shape)
        cs, sn = jnp.cos(ang), jnp.sin(ang)
        v1, v2 = v[..., :nf], v[..., nf:]
        return jnp.concatenate([v1 * cs - v2 * sn, v1 * sn + v2 * cs], axis=-1)

    out = jnp.concatenate([rot(xf[..., :half], pos // GRID_W), rot(xf[..., half:], pos % GRID_W)], axis=-1)
    return out.astype(x.dtype)


def full_attention(q, k, v, scale):
    s = jnp.einsum('bqhd,bkhd->bhqk', q, k).astype(jnp.float32) * scale
    p = jax.nn.softmax(s, axis=-1).astype(v.dtype)
    return jnp.einsum('bhqk,bkhd->bqhd', p, v)


def blocked_attention(q, k, v, scale):
    B, T, H, Dq = q.shape
    nb = T // MLA_Q_BLOCK
    qb = jnp.moveaxis(q.reshape(B, nb, MLA_Q_BLOCK, H, Dq), 1, 0)

    def one_block(qi):
        s = jnp.einsum('bqhd,bkhd->bhqk', qi, k).astype(jnp.float32) * scale
        p = jax.nn.softmax(s, axis=-1).astype(v.dtype)
        return jnp.einsum('bhqk,bkhd->bqhd', p, v)

    out = lax.map(one_block, qb)
    return jnp.moveaxis(out, 0, 1).reshape(B, T, H, v.shape[-1])


def neighbourhood_attention(q, k, v, k_ctx, v_ctx, rpb):
    B, T, H, Dh = q.shape
    rows = T // GRID_W
    kh = min(NA_WIN_H, rows)
    n_cb = GRID_W // NA_COL_BLOCK
    scale = 1.0 / math.sqrt(Dh)
    j = np.arange(GRID_W).reshape(n_cb, NA_COL_BLOCK)
    col_start = np.clip(j - NA_WIN_W // 2, 0, GRID_W - NA_WIN_W)
    halo_start = np.clip(np.arange(n_cb) * NA_COL_BLOCK - NA_WIN_W // 2, 0, GRID_W - NA_COL_HALO)
    halo_cols = halo_start[:, None] + np.arange(NA_COL_HALO)
    col_valid = ((halo_cols[:, None, :] >= col_start[:, :, None])
                 & (halo_cols[:, None, :] < col_start[:, :, None] + NA_WIN_W))
    col_off = np.clip(halo_cols[:, None, :] - j[:, :, None] + NA_WIN_W - 1, 0, 2 * NA_WIN_W - 2)
    rpb_cols = jnp.where(col_valid, rpb[:, :, col_off].astype(jnp.float32), NEG_INF)

    qg = q.reshape(B, rows, n_cb, NA_COL_BLOCK, H, Dh)
    k_halo = k.reshape(B, rows, GRID_W, H, Dh)[:, :, halo_cols]
    v_halo = v.reshape(B, rows, GRID_W, H, Dh)[:, :, halo_cols]
    n_loc = kh * NA_COL_HALO

    def one_row(i):
        r0 = jnp.clip(i - kh // 2, 0, rows - kh)
        kr = lax.dynamic_slice_in_dim(k_halo, r0, kh, axis=1)
        vr = lax.dynamic_slice_in_dim(v_halo, r0, kh, axis=1)
        qr = lax.dynamic_index_in_dim(qg, i, axis=1, keepdims=False)
        row_idx = r0 - i + jnp.arange(kh) + NA_WIN_H - 1
        bias = jnp.transpose(rpb_cols[:, row_idx], (0, 2, 3, 1, 4))
        s_loc = jnp.einsum('bnqhd,brnchd->bhnqrc', qr, kr).astype(jnp.float32) * scale + bias
        s_ctx = jnp.einsum('bnqhd,bchd->bhnqc', qr, k_ctx).astype(jnp.float32) * scale
        s = jnp.concatenate([s_loc.reshape(s_loc.shape[:4] + (n_loc,)), s_ctx], axis=-1)
        p = jax.nn.softmax(s, axis=-1).astype(v.dtype)
        p_loc = p[..., :n_loc].reshape(s_loc.shape)
        return (jnp.einsum('bhnqrc,brnchd->bnqhd', p_loc, vr)
                + jnp.einsum('bhnqc,bchd->bnqhd', p[..., n_loc:], v_ctx))

    out = lax.map(one_row, jnp.arange(rows))
    return jnp.moveaxis(out, 0, 1).reshape(B, T, H, Dh)


def centred_shift(u, mu):
    pad = jnp.pad(u, ((0, 0), (1, 1), (0, 0)))
    return u + mu * (0.5 * (pad[:, :-2] + pad[:, 2:]) - u)


def rwkv_prepare(u, mu, w0, w_up, a0, a_up, g_up, k_k, k_a):
    B, T, _ = u.shape
    u = centred_shift(u, mu)
    r, k, v, wd, ad, gd = _split(u, RW_SPLIT)
    wd = wd.reshape(B, T, 2, RW_DECAY_LORA)
    ad = ad.reshape(B, T, 2, RW_AAA_LORA)
    w = -jax.nn.softplus(-(w0 + jnp.einsum('btzl,zlc->btzc', jnp.tanh(wd), w_up)).astype(jnp.float32)) - 0.5
    decay = jnp.exp(-jnp.exp(w))
    a = jax.nn.sigmoid((a0 + jnp.einsum('btzl,zlc->btzc', ad, a_up)).astype(jnp.float32))
    g = jax.nn.sigmoid(gd) @ g_up
    heads = lambda t: t.reshape(t.shape[:-1] + (RW_HEADS, RW_HEAD))
    kk = heads(k * k_k).astype(jnp.float32)
    kk = kk / jnp.maximum(jnp.sqrt(jnp.sum(kk * kk, axis=-1, keepdims=True)), 1e-12)
    kd = k[:, :, None, :] * (1.0 + (a - 1.0) * k_a)
    return (heads(r), heads(decay), kk, heads(a), heads(kd), heads(v), g, heads(k))


def _rwkv_step(state, inp):
    r, w, kk, a, k, v = inp
    sk = jnp.einsum('zbhvk,zbhk->zbhv', state, kk)
    state = (state * w[..., None, :] - sk[..., :, None] * (kk * a)[..., None, :]
             + v[..., :, None] * k[..., None, :])
    return state, jnp.einsum('zbhvk,zbhk->zbhv', state, r)


def rwkv_bidir_scan(state0, prep):
    r, decay, kk, a, kd, v = prep[:6]
    both = lambda t: jnp.stack([t, t[:, ::-1]], axis=0)
    per_dir = lambda t: jnp.stack([t[:, :, 0], t[:, ::-1, 1]], axis=0)
    seq = (both(r), per_dir(decay), both(kk), per_dir(a), per_dir(kd), both(v))
    seq = tuple(jnp.moveaxis(t.astype(jnp.float32), 2, 0) for t in seq)
    state, ys = lax.scan(_rwkv_step, state0, seq)
    y = ys[:, 0] + ys[::-1, 1]
    return state, jnp.moveaxis(y, 0, 1)


def rwkv_output(y, prep, r_k, gn_w, gn_b, dtype):
    r, k, v, g = prep[0], prep[7], prep[5], prep[6]
    B, T = y.shape[:2]
    mean = jnp.mean(y, axis=-1, keepdims=True)
    var = jnp.mean(jnp.square(y - mean), axis=-1, keepdims=True)
    yn = ((y - mean) * lax.rsqrt(var + GN_EPS)).reshape(B, T, RW_W) * gn_w + gn_b
    bonus = (jnp.sum(r * k * r_k, axis=-1, keepdims=True) * v).reshape(B, T, RW_W)
    return ((yn + bonus) * g).astype(dtype)


def mla_q(cq, q_norm_g, w_uq, pos):
    B, T, _ = cq.shape
    q = (rms_norm(cq, q_norm_g) @ w_uq).reshape(B, T, MLA_HEADS, MLA_QK)
    if pos is None:
        return q
    return jnp.concatenate([q[..., :MLA_NOPE], axial_rope(q[..., MLA_NOPE:], pos)], axis=-1)


def mla_kv(ckv, k_rope, kv_norm_g, w_ukv, pos):
    B, T, _ = ckv.shape
    kv = (rms_norm(ckv, kv_norm_g) @ w_ukv).reshape(B, T, MLA_HEADS, MLA_NOPE + MLA_V)
    if pos is not None:
        k_rope = axial_rope(k_rope, pos)
    k = jnp.concatenate([kv[..., :MLA_NOPE],
                         jnp.broadcast_to(k_rope[:, :, None, :], (B, T, MLA_HEADS, MLA_ROPE))], axis=-1)
    return k, kv[..., MLA_NOPE:]


def branch_merge(ys, gates, w_branch, w_out):
    B, T, _ = gates.shape
    gate = jax.nn.sigmoid(gates.astype(jnp.float32)).astype(ys[0].dtype).reshape(B, T, N_BRANCH, D_MODEL)
    merged = gate[:, :, 0] * (ys[0] @ w_branch[0])
    for z in range(1, N_BRANCH):
        merged = merged + gate[:, :, z] * (ys[z] @ w_branch[z])
    return merged @ w_out


def hybrid_mixer(h, hc, pos, w_in, na_rpb, rw_shift_mu, rw_w0, rw_w_up, rw_a0, rw_a_up, rw_g_up,
                 rw_k_k, rw_k_a, rw_r_k, rw_gn_w, rw_gn_b, mla_q_norm_g, mla_w_uq, mla_kv_norm_g,
                 mla_w_ukv, w_branch, w_out, need_ctx):
    B, T, _ = h.shape
    Tc = hc.shape[1]
    na_u, rw_u, cq, ckv, kr, gates = _split(h @ w_in, IN_SPLIT)
    na_uc, rw_uc, cqc, ckvc, krc, gatesc = _split(hc @ w_in, IN_SPLIT)

    q, k, v = [t.reshape(B, T, NA_HEADS, NA_HEAD_DIM) for t in jnp.split(na_u, 3, axis=-1)]
    qc, kc, vc = [t.reshape(B, Tc, NA_HEADS, NA_HEAD_DIM) for t in jnp.split(na_uc, 3, axis=-1)]
    y_na = neighbourhood_attention(q, k, v, kc, vc, na_rpb).reshape(B, T, NA_W)

    rw_args = (rw_shift_mu, rw_w0, rw_w_up, rw_a0, rw_a_up, rw_g_up, rw_k_k, rw_k_a)
    prep = rwkv_prepare(rw_u, *rw_args)
    prep_c = rwkv_prepare(rw_uc, *rw_args)
    state0 = jnp.zeros((2, B, RW_HEADS, RW_HEAD, RW_HEAD), jnp.float32)
    state_ctx, yc_scan = rwkv_bidir_scan(state0, prep_c)
    _, y_scan = rwkv_bidir_scan(state_ctx, prep)
    y_rw = rwkv_output(y_scan, prep, rw_r_k, rw_gn_w, rw_gn_b, h.dtype)

    mla_scale = 1.0 / math.sqrt(MLA_QK)
    qm = mla_q(cq, mla_q_norm_g, mla_w_uq, pos)
    km, vm = mla_kv(ckv, kr, mla_kv_norm_g, mla_w_ukv, pos)
    kmc, vmc = mla_kv(ckvc, krc, mla_kv_norm_g, mla_w_ukv, None)
    y_mla = blocked_attention(qm, jnp.concatenate([km, kmc], axis=1), jnp.concatenate([vm, vmc], axis=1),
                              mla_scale).reshape(B, T, MLA_HEADS * MLA_V)

    out = branch_merge((y_na, y_rw, y_mla), gates, w_branch, w_out)
    if not need_ctx:
        return out, None
    yc_na = full_attention(qc, kc, vc, 1.0 / math.sqrt(NA_HEAD_DIM)).reshape(B, Tc, NA_W)
    yc_rw = rwkv_output(yc_scan, prep_c, rw_r_k, rw_gn_w, rw_gn_b, hc.dtype)
    qmc = mla_q(cqc, mla_q_norm_g, mla_w_uq, None)
    yc_mla = full_attention(qmc, kmc, vmc, mla_scale).reshape(B, Tc, MLA_HEADS * MLA_V)
    out_c = branch_merge((yc_na, yc_rw, yc_mla), gatesc, w_branch, w_out)
    return out, out_c


def moe_ffn(h, router_w, router_bias, w_gate_up, w_down):
    n = h.shape[0]
    scores = jax.nn.sigmoid((h @ router_w).astype(jnp.float32))
    sel = scores + router_bias.astype(jnp.float32)
    grp_score = jnp.sum(lax.top_k(sel.reshape(n, N_GROUPS, EXPERTS_PER_GROUP), TOP_K)[0], axis=-1)
    best_group = jnp.argmax(grp_score, axis=-1)
    in_group = (jnp.arange(N_EXPERTS) // EXPERTS_PER_GROUP)[None, :] == best_group[:, None]
    _, top_e = lax.top_k(jnp.where(in_group, sel, -jnp.inf), TOP_K)
    top_s = jnp.take_along_axis(scores, top_e, axis=-1)
    weights = top_s / jnp.sum(top_s, axis=-1, keepdims=True)

    m = n * TOP_K
    slot_e = top_e.reshape(-1)
    slot_tok = jnp.repeat(jnp.arange(n), TOP_K)
    order = jnp.argsort(slot_e, stable=True)
    e_s, tok_s, w_s = slot_e[order], slot_tok[order], weights.reshape(-1)[order]
    counts = jnp.bincount(slot_e, length=N_EXPERTS)
    padded = (counts + MOE_BLOCK - 1) // MOE_BLOCK * MOE_BLOCK
    starts = jnp.cumsum(counts) - counts
    pends = jnp.cumsum(padded)
    dest = (pends - padded)[e_s] + jnp.arange(m) - starts[e_s]
    n_blocks = m // MOE_BLOCK + N_EXPERTS
    buf = jnp.zeros((n_blocks * MOE_BLOCK, h.shape[1]), h.dtype).at[dest].set(h[tok_s])
    blk_e = jnp.clip(jnp.searchsorted(pends, jnp.arange(n_blocks) * MOE_BLOCK, side='right'), 0, N_EXPERTS - 1)

    def expert_block(args):
        xb, e = args
        gte, up = jnp.split(xb @ w_gate_up[e], 2, axis=-1)
        return (jax.nn.silu(gte) * up) @ w_down[e]

    out_buf = lax.map(expert_block, (buf.reshape(n_blocks, MOE_BLOCK, -1), blk_e)).reshape(buf.shape)
    contrib = out_buf[dest] * w_s[:, None].astype(h.dtype)
    return jnp.zeros_like(h).at[tok_s].add(contrib)


def setup_inputs(seed: int = 0) -> dict:
    key = jax.random.key(seed)
    ks = iter(jax.random.split(key, 40))
    L, D, F = DEPTH, D_MODEL, D_EXPERT
    nrm = lambda shape, s: jax.random.normal(next(ks), shape, jnp.float32) * s
    uni = lambda shape, lo, hi: jax.random.uniform(next(ks), shape, jnp.float32, lo, hi)
    return {
        "x": nrm((BATCH, SEQ, D), 1.0),
        "c": nrm((BATCH, D), 1.0),
        "ctx": nrm((BATCH, CTX_LEN, D), 1.0),
        "c_ctx": nrm((D,), 1.0),
        "w_mod": nrm((L, D, 6 * D), 0.5 * D ** -0.5),
        "b_mod": nrm((L, 6 * D), 0.01),
        "norm1_g": 1.0 + nrm((L, D), 0.01),
        "norm2_g": 1.0 + nrm((L, D), 0.01),
        "w_in": nrm((L, D, IN_W), D ** -0.5),
        "na_rpb": nrm((L, NA_HEADS, 2 * NA_WIN_H - 1, 2 * NA_WIN_W - 1), 0.1),
        "rw_shift_mu": uni((L, RW_SHIFT_W), 0.0, 1.0),
        "rw_w0": uni((L, 2, RW_W), -6.0, 0.0),
        "rw_w_up": nrm((L, 2, RW_DECAY_LORA, RW_W), 0.5 * RW_DECAY_LORA ** -0.5),
        "rw_a0": nrm((L, 2, RW_W), 0.5),
        "rw_a_up": nrm((L, 2, RW_AAA_LORA, RW_W), 0.5 * RW_AAA_LORA ** -0.5),
        "rw_g_up": nrm((L, RW_GATE_LORA, RW_W), RW_GATE_LORA ** -0.5),
        "rw_k_k": 0.85 + nrm((L, RW_W), 0.05),
        "rw_k_a": 1.0 + nrm((L, RW_W), 0.05),
        "rw_r_k": nrm((L, RW_HEADS, RW_HEAD), 0.1),
        "rw_gn_w": 1.0 + nrm((L, RW_W), 0.01),
        "rw_gn_b": nrm((L, RW_W), 0.01),
        "mla_q_norm_g": 1.0 + nrm((L, MLA_Q_LORA), 0.01),
        "mla_w_uq": nrm((L, MLA_Q_LORA, MLA_HEADS * MLA_QK), MLA_Q_LORA ** -0.5),
        "mla_kv_norm_g": 1.0 + nrm((L, MLA_KV_LORA), 0.01),
        "mla_w_ukv": nrm((L, MLA_KV_LORA, MLA_HEADS * (MLA_NOPE + MLA_V)), MLA_KV_LORA ** -0.5),
        "w_branch": nrm((L, N_BRANCH, BRANCH_W, D), BRANCH_W ** -0.5),
        "w_out": nrm((L, D, D), D ** -0.5),
        "router_w": nrm((D, N_EXPERTS), D ** -0.5),
        "router_bias": nrm((N_EXPERTS,), 0.01),
        "moe_w_gate_up": nrm((L, N_EXPERTS, D, 2 * F), D ** -0.5),
        "moe_w_down": nrm((L, N_EXPERTS, F, D), F ** -0.5),
        "final_norm_g": 1.0 + nrm((D,), 0.01),
    }


def reference(x, c, ctx, c_ctx, w_mod, b_mod, norm1_g, norm2_g, w_in, na_rpb, rw_shift_mu, rw_w0,
              rw_w_up, rw_a0, rw_a_up, rw_g_up, rw_k_k, rw_k_a, rw_r_k, rw_gn_w, rw_gn_b,
              mla_q_norm_g, mla_w_uq, mla_kv_norm_g, mla_w_ukv, w_branch, w_out, router_w,
              router_bias, moe_w_gate_up, moe_w_down, final_norm_g):
    B, T, D = x.shape
    Tc = ctx.shape[1]
    pos = jnp.arange(T, dtype=jnp.int32)
    xc = ctx
    for layer in range(DEPTH):
        need_ctx = layer < DEPTH - 1
        mod = jax.nn.silu(c) @ w_mod[layer] + b_mod[layer]
        mod_c = jax.nn.silu(c_ctx) @ w_mod[layer] + b_mod[layer]
        sh1, sc1, g1, sh2, sc2, g2 = [m[:, None, :] for m in jnp.split(mod, 6, axis=-1)]
        sh1c, sc1c, g1c, sh2c, sc2c, g2c = jnp.split(mod_c, 6, axis=-1)
        h = rms_norm(x, norm1_g[layer]) * (1.0 + sc1) + sh1
        hc = rms_norm(xc, norm1_g[layer]) * (1.0 + sc1c) + sh1c
        y, yc = hybrid_mixer(h, hc, pos, w_in[layer], na_rpb[layer], rw_shift_mu[layer], rw_w0[layer],
                             rw_w_up[layer], rw_a0[layer], rw_a_up[layer], rw_g_up[layer], rw_k_k[layer],
                             rw_k_a[layer], rw_r_k[layer], rw_gn_w[layer], rw_gn_b[layer],
                             mla_q_norm_g[layer], mla_w_uq[layer], mla_kv_norm_g[layer], mla_w_ukv[layer],
                             w_branch[layer], w_out[layer], need_ctx)
        x = x + g1 * y
        h2 = rms_norm(x, norm2_g[layer]) * (1.0 + sc2) + sh2
        if need_ctx:
            xc = xc + g1c * yc
            h2c = rms_norm(xc, norm2_g[layer]) * (1.0 + sc2c) + sh2c
            f = moe_ffn(jnp.concatenate([h2.reshape(-1, D), h2c.reshape(-1, D)], axis=0),
                        router_w, router_bias, moe_w_gate_up[layer], moe_w_down[layer])
            x = x + g2 * f[:B * T].reshape(B, T, D)
            xc = xc + g2c * f[B * T:].reshape(B, Tc, D)
        else:
            f = moe_ffn(h2.reshape(-1, D), router_w, router_bias, moe_w_gate_up[layer], moe_w_down[layer])
            x = x + g2 * f.reshape(B, T, D)
    return rms_norm(x, final_norm_g)
```

```python
import numpy as np
import concourse.bass as bass
import concourse.mybir as mybir
from contextlib import ExitStack

F32 = mybir.dt.float32
BF16 = mybir.dt.bfloat16
AF = mybir.ActivationFunctionType
ALU = mybir.AluOpType
AX = mybir.AxisListType

SEM_ROT = 30000
N_DMA_SEMS = 40


class V:
    __slots__ = ("ap", "buf", "key")

    def __init__(self, ap, buf, key=None):
        self.ap = ap
        self.buf = buf
        self.key = key

    def __getitem__(self, idx):
        return V(self.ap[idx], self.buf, self.key)

    def m(self, fn):
        return V(fn(self.ap), self.buf, self.key)

    def re(self, s, **kw):
        return V(self.ap.rearrange(s, **kw), self.buf, self.key)

    def bc(self, dt):
        return V(self.ap.bitcast(dt), self.buf, self.key)

    def k(self, key):
        return V(self.ap, self.buf, key)


class Buf:
    def __init__(self, name, t):
        self.name = name
        self.t = t
        self.state = {None: [None, []]}
        self.psum = False
        self.bank_ev = None

    def ap(self):
        t = self.t
        return t.ap() if hasattr(t, "ap") and callable(getattr(t, "ap")) and not isinstance(t, bass.AP) else t

    def __getitem__(self, idx):
        return V(self.ap()[idx], self, None)

    def v(self):
        return V(self.ap(), self, None)

    def part(self, key):
        if key not in self.state:
            w, r = self.state[None]
            self.state[key] = [w, list(r)]
        return V(self.ap(), self, key)

    def keys_for(self, key):
        if key is None:
            return list(self.state.keys())
        if isinstance(key, tuple):
            out = []
            for k in key:
                out += self.keys_for(k)
            return out
        if key not in self.state:
            w, r = self.state[None]
            self.state[key] = [w, list(r)]
        return [key]


class Prog:
    def __init__(self, nc):
        self.nc = nc
        self.eng = {"pe": nc.tensor, "act": nc.scalar, "dve": nc.vector, "pool": nc.gpsimd, "sp": nc.sync}
        self.esem = {}
        self.ecnt = {}
        self.etot = {}
        for e in self.eng:
            self.esem[e] = nc.alloc_semaphore(f"es_{e}_0")
            self.ecnt[e] = 0
            self.etot[e] = 0
        self.waited = {e: {} for e in self.eng}
        self.dsems = [nc.alloc_semaphore(f"dma_{i}") for i in range(N_DMA_SEMS)]
        self.dcnt = [0] * N_DMA_SEMS
        self.dnext = 0
        self.all_sems = {}
        self.ninst = 0
        self.pe_sems = {id(self.esem["pe"])}

    def _wait(self, e, ev):
        if ev is None:
            return
        sem, val = ev
        sid = id(sem)
        self.all_sems[sid] = sem
        if self.waited[e].get(sid, 0) >= val:
            return
        self.eng[e].wait_ge(sem, val)
        self.waited[e][sid] = val

    def _wait2(self, e, ev):
        if ev is not None and e == "pe" and id(ev[0]) in self.pe_sems:
            return
        self._wait(e, ev)

    def _bank(self, e, vs):
        for v in vs:
            if v.buf.psum and v.buf.bank_ev is not None and v.buf.bank_ev[1] != e:
                self._wait(e, v.buf.bank_ev[0])

    def _deps(self, e, reads, writes):
        self._bank(e, list(reads) + list(writes))
        for v in reads:
            for k in v.buf.keys_for(v.key):
                self._wait2(e, v.buf.state[k][0])
        for v in writes:
            for k in v.buf.keys_for(v.key):
                st = v.buf.state[k]
                self._wait2(e, st[0])
                for ev in st[1]:
                    self._wait2(e, ev)

    def _record(self, ev, reads, writes, e=None):
        for v in list(reads) + list(writes):
            if v.buf.psum:
                v.buf.bank_ev = (ev, e)
        for v in reads:
            for k in v.buf.keys_for(v.key):
                v.buf.state[k][1].append(ev)
        for v in writes:
            for k in v.buf.keys_for(v.key):
                v.buf.state[k][0] = ev
                v.buf.state[k][1] = []

    def _newev(self, e, inst):
        if self.ecnt[e] >= SEM_ROT:
            self.esem[e] = self.nc.alloc_semaphore(f"es_{e}_{self.etot[e]}")
            self.ecnt[e] = 0
            if e == "pe":
                self.pe_sems.add(id(self.esem[e]))
        self.ecnt[e] += 1
        self.etot[e] += 1
        inst.then_inc(self.esem[e], 1)
        ev = (self.esem[e], self.ecnt[e])
        self.waited[e][id(self.esem[e])] = 0 if id(self.esem[e]) not in self.waited[e] else self.waited[e][id(self.esem[e])]
        self.all_sems[id(self.esem[e])] = self.esem[e]
        self.last_ev = getattr(self, "last_ev", {})
        self.last_ev[e] = ev
        return ev

    def op(self, e, fn, reads, writes, nosync_same=False):
        self._deps(e, reads, writes)
        inst = fn()
        ev = self._newev(e, inst)
        self._record(ev, reads, writes, e)
        self.ninst += 1
        return inst

    def group(self, e, fns, reads, writes):
        self._deps(e, reads, writes)
        inst = None
        for fn in fns:
            inst = fn()
            self.ninst += 1
        ev = self._newev(e, inst)
        self._record(ev, reads, writes, e)
        return inst

    def dma(self, q, out, in_, **kw):
        i = self.dnext
        self.dnext = (self.dnext + 1) % N_DMA_SEMS
        sem = self.dsems[i]
        if self.dcnt[i] > 0:
            self._wait(q, (sem, self.dcnt[i]))
        self._deps(q, [in_], [out])
        inst = self.eng[q].dma_start(out=out.ap, in_=in_.ap, **kw)
        inst.then_inc(sem, 16)
        self.dcnt[i] += 16
        ev = (sem, self.dcnt[i])
        self.all_sems[id(sem)] = sem
        self._record(ev, [in_], [out])
        self.ninst += 1
        return ev

    def drain(self):
        evs = []
        for e in self.eng:
            if self.ecnt[e] > 0:
                evs.append((self.esem[e], self.ecnt[e]))
        for i in range(N_DMA_SEMS):
            if self.dcnt[i] > 0:
                evs.append((self.dsems[i], self.dcnt[i]))
        for e in self.eng:
            for ev in evs:
                self._wait(e, ev)

    def mm(self, out, lhsT, rhs, start=True, stop=True):
        return self.op("pe", lambda: self.nc.tensor.matmul(out.ap, lhsT.ap, rhs.ap, start=start, stop=stop),
                       [lhsT, rhs] + ([] if start else [out]), [out])

    def mmg(self, out, pairs):
        n = len(pairs)
        fns = []
        reads = []
        for j, (l, r) in enumerate(pairs):
            fns.append((lambda l=l, r=r, j=j: self.nc.tensor.matmul(out.ap, l.ap, r.ap, start=(j == 0), stop=(j == n - 1))))
            reads += [l, r]
        return self.group("pe", fns, reads, [out])

    def transpose(self, out, in_, ident):
        return self.op("pe", lambda: self.nc.tensor.transpose(out.ap, in_.ap, ident.ap), [in_, ident], [out])

    def act(self, out, in_, func, bias=None, scale=None, accum=None, e="act"):
        kw = {}
        reads = [in_]
        if bias is not None:
            if isinstance(bias, V):
                kw["bias"] = bias.ap
                reads.append(bias)
            else:
                kw["bias"] = bias
        if scale is not None:
            if isinstance(scale, V):
                kw["scale"] = scale.ap
                reads.append(scale)
            else:
                kw["scale"] = scale
        writes = [out]
        if accum is not None:
            kw["accum_out"] = accum.ap
            writes.append(accum)
        return self.op("act", lambda: self.nc.scalar.activation(out.ap, in_.ap, func, **kw), reads, writes)

    def tt(self, out, a, b, op, e="dve"):
        return self.op(e, lambda: self.eng[e].tensor_tensor(out.ap, a.ap, b.ap, op), [a, b], [out])

    def ts(self, out, a, s1, s2, op0, op1=None, accum=None, e="dve"):
        reads = [a]
        s1a = s1
        s2a = s2
        if isinstance(s1, V):
            reads.append(s1)
            s1a = s1.ap
        if isinstance(s2, V):
            reads.append(s2)
            s2a = s2.ap
        writes = [out]
        kw = {}
        if op1 is not None:
            kw["op1"] = op1
        if accum is not None:
            kw["accum_out"] = accum.ap
            writes.append(accum)
        return self.op(e, lambda: self.eng[e].tensor_scalar(out.ap, a.ap, s1a, s2a, op0, **kw), reads, writes)

    def stt(self, out, a, s, b, op0, op1, accum=None):
        reads = [a, b]
        sa = s
        if isinstance(s, V):
            reads.append(s)
            sa = s.ap
        writes = [out]
        kw = {}
        if accum is not None:
            kw["accum_out"] = accum.ap
            writes.append(accum)
        return self.op("dve", lambda: self.nc.vector.scalar_tensor_tensor(out.ap, a.ap, sa, b.ap, op0, op1, **kw), reads, writes)

    def copy(self, out, in_, e="dve"):
        if e == "act":
            return self.op("act", lambda: self.nc.scalar.copy(out.ap, in_.ap), [in_], [out])
        return self.op(e, lambda: self.eng[e].tensor_copy(out.ap, in_.ap), [in_], [out])

    def memset(self, out, val, e="dve"):
        return self.op(e, lambda: self.eng[e].memset(out.ap, val), [], [out])

    def reduce(self, out, in_, op, axis=None, e="dve"):
        axis = axis or AX.X
        return self.op(e, lambda: self.eng[e].tensor_reduce(out.ap, in_.ap, axis, op), [in_], [out])

    def recip(self, out, in_):
        return self.op("dve", lambda: self.nc.vector.reciprocal(out.ap, in_.ap), [in_], [out])


class Stage:
    uid = 0

    def __init__(self, P):
        self.P = P
        self.es = ExitStack()

    def __enter__(self):
        self.es.__enter__()
        return self

    def sb(self, name, shape, dtype=F32):
        Stage.uid += 1
        name = f"{name}_s{Stage.uid}"
        t = self.es.enter_context(self.P.nc.sbuf_tensor(name, list(shape), dtype))
        return Buf(name, t)

    def ps(self, name, shape, dtype=F32):
        Stage.uid += 1
        name = f"{name}_p{Stage.uid}"
        t = self.es.enter_context(self.P.nc.psum_tensor(name, list(shape), dtype))
        b = Buf(name, t)
        b.psum = True
        return b

    def __exit__(self, *a):
        self.P.drain()
        return self.es.__exit__(*a)

from concourse.bass_utils import run_bass_kernel_spmd
import ml_dtypes
import math

DM = 2048
T = 2048
TC = 256
NT = T + TC
NTILE = NT // 128
IN_W = 13504
IN_WP = 13568
OFF_NA = 0
OFF_RW = 3072
OFF_CQ = 6560
OFF_CKV = 7072
OFF_KR = 7328
OFF_G = 7360
TOKCH = [(0, 512), (512, 512), (1024, 512), (1536, 512), (2048, 256)]


class NS:
    pass


def dram(nc, name, shape, dtype, kind="Internal"):
    return Buf(name, nc.dram_tensor(name, list(shape), dtype, kind=kind))


def stage_mod(P, D, L):
    with Stage(P) as st:
        cT = st.sb("cT", [128, 16, 2], F32)
        cs = st.sb("cs", [128, 16, 2], F32)
        P.dma("sp", cT.v(), D.cT.v())
        P.act(cs.v(), cT.v(), AF.Silu)
        wb = [st.sb(f"wm{i}", [128, 16, 512], F32) for i in range(2)]
        bt = [st.sb(f"bm{i}", [2, 512], F32) for i in range(2)]
        ot = [st.sb(f"om{i}", [2, 512], F32) for i in range(2)]
        ps = [st.ps(f"pm{i}", [2, 512], F32) for i in range(2)]
        for j in range(24):
            w = wb[j % 2]
            for q4 in range(4):
                P.dma("sp", w[:, q4 * 4:(q4 + 1) * 4, :],
                      D.w_mod[L, q4 * 512:(q4 + 1) * 512, j * 512:(j + 1) * 512].re("(kc p) n -> p kc n", p=128))
            P.dma("sp", bt[j % 2].v(), D.b_mod[L:L + 1, j * 512:(j + 1) * 512].m(lambda a: a.broadcast_to([2, 512])))
            P.mmg(ps[j % 2].v(), [(cs[:, kc, :], w[:, kc, :]) for kc in range(16)])
            P.tt(ot[j % 2].v(), ps[j % 2].v(), bt[j % 2].v(), ALU.add)
            P.dma("sp", D.modv[L][:, j * 512:(j + 1) * 512], ot[j % 2].v())


def bc_row(v, n=128):
    return v.m(lambda a: a.broadcast_to([n, a.shape[-1]]))


def stage_norm(P, D, L, which, hT, st_outer):
    gsrc = D.norm1_g if which == 0 else D.norm2_g
    sh_i, sc_i = (0, 1) if which == 0 else (3, 4)
    with Stage(P) as st:
        g = st.sb("ng", [128, DM], F32)
        P.dma("sp", g.v(), bc_row(gsrc[L:L + 1, :]))
        A = []
        Bs = []
        for r in range(2):
            a = st.sb(f"nA{r}", [128, DM], F32)
            b = st.sb(f"nB{r}", [128, DM], F32)
            P.dma("sp", a.v(), bc_row(D.modv[L][r:r + 1, sc_i * DM:(sc_i + 1) * DM]))
            P.dma("sp", b.v(), bc_row(D.modv[L][r:r + 1, sh_i * DM:(sh_i + 1) * DM]))
            P.stt(a.v(), a.v(), 1.0, g.v(), ALU.add, ALU.mult)
            A.append(a)
            Bs.append(b)
        xt = [st.sb(f"nx{i}", [128, DM], F32) for i in range(2)]
        sq = st.sb("nsq", [128, DM], F32)
        hb = [st.sb(f"nh{i}", [128, DM], BF16) for i in range(2)]
        ss = [st.sb(f"nss{i}", [128, 1], F32) for i in range(2)]
        pt = [st.ps(f"npt{i}", [128, 16, 128], BF16) for i in range(2)]
        for t in range(NTILE):
            r = 0 if t < 16 else 1
            x = xt[t % 2]
            s = ss[t % 2]
            h = hb[t % 2]
            p = pt[t % 2]
            P.dma("sp", x.v(), D.xcur[t * 128:(t + 1) * 128, :])
            P.act(sq.v(), x.v(), AF.Square, accum=s.v())
            P.ts(s.v(), s.v(), 1.0 / DM, 1e-6, ALU.mult, ALU.add)
            P.act(s.v(), s.v(), AF.Sqrt)
            P.recip(s.v(), s.v())
            P.stt(sq.v(), x.v(), s.v(), A[r].v(), ALU.mult, ALU.mult)
            P.tt(h.v(), sq.v(), Bs[r].v(), ALU.add)
            for c in range(16):
                P.transpose(p[:, c, :], h[:, c * 128:(c + 1) * 128], D.ident_bf.v())
            P.copy(hT[:, :, t * 128:(t + 1) * 128], p.v(), e=("act" if t % 2 else "dve"))


def stage_win(P, D, L, hT):
    with Stage(P) as st:
        wb = [st.sb(f"ww{i}", [128, 16, 512], BF16) for i in range(2)]
        ob = [st.sb(f"wo{i}", [128, NT], BF16) for i in range(2)]
        ps = [st.ps(f"wp{i}", [128, 512], F32) for i in range(4)]
        nslab = (IN_W + 511) // 512
        pi = 0
        oi = 0
        for s in range(nslab):
            c0 = s * 512
            cw = min(512, IN_W - c0)
            w = wb[s % 2]
            for q4 in range(4):
                P.dma("pool", w[:, q4 * 4:(q4 + 1) * 4, :cw],
                      D.w_in[L, q4 * 512:(q4 + 1) * 512, c0:c0 + cw].re("(kc p) n -> p kc n", p=128))
            tokmajor = (2048 <= c0 < 3072)
            if tokmajor:
                for t in range(NTILE):
                    p = ps[pi % 4]
                    pi += 1
                    P.mmg(p.v(), [(hT[:, kc, t * 128:(t + 1) * 128], w[:, kc, :]) for kc in range(16)])
                    o = ob[oi % 2]
                    oi += 1
                    P.copy(o[:, :512], p.v(), e=("act" if t % 2 else "dve"))
                    P.dma("sp", D.utok[t * 128:(t + 1) * 128, c0 - 2048:c0 - 2048 + 512], o[:, :512])
                continue
            for blk in range((cw + 127) // 128):
                m = min(128, cw - blk * 128)
                o = ob[oi % 2]
                oi += 1
                for ci, (t0, tn) in enumerate(TOKCH):
                    p = ps[pi % 4]
                    pi += 1
                    P.mmg(p[:m, :tn], [(w[:, kc, blk * 128:blk * 128 + m], hT[:, kc, t0:t0 + tn]) for kc in range(16)])
                    P.copy(o[:m, t0:t0 + tn], p[:m, :tn], e=("act" if ci % 2 else "dve"))
                r0 = c0 + blk * 128
                P.dma("sp", D.uT[r0:r0 + m, :], o[:m, :])


def na_kr0(j):
    return min(max(2 * j - 4, 0), 22)


def na_pat(j):
    return 0 if j == 0 else 1 if j == 1 else 2 if j <= 13 else 3 if j == 14 else 4


def stage_na(P, D, L, need_ctx):
    with Stage(P) as st:
        qT = [st.sb(f"naq{i}", [64, NT], BF16) for i in range(2)]
        kT = [st.sb(f"nak{i}", [64, NT], BF16) for i in range(2)]
        va = [st.sb(f"nav{i}", [128, NTILE, 65], BF16) for i in range(2)]
        bias = [st.sb(f"nab{i}", [128, 5, 5, 128], F32) for i in range(2)]
        eloc = [st.sb(f"nae{i}", [128, 5, 128], F32) for i in range(2)]
        pT = [st.sb(f"nap{i}", [128, 7, 128], BF16) for i in range(2)]
        rec = [st.sb(f"nar{i}", [128, 1], F32) for i in range(2)]
        ytok = st.sb("nay", [128, NTILE, 64], BF16)
        yT = [st.sb(f"nayT{i}", [64, NT], BF16) for i in range(2)]
        ps_s = [st.ps(f"nps{i}", [128, 8, 128], F32) for i in range(2)]
        ps_o = [st.ps(f"npo{i}", [128, 65], F32) for i in range(2)]
        ps_t = st.ps("npt", [64, 8, 128], BF16)
        for i in range(2):
            P.memset(va[i][:, :, 64:65], 1.0)
        ntq = NTILE if need_ctx else 16
        u = 0
        for h in range(16):
            b = h % 2
            P.dma("sp", qT[b].v(), D.uT[h * 64:(h + 1) * 64, :])
            P.dma("sp", kT[b].v(), D.uT[1024 + h * 64:1024 + (h + 1) * 64, :])
            P.dma("sp", va[b][:, :, 0:64], D.utok[:, h * 64:(h + 1) * 64].re("(c p) d -> p c d", p=128))
            P.dma("sp", bias[b].v(), D.na_bias[L, h])
            for j in range(ntq):
                s = ps_s[u % 2]
                o = ps_o[u % 2]
                e_ = eloc[u % 2]
                p_ = pT[u % 2]
                r_ = rec[u % 2]
                u += 1
                q = qT[b][:, j * 128:(j + 1) * 128]
                if j < 16:
                    t0 = na_kr0(j) // 2
                    tiles = [t0 + c for c in range(5)] + [16, 17]
                else:
                    tiles = [16, 17]
                nk = len(tiles)
                P.group("pe", [(lambda c=c, tt_=tt_: P.nc.tensor.matmul(s[:, c, :].ap, kT[b][:, tt_ * 128:(tt_ + 1) * 128].ap, q.ap, start=True, stop=True))
                               for c, tt_ in enumerate(tiles)], [kT[b].v(), qT[b].v()], [s.v()])
                if j < 16:
                    P.stt(e_.v(), s[:, 0:5, :], 0.125, bias[b][:, na_pat(j), :, :], ALU.mult, ALU.add)
                    P.act(p_[:, 0:5, :], e_.v(), AF.Exp)
                    P.act(p_[:, 5:7, :], s[:, 5:7, :], AF.Exp, scale=0.125)
                else:
                    P.act(p_[:, 0:2, :], s[:, 0:2, :], AF.Exp, scale=0.125)
                P.mmg(o.v(), [(p_[:, c, :], va[b][:, tt_, :]) for c, tt_ in enumerate(tiles)])
                P.recip(r_.v(), o[:, 64:65])
                P.ts(ytok[:, j, :], o[:, 0:64], r_.v(), None, ALU.mult)
            for g0 in range(0, ntq, 8):
                gn = min(8, ntq - g0)
                for jj in range(gn):
                    P.transpose(ps_t[:, jj, :], ytok[:, g0 + jj, :], D.ident_bf.v())
                P.copy(yT[b][:, g0 * 128:(g0 + gn) * 128], ps_t[:, 0:gn, :], e="act")
            P.dma("sp", D.ys[0][h * 64:(h + 1) * 64, 0:ntq * 128], yT[b][:, 0:ntq * 128])


def stage_mla_prep(P, D, L, need_ctx):
    with Stage(P) as st:
        cq = st.sb("mcq", [128, 4, NT], BF16)
        ckv = st.sb("mckv", [128, 2, NT], BF16)
        kr = st.sb("mkr", [96, NT], BF16)
        krr = st.sb("mkrr", [96, NT], BF16)
        cs = st.sb("mcs", [96, NT], F32)
        sg = st.sb("msg", [96, NT], F32)
        perm = st.sb("mperm", [96, 96], BF16)
        ones = st.sb("mones", [128, 128], BF16)
        gq = st.sb("mgq", [128, 4], F32)
        gkv = st.sb("mgkv", [128, 2], F32)
        wq = st.sb("mwq", [128, 4, 1536], BF16)
        wkv = st.sb("mwkv", [128, 2, 2048], BF16)
        P.dma("sp", cq.v(), D.uT[OFF_CQ:OFF_CQ + 512, :].re("(c p) t -> p c t", p=128))
        P.dma("sp", ckv.v(), D.uT[OFF_CKV:OFF_CKV + 256, :].re("(c p) t -> p c t", p=128))
        P.memset(kr.v(), 0.0)
        P.dma("sp", kr[64:96, :], D.uT[OFF_KR:OFF_KR + 32, :])
        P.dma("sp", cs[64:96, :], D.rope_c.v())
        P.dma("sp", sg[64:96, :], D.rope_s.v())
        P.dma("pool", perm.v(), D.perm96.v())
        P.memset(ones.v(), 1.0)
        P.dma("sp", gq.v(), D.mla_q_norm_g[L].re("(c p) -> p c", p=128), allow_slow_non_contiguous=True)
        P.dma("sp", gkv.v(), D.mla_kv_norm_g[L].re("(c p) -> p c", p=128), allow_slow_non_contiguous=True)
        for kc in range(4):
            P.dma("pool", wq[:, kc, :], D.mla_w_uq[L, kc * 128:(kc + 1) * 128, :])
        for kc in range(2):
            P.dma("pool", wkv[:, kc, :], D.mla_w_ukv[L, kc * 128:(kc + 1) * 128, :])
        sq = st.sb("msq", [128, 4, 512], BF16)
        rs = st.sb("mrs", [128, 512], F32)
        ps = [st.ps(f"mps{i}", [128, 512], F32) for i in range(2)]
        psb = [st.ps(f"mpb{i}", [96, 512], F32) for i in range(2)]
        for (src, g, nblk) in ((cq, gq, 4), (ckv, gkv, 2)):
            for ci, (t0, tn) in enumerate(TOKCH):
                p = ps[ci % 2]
                P.act(sq[:, 0:nblk, :tn], src[:, :, t0:t0 + tn], AF.Square)
                P.mmg(p[:, :tn], [(ones.v(), sq[:, c, :tn]) for c in range(nblk)])
                P.ts(rs[:, :tn], p[:, :tn], 1.0 / (128 * nblk), 1e-6, ALU.mult, ALU.add)
                P.act(rs[:, :tn], rs[:, :tn], AF.Sqrt)
                P.recip(rs[:, :tn], rs[:, :tn])
                for c in range(nblk):
                    P.stt(src[:, c, t0:t0 + tn], src[:, c, t0:t0 + tn], g[:, c:c + 1], rs[:, :tn], ALU.mult, ALU.mult)
        tmp = st.sb("mtmp", [96, 512], F32)
        tmp2 = st.sb("mtmp2", [96, 512], F32)
        for ci, (t0, tn) in enumerate(TOKCH):
            p = psb[ci % 2]
            P.mm(p[:, :tn], perm.v(), kr[:, t0:t0 + tn])
            P.tt(tmp[64:96, :tn], p[64:96, :tn], sg[64:96, t0:t0 + tn], ALU.mult)
            P.tt(tmp2[64:96, :tn], kr[64:96, t0:t0 + tn], cs[64:96, t0:t0 + tn], ALU.mult)
            P.tt(krr[64:96, t0:t0 + tn], tmp[64:96, :tn], tmp2[64:96, :tn], ALU.add)
        for h in range(16):
            P.dma("sp", D.kmT[h, 64:96, :], krr[64:96, :])
        qh = [st.sb(f"mqh{i}", [96, NT], BF16) for i in range(2)]
        kh = [st.sb(f"mkh{i}", [64, NT], BF16) for i in range(2)]
        for h in range(16):
            q_ = qh[h % 2]
            k_ = kh[h % 2]
            for ci, (t0, tn) in enumerate(TOKCH):
                if t0 >= T and not need_ctx:
                    continue
                pa = ps[ci % 2]
                pb = psb[ci % 2]
                P.mmg(pa[:96, :tn], [(wq[:, kc, h * 96:(h + 1) * 96], cq[:, kc, t0:t0 + tn]) for kc in range(4)])
                P.copy(q_[:, t0:t0 + tn], pa[:96, :tn], e="act")
                P.mm(pb[:, :tn], perm.v(), q_[:, t0:t0 + tn])
                P.tt(tmp[64:96, :tn], pb[64:96, :tn], sg[64:96, t0:t0 + tn], ALU.mult)
                P.tt(tmp2[64:96, :tn], pa[64:96, :tn], cs[64:96, t0:t0 + tn], ALU.mult)
                P.tt(q_[64:96, t0:t0 + tn], tmp[64:96, :tn], tmp2[64:96, :tn], ALU.add)
            nq = NT if need_ctx else T
            P.dma("sp", D.qmT[h, :, 0:nq], q_[:, 0:nq])
            for ci, (t0, tn) in enumerate(TOKCH):
                pa = ps[ci % 2]
                P.mmg(pa[:64, :tn], [(wkv[:, kc, h * 128:h * 128 + 64], ckv[:, kc, t0:t0 + tn]) for kc in range(2)])
                P.copy(k_[:, t0:t0 + tn], pa[:64, :tn], e="act")
            P.dma("sp", D.kmT[h, 0:64, :], k_.v())
        vo = [st.sb(f"mvo{i}", [128, 1024], BF16) for i in range(2)]
        wv = wkv.v().re("p kc (h two d) -> p kc h two d", two=2, d=64)
        for t in range(NTILE):
            o = vo[t % 2]
            for half in range(2):
                pa = ps[half]
                P.mmg(pa.v().re("p (h d) -> p h d", d=64),
                      [(ckv[:, kc, t * 128:(t + 1) * 128], wv[:, kc, half * 8:(half + 1) * 8, 1, :]) for kc in range(2)])
                P.copy(o[:, half * 512:(half + 1) * 512], pa.v(), e=("act" if half else "dve"))
            P.dma("sp", D.vmtok[t * 128:(t + 1) * 128, :], o.v())


def stage_mla_attn(P, D, L, need_ctx):
    sc = 1.0 / math.sqrt(96.0)
    with Stage(P) as st:
        qT = [st.sb(f"aq{i}", [96, NT], BF16) for i in range(2)]
        kT = [st.sb(f"ak{i}", [96, NT], BF16) for i in range(2)]
        va = [st.sb(f"av{i}", [128, NTILE, 65], BF16) for i in range(2)]
        pT = [st.sb(f"ap{i}", [128, 512], BF16) for i in range(3)]
        rec = [st.sb(f"ar{i}", [128, 1], F32) for i in range(2)]
        ytok = st.sb("ay", [128, NTILE, 64], BF16)
        yT = [st.sb(f"ayT{i}", [64, NT], BF16) for i in range(2)]
        ps_s = [st.ps(f"aps{i}", [128, 512], F32) for i in range(2)]
        ps_o = [st.ps(f"apo{i}", [128, 65], F32) for i in range(4)]
        ps_t = st.ps("apt", [64, 8, 128], BF16)
        for i in range(2):
            P.memset(va[i][:, :, 64:65], 1.0)
        ntq = NTILE if need_ctx else 16
        u = 0
        for h in range(16):
            b = h % 2
            nq = NT if need_ctx else T
            P.dma("sp", qT[b][:, 0:nq], D.qmT[h, :, 0:nq])
            P.dma("sp", kT[b].v(), D.kmT[h])
            P.dma("sp", va[b][:, :, 0:64], D.vmtok[:, h * 64:(h + 1) * 64].re("(c p) d -> p c d", p=128))
            blocks = [(0, 512, list(range(18))), (512, 512, list(range(18))), (1024, 512, list(range(18))), (1536, 512, list(range(18)))]
            if need_ctx:
                blocks.append((2048, 256, [16, 17]))
            for (q0, qn, tiles) in blocks:
                nqi = qn // 128
                for ci, tt_ in enumerate(tiles):
                    s = ps_s[u % 2]
                    p_ = pT[u % 3]
                    u += 1
                    P.mm(s[:, :qn], kT[b][:, tt_ * 128:(tt_ + 1) * 128], qT[b][:, q0:q0 + qn])
                    P.act(p_[:, :qn], s[:, :qn], AF.Exp, scale=sc)
                    for qi in range(nqi):
                        P.mm(ps_o[qi].v(), p_[:, qi * 128:(qi + 1) * 128], va[b][:, tt_, :], start=(ci == 0), stop=(ci == len(tiles) - 1))
                for qi in range(nqi):
                    r_ = rec[qi % 2]
                    P.recip(r_.v(), ps_o[qi][:, 64:65])
                    P.ts(ytok[:, q0 // 128 + qi, :], ps_o[qi][:, 0:64], r_.v(), None, ALU.mult)
            for g0 in range(0, ntq, 8):
                gn = min(8, ntq - g0)
                for jj in range(gn):
                    P.transpose(ps_t[:, jj, :], ytok[:, g0 + jj, :], D.ident_bf.v())
                P.copy(yT[b][:, g0 * 128:(g0 + gn) * 128], ps_t[:, 0:gn, :], e="dve")
            P.dma("sp", D.ys[2][h * 64:(h + 1) * 64, 0:ntq * 128], yT[b][:, 0:ntq * 128])


def ek(a, b):
    return tuple(f"e{i}" for i in range(a, b))


def rw_shift(P, X, om, hm, out, tmp, tmp2, n_):
    for (a, n) in ((0, T), (T, TC)):
        P.tt(tmp[:n_, a + 1:a + n - 1], X[:n_, a:a + n - 2], X[:n_, a + 2:a + n], ALU.add, e="pool")
        P.copy(tmp[:n_, a:a + 1], X[:n_, a + 1:a + 2], e="pool")
        P.copy(tmp[:n_, a + n - 1:a + n], X[:n_, a + n - 2:a + n - 1], e="pool")
    P.ts(tmp2[:n_], X[:n_], om, None, ALU.mult)
    P.stt(out[:n_], tmp[:n_], hm, tmp2[:n_], ALU.mult, ALU.add)


def stage_rw(P, D, L, need_ctx, heads=range(16), dbgd=None):
    C0 = math.exp(-0.5)
    nc = P.nc
    NS_ = 4
    with Stage(P) as st:
        masks = [st.sb(f"rwm{i}", [128, 128], F32) for i in range(4)]
        for i in range(4):
            P.dma("sp", masks[i].v(), D.tri_masks[i])
        identf = st.sb("rwid", [128, 128], F32)
        P.dma("sp", identf.v(), D.ident_f.v())
        ones_f = st.sb("rwof", [64, 64], F32)
        P.memset(ones_f.v(), 1.0)
        rmask = st.sb("rwrm", [64, NTILE, 128], BF16)
        P.memset(rmask.v(), 1.0)
        P.memset(rmask[:, :, 0:1], 0.0)

        def col16(name, src):
            t = st.sb(name, [64, 16], F32)
            P.dma("sp", t.v(), src.re("(h p) -> p h", p=64), allow_slow_non_contiguous=True)
            return t

        def col1(name, src, n):
            t = st.sb(name, [n, 1], F32)
            P.dma("sp", t.v(), src.re("(p o) -> p o", o=1), allow_slow_non_contiguous=True)
            return t

        kk_c = col16("ckk", D.rw_k_k[L])
        ka_c = col16("cka", D.rw_k_a[L])
        rk_c = col16("crk", D.rw_r_k[L].re("h d -> (h d)"))
        gnw_c = col16("cgw", D.rw_gn_w[L])
        gnb_c = col16("cgb", D.rw_gn_b[L])
        w0_c = [col16(f"cw0{z}", D.rw_w0[L, z]) for z in range(2)]
        a0_c = [col16(f"ca0{z}", D.rw_a0[L, z]) for z in range(2)]
        omka = st.sb("comka", [64, 16], F32)
        P.ts(omka.v(), ka_c.v(), -1.0, 1.0, ALU.mult, ALU.add)
        mu = D.rw_shift_mu[L]
        mus = {}
        for nm, src, kind in (("r", mu[0:1024], 16), ("k", mu[1024:2048], 16), ("v", mu[2048:3072], 16),
                              ("wd", mu[3072:3200], 128), ("ad", mu[3200:3328], 128), ("gd", mu[3328:3456], 128), ("gd2", mu[3456:3488], 32)):
            m_ = col16("mu" + nm, src) if kind == 16 else col1("mu" + nm, src, kind)
            shp = [64, 16] if kind == 16 else [kind, 1]
            om = st.sb("om" + nm, shp, F32)
            hm = st.sb("hm" + nm, shp, F32)
            P.ts(om.v(), m_.v(), -1.0, 1.0, ALU.mult, ALU.add)
            P.ts(hm.v(), m_.v(), 0.5, None, ALU.mult)
            mus[nm] = (om, hm)

        wup = st.sb("rwwup", [128, 1024], BF16)
        aup = st.sb("rwaup", [128, 1024], BF16)
        gup = st.sb("rwgup", [128, 1024], BF16)
        gup2 = st.sb("rwgup2", [32, 1024], BF16)
        P.dma("pool", wup.v(), D.rw_w_up[L].re("z l c -> (z l) c"))
        P.dma("pool", aup.v(), D.rw_a_up[L].re("z l c -> (z l) c"))
        P.dma("pool", gup.v(), D.rw_g_up[L, 0:128, :])
        P.dma("pool", gup2.v(), D.rw_g_up[L, 128:160, :])

        tA = st.sb("rwtA", [128, NT], BF16)
        tB = st.sb("rwtB", [128, NT], BF16)
        xin = st.sb("rwxin", [128, NT], BF16)
        twd = st.sb("rwtwd", [128, NT], BF16)
        ads = st.sb("rwads", [128, NT], BF16)
        sgd = st.sb("rwsgd", [128, NT], BF16)
        sgd2 = st.sb("rwsgd2", [32, NT], BF16)
        base = OFF_RW
        for (dst, r0, n_, key, fn) in ((twd, 3072, 128, "wd", AF.Tanh), (ads, 3200, 128, "ad", AF.Copy),
                                       (sgd, 3328, 128, "gd", AF.Sigmoid), (sgd2, 3456, 32, "gd2", AF.Sigmoid)):
            P.dma("sp", xin[:n_, :], D.uT[base + r0:base + r0 + n_, :])
            rw_shift(P, xin, mus[key][0].v(), mus[key][1].v(), tA, tA, tB, n_)
            P.act(dst[:n_, :], tA[:n_, :], fn)

        bk = [st.ps(f"rwbk{i}", [128, 512], F32) for i in range(8)]
        r_s = st.sb("rwr", [64, NT], BF16)
        k_s = st.sb("rwk", [64, NT], BF16)
        v_s = st.sb("rwv", [64, NT], BF16)
        g_s = st.sb("rwg", [64, NT], BF16)
        kk = st.sb("rwkk", [64, NT], F32)
        f_sg = st.sb("rwsg", [64, NT], F32)
        f_a = st.sb("rwa", [64, NT], F32)
        f_t = st.sb("rwt", [64, NT], F32)
        f_L = st.sb("rwL", [64, NTILE, 128], F32)
        f_E = st.sb("rwE", [64, NTILE, 128], F32)
        l63 = st.sb("rwl63", [64, NTILE, 1], F32)
        eA = st.sb("rweA", [64, NTILE, 1], F32)
        eB = st.sb("rweB", [64, NTILE, 1], F32)
        scl = st.sb("rwscl", [64, NTILE], F32)
        rT = st.sb("rwrT", [64, NT], BF16)
        kapT = st.sb("rwkapT", [64, NT], BF16)
        ktT = st.sb("rwktT", [64, NT], BF16)
        bT = st.sb("rwbT", [64, NT], BF16)
        tokz = st.sb("rwtokz", [128, NTILE, 3, 64], BF16)
        vtok = st.sb("rwvtok", [128, NTILE, 64], BF16)
        MA = st.sb("rwMA", [64, NTILE, 192], BF16)
        MB = st.sb("rwMB", [128, NTILE, 192], BF16)
        yacc = st.sb("rwy", [64, NT], F32)
        Nsb = [st.sb(f"rwN{i}", [128, 128], F32) for i in range(NS_)]
        NTsb = [st.sb(f"rwNT{i}", [128, 128], F32) for i in range(NS_)]
        PP = [[st.sb(f"rwPP{i}_{j}", [128, 2, 128], F32) for j in range(2)] for i in range(NS_)]
        ZZ = [[st.sb(f"rwZ{i}_{j}", [128, 192], F32) for j in range(2)] for i in range(NS_)]
        rc = [st.sb(f"rwrc{i}", [128, 192], F32) for i in range(NS_)]
        akr = [st.sb(f"rwakr{i}", [128, 128], F32) for i in range(NS_)]
        ST = [st.sb(f"rwST{i}", [64, 64], BF16) for i in range(2)]
        idb = D.ident_bf

        for h in heads:
            hc = slice(h, h + 1)
            for (dst, r0, key) in ((r_s, 0, "r"), (k_s, 1024, "k"), (v_s, 2048, "v")):
                P.dma("sp", xin[:64, :], D.uT[base + r0 + h * 64:base + r0 + (h + 1) * 64, :])
                rw_shift(P, xin, mus[key][0][:, hc], mus[key][1][:, hc], dst, tA, tB, 64)
            for ci, (t0, tn) in enumerate(TOKCH):
                ps = bk[ci % 8]
                P.mmg(ps[:64, :tn], [(gup[:, h * 64:(h + 1) * 64], sgd[:, t0:t0 + tn]), (gup2[:32, h * 64:(h + 1) * 64], sgd2[:32, t0:t0 + tn])])
                P.copy(g_s[:, t0:t0 + tn], ps[:64, :tn], e="act")
            P.ts(f_t.v(), k_s.v(), kk_c[:, hc], None, ALU.mult)
            P.act(f_a.v(), f_t.v(), AF.Square)
            for ci, (t0, tn) in enumerate(TOKCH):
                ps = bk[(ci + 5) % 8]
                P.mm(ps[:64, :tn], ones_f.v(), f_a[:, t0:t0 + tn])
                P.act(f_sg[:, t0:t0 + tn], ps[:64, :tn], AF.Sqrt)
            P.ts(f_sg.v(), f_sg.v(), 1e-12, None, ALU.max)
            P.recip(f_sg.v(), f_sg.v())
            P.tt(kk.v(), f_t.v(), f_sg.v(), ALU.mult)
            for c in range(NTILE):
                pb = bk[c % 8].v().bc(BF16)
                P.transpose(pb[:, 0:64], v_s[:, c * 128:(c + 1) * 128], idb[0:64, 0:64])
                P.copy(vtok[:, c, :], pb[:, 0:64], e="act")

            for z in range(2):
                zs = slice(z * 64, (z + 1) * 64)
                mS, mST, mD = (masks[0], masks[1], masks[2]) if z == 0 else (masks[1], masks[0], masks[3])
                for ci, (t0, tn) in enumerate(TOKCH):
                    ps = bk[ci % 8]
                    P.mm(ps[:64, :tn], wup[zs, h * 64:(h + 1) * 64], twd[zs, t0:t0 + tn])
                    P.act(f_sg[:, t0:t0 + tn], ps[:64, :tn], AF.Sigmoid, bias=w0_c[z][:, hc])
                    ps2 = bk[(ci + 4) % 8]
                    P.mm(ps2[:64, :tn], aup[zs, h * 64:(h + 1) * 64], ads[zs, t0:t0 + tn])
                    P.act(f_a[:, t0:t0 + tn], ps2[:64, :tn], AF.Sigmoid, bias=a0_c[z][:, hc])
                P.ts(f_t.v(), f_a.v(), ka_c[:, hc], omka[:, hc], ALU.mult, ALU.add)
                P.tt(f_t.v(), f_t.v(), k_s.v(), ALU.mult)
                P.tt(f_a.v(), f_a.v(), kk.v(), ALU.mult)
                L2 = f_L.v().re("p c t -> p (c t)")
                P.op("dve", lambda: nc.vector.tensor_tensor_scan(L2.ap, rmask.v().re("p c t -> p (c t)").ap, f_sg.ap() , 0.0, ALU.mult, ALU.add),
                     [rmask.v(), f_sg.v()], [f_L.v()])
                P.copy(l63.v(), f_L[:, :, 63:64])
                P.tt(f_L.v(), f_L.v(), l63.v().m(lambda a: a.broadcast_to([64, NTILE, 128])), ALU.subtract)
                P.tt(f_sg.v(), L2, f_sg.v(), ALU.subtract)
                E2d = f_E.v().re("p c t -> p (c t)")
                P.act(E2d, L2, AF.Exp, scale=-C0)
                P.copy(eA.v(), f_E[:, :, 127:128])
                if z == 0:
                    P.tt(rT.v(), r_s.v(), E2d, ALU.mult)
                P.act(E2d, L2, AF.Exp, scale=C0)
                if z == 0:
                    P.tt(ktT.v(), f_t.v(), E2d, ALU.mult)
                    P.tt(bT.v(), f_a.v(), E2d, ALU.mult)
                else:
                    P.tt(kapT.v(), kk.v(), E2d, ALU.mult)
                P.act(E2d, f_sg.v(), AF.Exp, scale=-C0)
                if z == 0:
                    P.tt(kapT.v(), kk.v(), E2d, ALU.mult)
                else:
                    P.tt(ktT.v(), f_t.v(), E2d, ALU.mult)
                    P.tt(bT.v(), f_a.v(), E2d, ALU.mult)
                P.act(E2d, f_sg.v(), AF.Exp, scale=C0)
                P.copy(eB.v(), f_E[:, :, 0:1])
                if z == 1:
                    P.tt(rT.v(), r_s.v(), E2d, ALU.mult)
                eA2 = eA.v().re("p c o -> p (c o)")
                eB2 = eB.v().re("p c o -> p (c o)")
                P.memset(scl.v(), 1.0)
                if z == 0:
                    P.tt(scl[:, 0:15], eA2[:, 0:15], eB2[:, 1:16], ALU.mult)
                    P.tt(scl[:, 16:17], eA2[:, 16:17], eB2[:, 17:18], ALU.mult)
                    P.tt(scl[:, 17:18], eA2[:, 17:18], eB2[:, 0:1], ALU.mult)
                else:
                    P.tt(scl[:, 1:18], eB2[:, 1:18], eA2[:, 0:17], ALU.mult)
                for c in range(NTILE):
                    cs_ = slice(c * 128, (c + 1) * 128)
                    pb = bk[c % 8].v().bc(BF16)
                    P.transpose(pb[:, 0:64], kapT[:, cs_], idb[0:64, 0:64])
                    P.transpose(pb[:, 64:128], ktT[:, cs_], idb[0:64, 0:64])
                    P.transpose(pb[:, 128:192], bT[:, cs_], idb[0:64, 0:64])
                    P.copy(tokz[:, c, :, :], pb[:, 0:192].re("p (a d) -> p a d", a=3), e=("act" if c % 2 else "dve"))
                for c0 in range(0, NTILE, NS_):
                    units = list(range(c0, min(NTILE, c0 + NS_)))
                    for s, c in enumerate(units):
                        cs_ = slice(c * 128, (c + 1) * 128)
                        b = bk[s]
                        P.mm(b[:, 0:128].k(ek(0, 2)), bT[:, cs_], kapT[:, cs_])
                        P.mm(b[:, 128:256].k(ek(2, 4)), kapT[:, cs_], bT[:, cs_])
                        P.mm(b[:, 256:384].k(ek(4, 6)), kapT[:, cs_], ktT[:, cs_])
                        P.mm(b[:, 384:512].k(ek(6, 8)), bT[:, cs_], rT[:, cs_])
                        P.tt(Nsb[s].v(), b[:, 0:128].k(ek(0, 2)), mS.v(), ALU.mult)
                        P.tt(NTsb[s].v(), b[:, 128:256].k(ek(2, 4)), mST.v(), ALU.mult)
                        P.tt(ZZ[s][0][:, 64:192], b[:, 256:384].k(ek(4, 6)), mST.v(), ALU.mult)
                        P.tt(rc[s][:, 0:128], b[:, 384:512].k(ek(6, 8)), mD.v(), ALU.mult)
                        P.copy(ZZ[s][0][:, 0:64], tokz[:, c, 0, :], e="pool")
                        P.copy(rc[s][:, 128:192], tokz[:, c, 2, :], e="pool")
                    for s, c in enumerate(units):
                        b = bk[s]
                        P.mm(b[:, 256:448].k(ek(4, 7)), Nsb[s].v(), ZZ[s][0].v())
                        P.mm(b[:, 0:128].k(ek(0, 2)), NTsb[s].v(), Nsb[s].v())
                        P.mm(b[:, 128:256].k(ek(2, 4)), Nsb[s].v(), NTsb[s].v())
                        P.tt(ZZ[s][1].v(), ZZ[s][0].v(), b[:, 256:448].k(ek(4, 7)), ALU.subtract)
                        P.copy(PP[s][0].v(), b[:, 0:256].k(ek(0, 4)), e="act")
                    for j in range(1, 7):
                        for s, c in enumerate(units):
                            b = bk[s]
                            pc = PP[s][(j - 1) % 2]
                            pn = PP[s][j % 2]
                            zc = ZZ[s][j % 2]
                            zn = ZZ[s][(j + 1) % 2]
                            P.mm(b[:, 256:448].k(ek(4, 7)), pc[:, 0, :], zc.v())
                            if j < 6:
                                P.mm(b[:, 0:128].k(ek(0, 2)), pc[:, 1, :], pc[:, 0, :])
                                if j < 5:
                                    P.mm(b[:, 128:256].k(ek(2, 4)), pc[:, 0, :], pc[:, 1, :])
                            P.tt(zn.v(), zc.v(), b[:, 256:448].k(ek(4, 7)), ALU.add)
                            if j < 5:
                                P.copy(pn.v(), b[:, 0:256].k(ek(0, 4)), e="act")
                            elif j == 5:
                                P.copy(pn[:, 0, :], b[:, 0:128].k(ek(0, 2)), e="act")
                    for s, c in enumerate(units):
                        cs_ = slice(c * 128, (c + 1) * 128)
                        b = bk[s]
                        zf = ZZ[s][1]
                        P.mm(b[:64, 0:192].k(ek(0, 3)), zf[:, 0:64], rc[s].v())
                        P.mm(b[:, 192:384].k(ek(3, 6)), zf[:, 64:192], rc[s].v())
                        P.mm(b[:, 384:512].k(ek(6, 8)), ktT[:, cs_], rT[:, cs_])
                        P.tt(akr[s].v(), b[:, 384:512].k(ek(6, 8)), mD.v(), ALU.mult)
                        P.tt(MA[:, c, 0:128], rT[:, cs_], b[:64, 0:128].k(ek(0, 2)), ALU.subtract)
                        P.tt(MA[:, c, 128:192], identf[0:64, 0:64], b[:64, 128:192].k(ek(2, 3)), ALU.subtract)
                        P.tt(MB[:, c, 0:128], akr[s].v(), b[:, 192:320].k(ek(3, 5)), ALU.subtract)
                        P.tt(MB[:, c, 128:192], tokz[:, c, 1, :], b[:, 320:384].k(ek(5, 6)), ALU.subtract)
                order = ([16, 17] + list(range(16))) if z == 0 else ([17, 16] + list(range(15, -1, -1)))
                P.memset(ST[0].v(), 0.0)
                for i, c in enumerate(order):
                    cur = ST[i % 2]
                    nxt = ST[(i + 1) % 2]
                    bY = bk[(2 * i) % 8]
                    bS = bk[(2 * i + 1) % 8]
                    P.mmg(bY[:64, 0:128].k(ek(0, 2)), [(cur.v(), MA[:, c, 0:128]), (vtok[:, c, :], MB[:, c, 0:128])])
                    P.mmg(bS[:64, 0:64].k(ek(0, 1)), [(MA[:, c, 128:192], cur.v()), (MB[:, c, 128:192], vtok[:, c, :])])
                    if i < NTILE - 1:
                        P.ts(nxt.v(), bS[:64, 0:64].k(ek(0, 1)), scl[:, c:c + 1], None, ALU.mult)
                    ysl = yacc[:, c * 128:(c + 1) * 128]
                    if z == 0:
                        P.copy(ysl, bY[:64, 0:128].k(ek(0, 2)), e="act")
                    else:
                        P.tt(ysl, ysl, bY[:64, 0:128].k(ek(0, 2)), ALU.add)
            if dbgd is not None:
                P.dma("sp", dbgd[h * 64:(h + 1) * 64, :], yacc.v())
            E2o = f_E.v().re("p c t -> p (c t)")
            yo = rT
            L2o = f_L.v().re("p c t -> p (c t)")
            for ci, (t0, tn) in enumerate(TOKCH):
                ts_ = slice(t0, t0 + tn)
                b1, b2, b3 = bk[(3 * ci) % 8], bk[(3 * ci + 1) % 8], bk[(3 * ci + 2) % 8]
                P.act(L2o[:, 0:tn], yacc[:, ts_], AF.Square)
                P.mm(b1[:64, :tn], ones_f.v(), yacc[:, ts_])
                P.mm(b2[:64, :tn], ones_f.v(), L2o[:, 0:tn])
                P.stt(L2o[:, 512:512 + tn], r_s[:, ts_], rk_c[:, hc], k_s[:, ts_], ALU.mult, ALU.mult)
                P.mm(b3[:64, :tn], ones_f.v(), L2o[:, 512:512 + tn])
                mean = E2o[:, 0:tn]
                var = E2o[:, 512:512 + tn]
                dd = E2o[:, 1024:1024 + tn]
                P.ts(mean, b1[:64, :tn], 1.0 / 64, None, ALU.mult)
                P.tt(var, mean, mean, ALU.mult)
                P.stt(var, b2[:64, :tn], 1.0 / 64, var, ALU.mult, ALU.subtract)
                P.ts(var, var, 64e-5, None, ALU.add)
                P.act(var, var, AF.Sqrt)
                P.recip(var, var)
                P.tt(dd, yacc[:, ts_], mean, ALU.subtract)
                P.tt(dd, dd, var, ALU.mult)
                P.ts(dd, dd, gnw_c[:, hc], gnb_c[:, hc], ALU.mult, ALU.add)
                P.tt(var, b3[:64, :tn], v_s[:, ts_], ALU.mult)
                P.tt(dd, dd, var, ALU.add)
                P.tt(yo[:, ts_], dd, g_s[:, ts_], ALU.mult)
            P.dma("sp", D.ys[1][h * 64:(h + 1) * 64, :], yo.v())

def stage_merge(P, D, L, need_ctx):
    chunks = TOKCH if need_ctx else TOKCH[:4]
    ntile = NTILE if need_ctx else 16
    with Stage(P) as st:
        mT = st.sb("mgT", [128, 16, NT], BF16)
        with Stage(P) as sa:
            Y = [sa.sb(f"mgY{i}", [128, 24, 512], BF16) for i in range(2)]
            wb = [sa.sb(f"mgw{i}", [128, 3, 8, 128], BF16) for i in range(2)]
            gt = [sa.sb(f"mgg{i}", [128, 3, 512], BF16) for i in range(2)]
            sg = [sa.sb(f"mgs{i}", [128, 3, 512], F32) for i in range(2)]
            t0_ = sa.sb("mgt0", [128, 512], F32)
            t1_ = sa.sb("mgt1", [128, 512], F32)
            ps = [sa.ps(f"mgp{i}", [128, 512], F32) for i in range(6)]
            u = 0
            for ci, (t0, tn) in enumerate(chunks):
                y = Y[ci % 2]
                for z in range(3):
                    P.dma("sp", y[:, z * 8:(z + 1) * 8, :tn], D.ys[z][:, t0:t0 + tn].re("(kc p) t -> p kc t", p=128))
                for ob in range(16):
                    w = wb[u % 2]
                    g = gt[u % 2]
                    s_ = sg[u % 2]
                    pp = ps[(u % 2) * 3:(u % 2) * 3 + 3]
                    u += 1
                    for z in range(3):
                        P.dma("pool", w[:, z, :, :], D.w_branch[L, z, :, ob * 128:(ob + 1) * 128].re("(kc p) n -> p kc n", p=128))
                        r0 = OFF_G + z * DM + ob * 128
                        P.dma("sp", g[:, z, :tn], D.uT[r0:r0 + 128, t0:t0 + tn])
                    P.act(s_[:, :, :tn], g[:, :, :tn], AF.Sigmoid)
                    for z in range(3):
                        P.mmg(pp[z][:, :tn], [(w[:, z, kc, :], y[:, z * 8 + kc, :tn]) for kc in range(8)])
                    P.tt(t0_[:, :tn], pp[0][:, :tn], s_[:, 0, :tn], ALU.mult)
                    P.tt(t1_[:, :tn], pp[1][:, :tn], s_[:, 1, :tn], ALU.mult)
                    P.tt(t0_[:, :tn], t0_[:, :tn], t1_[:, :tn], ALU.add, e="pool")
                    P.tt(t1_[:, :tn], pp[2][:, :tn], s_[:, 2, :tn], ALU.mult)
                    P.tt(mT[:, ob, t0:t0 + tn], t0_[:, :tn], t1_[:, :tn], ALU.add, e="pool")
        with Stage(P) as sb_:
            wo = [sb_.sb(f"mow{i}", [128, 16, 512], BF16) for i in range(2)]
            g1 = [[sb_.sb(f"mog{i}_{r}", [128, 512], F32) for r in range(2)] for i in range(2)]
            xt = [sb_.sb(f"mox{i}", [128, 512], F32) for i in range(3)]
            tm = [sb_.sb(f"mot{i}", [128, 512], F32) for i in range(2)]
            ps = [sb_.ps(f"mop{i}", [128, 512], F32) for i in range(4)]
            u = 0
            for s in range(4):
                cs_ = slice(s * 512, (s + 1) * 512)
                w = wo[s % 2]
                for q4 in range(4):
                    P.dma("pool", w[:, q4 * 4:(q4 + 1) * 4, :], D.w_out[L, q4 * 512:(q4 + 1) * 512, cs_].re("(kc p) n -> p kc n", p=128))
                for r in range(2):
                    P.dma("sp", g1[s % 2][r].v(), bc_row(D.modv[L][r:r + 1, 2 * DM + s * 512:2 * DM + (s + 1) * 512]))
                for t in range(ntile):
                    r = 0 if t < 16 else 1
                    x = xt[u % 3]
                    tmp = tm[u % 2]
                    p = ps[u % 4]
                    u += 1
                    P.dma("sp", x.v(), D.xcur[t * 128:(t + 1) * 128, cs_])
                    P.mmg(p.v(), [(mT[:, kc, t * 128:(t + 1) * 128], w[:, kc, :]) for kc in range(16)])
                    P.tt(tmp.v(), p.v(), g1[s % 2][r].v(), ALU.mult)
                    P.tt(x.v(), x.v(), tmp.v(), ALU.add, e="pool")
                    P.dma("sp", D.xcur[t * 128:(t + 1) * 128, cs_], x.v())


def stage_norm2_router(P, D, L, need_ctx):
    ntile = NTILE if need_ctx else 16
    with Stage(P) as st:
        h2T = st.sb("h2T", [128, 16, NT], BF16)
        stage_norm(P, D, L, 1, h2T, st)
        P.dma("sp", D.h2Td.v().re("(c p) t -> p c t", p=128), h2T.v())
        rw = st.sb("rtw", [128, 16, 16], BF16)
        P.dma("pool", rw.v(), D.router_w.v().re("(kc p) e -> p kc e", p=128))
        rb = st.sb("rtb", [128, 16], F32)
        P.dma("sp", rb.v(), bc_row(D.router_bias.v().re("(o e) -> o e", o=1)))
        identf = st.sb("rtid", [128, 128], F32)
        P.dma("sp", identf.v(), D.ident_f.v())
        wT = st.sb("rtwT", [16, NT], F32)
        ps = [st.ps(f"rtp{i}", [128, 16], F32) for i in range(2)]
        pst = [st.ps(f"rtq{i}", [16, 128], F32) for i in range(2)]

        def tl(n, shape):
            return [st.sb(f"{n}{i}", shape, F32) for i in range(2)]
        sc, sel, eq, s2, m1, m2, grp, gm, geq, t1, oh1, t2, oh2, den = (tl("rsc", [128, 16]), tl("rsel", [128, 16]), tl("req", [128, 16]), tl("rs2", [128, 16]),
                                                                     tl("rm1", [128, 4]), tl("rm2", [128, 4]), tl("rgrp", [128, 4]), tl("rgm", [128, 1]), tl("rgeq", [128, 4]),
                                                                     tl("rt1", [128, 1]), tl("roh1", [128, 16]), tl("rt2", [128, 1]), tl("roh2", [128, 16]), tl("rden", [128, 1]))
        BIG = 1.0e9

        def g4(v):
            return v.re("p (g e) -> p g e", e=4)

        def b4(v):
            return v.m(lambda a: a.unsqueeze(2).broadcast_to([128, 4, 4]))

        def b16(v):
            return v.m(lambda a: a.broadcast_to([128, 16]))
        for t in range(ntile):
            i = t % 2
            P.mmg(ps[i].v(), [(h2T[:, kc, t * 128:(t + 1) * 128], rw[:, kc, :]) for kc in range(16)])
            P.act(sc[i].v(), ps[i].v(), AF.Sigmoid)
            P.tt(sel[i].v(), sc[i].v(), rb.v(), ALU.add)
            P.reduce(m1[i].v(), g4(sel[i].v()), ALU.max)
            P.tt(g4(eq[i].v()), g4(sel[i].v()), b4(m1[i].v()), ALU.is_equal)
            P.stt(s2[i].v(), eq[i].v(), -BIG, sel[i].v(), ALU.mult, ALU.add)
            P.reduce(m2[i].v(), g4(s2[i].v()), ALU.max)
            P.tt(grp[i].v(), m1[i].v(), m2[i].v(), ALU.add)
            P.reduce(gm[i].v(), grp[i].v(), ALU.max)
            P.ts(geq[i].v(), grp[i].v(), gm[i].v(), None, ALU.is_equal)
            P.ts(geq[i].v(), geq[i].v(), BIG, -BIG, ALU.mult, ALU.add)
            P.tt(g4(s2[i].v()), g4(sel[i].v()), b4(geq[i].v()), ALU.add)
            P.reduce(t1[i].v(), s2[i].v(), ALU.max)
            P.ts(oh1[i].v(), s2[i].v(), t1[i].v(), None, ALU.is_equal)
            P.stt(eq[i].v(), oh1[i].v(), -BIG, s2[i].v(), ALU.mult, ALU.add)
            P.reduce(t2[i].v(), eq[i].v(), ALU.max)
            P.ts(oh2[i].v(), eq[i].v(), t2[i].v(), None, ALU.is_equal)
            P.tt(oh1[i].v(), oh1[i].v(), oh2[i].v(), ALU.add)
            P.tt(oh1[i].v(), oh1[i].v(), sc[i].v(), ALU.mult)
            P.reduce(den[i].v(), oh1[i].v(), ALU.add)
            P.recip(den[i].v(), den[i].v())
            P.ts(oh1[i].v(), oh1[i].v(), den[i].v(), None, ALU.mult)
            P.transpose(pst[i].v(), oh1[i].v(), identf.v())
            P.copy(wT[:, t * 128:(t + 1) * 128], pst[i].v(), e="act")
        P.dma("sp", D.wgtT[:, 0:ntile * 128], wT[:, 0:ntile * 128])


def stage_moe(P, D, L, need_ctx):
    ntok = NT if need_ctx else T
    passes = []
    p0 = 0
    while p0 < ntok:
        pn = min(768, ntok - p0)
        passes.append((p0, pn))
        p0 += pn
    with Stage(P) as st:
        identf = st.sb("moid", [128, 128], F32)
        P.dma("sp", identf.v(), D.ident_f.v())
        selE = st.sb("mosel", [16, 16, 128], F32)
        P.dma("sp", selE.v(), D.selE.v())
        wT = st.sb("mowT", [16, NT], F32)
        P.dma("sp", wT[:, 0:ntok], D.wgtT[:, 0:ntok])
        g2c = [st.sb(f"mog2{r}", [128, 16], F32) for r in range(2)]
        for r in range(2):
            P.dma("sp", g2c[r].v(), D.modv[L][r, 5 * DM:6 * DM].re("(o p) -> p o", p=128), allow_slow_non_contiguous=True)
        h2 = st.sb("moh2", [128, 16, 768], BF16)
        facc = st.sb("mofacc", [128, 16, 768], F32)
        act_ = st.sb("moact", [128, 8, 768], BF16)
        wbc = st.sb("mowbc", [128, 768], F32)
        wgu = [st.sb(f"mowgu{i}", [128, 16, 2, 256], BF16) for i in range(2)]
        wd = [st.sb(f"mowd{i}", [128, 8, 512], BF16) for i in range(2)]
        sgt = [st.sb(f"mosg{i}", [128, 384], F32) for i in range(2)]
        tt_ = [st.sb(f"mott{i}", [128, 384], F32) for i in range(2)]
        xt = [st.sb(f"moxt{i}", [128, DM], F32) for i in range(2)]
        ps = [st.ps(f"mop{i}", [128, 512], F32) for i in range(8)]
        ug = 0
        ud = 0
        pi = 0
        for (p0, pn) in passes:
            chunks = [(c0, min(384, pn - c0)) for c0 in range(0, pn, 384)]
            P.dma("sp", h2[:, :, :pn], D.h2Td[:, p0:p0 + pn].re("(c p) t -> p c t", p=128))
            for e in range(16):
                for (c0, cn) in chunks:
                    p = ps[pi % 8]
                    pi += 1
                    P.mm(p[:, :cn], selE[:, e, :], wT[:, p0 + c0:p0 + c0 + cn])
                    P.copy(wbc[:, c0:c0 + cn], p[:, :cn], e="act")
                for s2_ in range(4):
                    w = wgu[ug % 2]
                    ug += 1
                    for gu in range(2):
                        for q2 in range(2):
                            P.dma("pool", w[:, q2 * 8:(q2 + 1) * 8, gu, :],
                                  D.moe_w_gate_up[L, e, q2 * 1024:(q2 + 1) * 1024, gu * 1024 + s2_ * 256:gu * 1024 + (s2_ + 1) * 256].re("(kc p) n -> p kc n", p=128))
                    for b2 in range(2):
                        blk = s2_ * 2 + b2
                        for ci, (c0, cn) in enumerate(chunks):
                            pg = ps[pi % 8]
                            pu = ps[(pi + 1) % 8]
                            pi += 2
                            P.mmg(pg[:, :cn], [(w[:, kc, 0, b2 * 128:(b2 + 1) * 128], h2[:, kc, c0:c0 + cn]) for kc in range(16)])
                            P.mmg(pu[:, :cn], [(w[:, kc, 1, b2 * 128:(b2 + 1) * 128], h2[:, kc, c0:c0 + cn]) for kc in range(16)])
                            sg_ = sgt[ci % 2]
                            t_ = tt_[ci % 2]
                            P.act(sg_[:, :cn], pg[:, :cn], AF.Silu)
                            P.tt(t_[:, :cn], pu[:, :cn], sg_[:, :cn], ALU.mult)
                            P.tt(act_[:, blk, c0:c0 + cn], t_[:, :cn], wbc[:, c0:c0 + cn], ALU.mult, e="pool")
                for s4 in range(4):
                    w = wd[ud % 2]
                    ud += 1
                    for q2 in range(2):
                        P.dma("pool", w[:, q2 * 4:(q2 + 1) * 4, :],
                              D.moe_w_down[L, e, q2 * 512:(q2 + 1) * 512, s4 * 512:(s4 + 1) * 512].re("(kc p) n -> p kc n", p=128))
                    for o4 in range(4):
                        ob = s4 * 4 + o4
                        for (c0, cn) in chunks:
                            p = ps[pi % 8]
                            pi += 1
                            P.mmg(p[:, :cn], [(w[:, kc, o4 * 128:(o4 + 1) * 128], act_[:, kc, c0:c0 + cn]) for kc in range(8)])
                            if e == 0:
                                P.copy(facc[:, ob, c0:c0 + cn], p[:, :cn], e="act")
                            else:
                                P.tt(facc[:, ob, c0:c0 + cn], facc[:, ob, c0:c0 + cn], p[:, :cn], ALU.add)
            for ob in range(16):
                lat = max(0, min(pn, T - p0))
                if lat > 0:
                    P.ts(facc[:, ob, 0:lat], facc[:, ob, 0:lat], g2c[0][:, ob:ob + 1], None, ALU.mult)
                if lat < pn:
                    P.ts(facc[:, ob, lat:pn], facc[:, ob, lat:pn], g2c[1][:, ob:ob + 1], None, ALU.mult)
            for ti in range(pn // 128):
                t = p0 // 128 + ti
                x = xt[ti % 2]
                P.dma("sp", x.v(), D.xcur[t * 128:(t + 1) * 128, :])
                for o4 in range(4):
                    p = ps[pi % 8]
                    pi += 1
                    for j in range(4):
                        ob = o4 * 4 + j
                        P.transpose(p[:, j * 128:(j + 1) * 128], facc[:, ob, ti * 128:(ti + 1) * 128], identf.v())
                    P.tt(x[:, o4 * 512:(o4 + 1) * 512], x[:, o4 * 512:(o4 + 1) * 512], p.v(), ALU.add)
                P.dma("sp", D.xcur[t * 128:(t + 1) * 128, :], x.v())


def stage_final(P, D):
    with Stage(P) as st:
        g = st.sb("fng", [128, DM], F32)
        P.dma("sp", g.v(), bc_row(D.final_norm_g.v().re("(o d) -> o d", o=1)))
        xt = [st.sb(f"fnx{i}", [128, DM], F32) for i in range(2)]
        sq = st.sb("fnsq", [128, DM], F32)
        ot = [st.sb(f"fno{i}", [128, DM], F32) for i in range(2)]
        ss = [st.sb(f"fns{i}", [128, 1], F32) for i in range(2)]
        for t in range(16):
            x = xt[t % 2]
            s = ss[t % 2]
            o = ot[t % 2]
            P.dma("sp", x.v(), D.xcur[t * 128:(t + 1) * 128, :])
            P.act(sq.v(), x.v(), AF.Square, accum=s.v())
            P.ts(s.v(), s.v(), 1.0 / DM, 1e-6, ALU.mult, ALU.add)
            P.act(s.v(), s.v(), AF.Sqrt)
            P.recip(s.v(), s.v())
            P.stt(o.v(), x.v(), s.v(), g.v(), ALU.mult, ALU.mult)
            P.dma("sp", D.out[t * 128:(t + 1) * 128, :], o.v())

ORDER = ["mod", "norm1", "win", "na", "mlaprep", "mla", "rw", "merge", "norm2", "moe"]


def build(L_list=(0, 1), upto=None, dbg=(), skip=(), rw_heads=range(16)):
    nc = bass.Bass("TRN2", target_bir_lowering=False)
    P = Prog(nc)
    D = NS()

    def inp(name, shape, dt=F32):
        b = dram(nc, name, shape, dt, kind="ExternalInput")
        setattr(D, name, b)
        return b

    inp("xin", [NT, DM]); inp("cT", [128, 16, 2])
    inp("w_mod", [2, DM, 6 * DM]); inp("b_mod", [2, 6 * DM])
    inp("norm1_g", [2, DM]); inp("norm2_g", [2, DM])
    inp("w_in", [2, DM, IN_W])
    inp("ident_f", [128, 128])
    inp("na_bias", [2, 16, 128, 5, 5, 128])
    inp("rope_c", [32, NT]); inp("rope_s", [32, NT]); inp("perm96", [96, 96])
    inp("mla_q_norm_g", [2, 512]); inp("mla_kv_norm_g", [2, 256])
    inp("mla_w_uq", [2, 512, 1536]); inp("mla_w_ukv", [2, 256, 2048])
    inp("tri_masks", [4, 128, 128])
    inp("rw_shift_mu", [2, 3488]); inp("rw_w0", [2, 2, 1024]); inp("rw_w_up", [2, 2, 64, 1024])
    inp("rw_a0", [2, 2, 1024]); inp("rw_a_up", [2, 2, 64, 1024]); inp("rw_g_up", [2, 160, 1024])
    inp("rw_k_k", [2, 1024]); inp("rw_k_a", [2, 1024]); inp("rw_r_k", [2, 16, 64])
    inp("rw_gn_w", [2, 1024]); inp("rw_gn_b", [2, 1024])
    inp("w_branch", [2, 3, 1024, DM]); inp("w_out", [2, DM, DM])
    inp("router_w", [DM, 16]); inp("router_bias", [16])
    inp("moe_w_gate_up", [2, 16, DM, 2048]); inp("moe_w_down", [2, 16, 1024, DM])
    inp("final_norm_g", [DM]); inp("selE", [16, 16, 128])
    D.out = dram(nc, "out", [T, DM], F32, kind="ExternalOutput")

    def scratch(name, shape, dt):
        kind = "ExternalOutput" if name in dbg else "Internal"
        b = dram(nc, name, shape, dt, kind=kind)
        setattr(D, name, b)
        return b

    scratch("xcur", [NT, DM], F32)
    D.modv = [scratch(f"modv{l}", [2, 6 * DM], F32) for l in range(2)]
    scratch("uT", [IN_WP, NT], BF16)
    scratch("utok", [NT, 1024], BF16)
    scratch("hTd", [DM, NT], BF16)
    D.ys = [scratch(f"ys{z}", [1024, NT], BF16) for z in range(3)]
    scratch("qmT", [16, 96, NT], BF16)
    scratch("kmT", [16, 96, NT], BF16)
    scratch("vmtok", [NT, 1024], BF16)
    scratch("yscan", [1024, NT], F32)
    scratch("h2Td", [DM, NT], BF16)
    scratch("wgtT", [16, NT], F32)

    def stop(name):
        return upto is not None and ORDER.index(name) >= ORDER.index(upto)

    with Stage(P) as st0:
        ident_bf = st0.sb("ident_bf", [128, 128], BF16)
        D.ident_bf = ident_bf
        P.dma("pool", ident_bf.v(), D.ident_f.v())
        for i in range(6):
            P.dma("sp", D.xcur[i * 384:(i + 1) * 384, :], D.xin[i * 384:(i + 1) * 384, :])
        for L in L_list:
            need_ctx = (L == 0)
            if "mod" not in skip:
                stage_mod(P, D, L)
            if stop("mod"):
                break
            if "win" not in skip:
                with Stage(P) as stl:
                    hT = stl.sb("hT", [128, 16, NT], BF16)
                    stage_norm(P, D, L, 0, hT, stl)
                    if "hTd" in dbg:
                        P.dma("sp", D.hTd.v().re("(c p) t -> p c t", p=128), hT.v())
                    stage_win(P, D, L, hT)
            if stop("win"):
                break
            if "na" not in skip:
                stage_na(P, D, L, need_ctx)
            if stop("na"):
                break
            if "mla" not in skip:
                stage_mla_prep(P, D, L, need_ctx)
                stage_mla_attn(P, D, L, need_ctx)
            if stop("mla"):
                break
            if "rw" not in skip:
                stage_rw(P, D, L, need_ctx, heads=rw_heads, dbgd=(D.yscan if "yscan" in dbg else None))
            if stop("rw"):
                break
            if "merge" not in skip:
                stage_merge(P, D, L, need_ctx)
            if stop("merge"):
                break
            if "moe" not in skip:
                stage_norm2_router(P, D, L, need_ctx)
                stage_moe(P, D, L, need_ctx)
            if stop("moe"):
                break
        else:
            stage_final(P, D)
        P.drain()
    print("ninst", P.ninst, {e: P.etot[e] for e in P.eng})
    return nc


def host_consts():
    f32 = np.float32
    c = {}
    c["ident_f"] = np.eye(128, dtype=f32)
    nf = 8
    inv = (10000.0 ** (-np.arange(nf, dtype=np.float32) / nf)).astype(np.float32)
    pos = np.arange(T)
    row = (pos // 64).astype(np.float32)
    col = (pos % 64).astype(np.float32)
    rc = np.ones((32, NT), f32)
    rs = np.zeros((32, NT), f32)
    for d in range(32):
        p = row if d < 16 else col
        dd = d % 16
        f = dd % 8
        ang = (p * inv[f]).astype(np.float32)
        rc[d, :T] = np.cos(ang)
        rs[d, :T] = (-np.sin(ang)) if dd < 8 else np.sin(ang)
    c["rope_c"] = rc
    c["rope_s"] = rs
    pm = np.zeros((96, 96), f32)
    for d in range(32):
        dd = d % 16
        partner = (d + 8) if dd < 8 else (d - 8)
        pm[64 + partner, 64 + d] = 1.0
    c["perm96"] = pm
    i = np.arange(128)
    mu_ = (i[:, None] < i[None, :]).astype(f32)
    ml_ = (i[:, None] > i[None, :]).astype(f32)
    se = np.zeros((16, 16, 128), f32)
    for e in range(16):
        se[e, e, :] = 1.0
    c["selE"] = se
    c["tri_masks"] = np.stack([mu_, ml_, mu_ + np.eye(128, dtype=f32), ml_ + np.eye(128, dtype=f32)], 0)
    return c


def na_bias_table(rpb):
    Lr = rpb.shape[0]
    out = np.full((Lr, 16, 128, 5, 5, 128), -1e30, np.float32)
    jrep = [0, 1, 2, 14, 15]
    for pi, j in enumerate(jrep):
        kr0 = na_kr0(j)
        qtok = np.arange(128)
        qi = 2 * j + qtok // 64
        qj = qtok % 64
        r0 = np.clip(qi - 4, 0, 24)
        cstart = np.clip(qj - 8, 0, 48)
        for c in range(5):
            ktok = np.arange(128)
            ar = kr0 + 2 * c + ktok // 64
            kc = ktok % 64
            rv = (ar[:, None] >= r0[None, :]) & (ar[:, None] < r0[None, :] + 8)
            cv = (kc[:, None] >= cstart[None, :]) & (kc[:, None] < cstart[None, :] + 16)
            ridx = np.clip(ar[:, None] - qi[None, :] + 7, 0, 14)
            cidx = np.clip(kc[:, None] - qj[None, :] + 15, 0, 30)
            val = rpb[:, :, ridx, cidx]
            out[:, :, :, pi, c, :] = np.where((rv & cv)[None, None], val, np.float32(-1e30))
    return out


def make_inputs(inputs, b, consts, nab):
    f32 = np.float32
    x = np.asarray(inputs["x"][b], f32)
    ctx = np.asarray(inputs["ctx"][b], f32)
    d = dict(consts)
    d["xin"] = np.ascontiguousarray(np.concatenate([x, ctx], axis=0))
    cc = np.stack([np.asarray(inputs["c"][b], f32), np.asarray(inputs["c_ctx"], f32)], axis=0)
    d["cT"] = np.ascontiguousarray(cc.reshape(2, 16, 128).transpose(2, 1, 0))
    for k in ["w_mod", "b_mod", "norm1_g", "norm2_g", "w_in", "mla_q_norm_g", "mla_kv_norm_g", "mla_w_uq", "mla_w_ukv",
              "rw_shift_mu", "rw_w0", "rw_w_up", "rw_a0", "rw_a_up", "rw_g_up", "rw_k_k", "rw_k_a", "rw_r_k", "rw_gn_w", "rw_gn_b",
              "w_branch", "w_out", "router_w", "router_bias", "moe_w_gate_up", "moe_w_down", "final_norm_g"]:
        d[k] = np.asarray(inputs[k], f32)
    d["na_bias"] = nab
    return d


def kernel(**inputs):
    nc = build()
    consts = host_consts()
    nab = na_bias_table(np.asarray(inputs["na_rpb"], np.float32))
    in_maps = [make_inputs(inputs, b, consts, nab) for b in range(8)]
    res = run_bass_kernel_spmd(nc, in_maps, core_ids=list(range(8)))
    return np.stack([np.asarray(r["out"], np.float32) for r in res.results], axis=0)
```

```python
import numpy as np
import concourse.bass as bass
import concourse.mybir as mybir
from contextlib import ExitStack

F32 = mybir.dt.float32
BF16 = mybir.dt.bfloat16
AF = mybir.ActivationFunctionType
ALU = mybir.AluOpType
AX = mybir.AxisListType

SEM_ROT = 30000
N_DMA_SEMS = 40


class V:
    __slots__ = ("ap", "buf", "key")

    def __init__(self, ap, buf, key=None):
        self.ap = ap
        self.buf = buf
        self.key = key

    def __getitem__(self, idx):
        return V(self.ap[idx], self.buf, self.key)

    def m(self, fn):
        return V(fn(self.ap), self.buf, self.key)

    def re(self, s, **kw):
        return V(self.ap.rearrange(s, **kw), self.buf, self.key)

    def bc(self, dt):
        return V(self.ap.bitcast(dt), self.buf, self.key)

    def k(self, key):
        return V(self.ap, self.buf, key)


class Buf:
    def __init__(self, name, t):
        self.name = name
        self.t = t
        self.state = {None: [None, []]}
        self.psum = False
        self.bank_ev = None

    def ap(self):
        t = self.t
        return t.ap() if hasattr(t, "ap") and callable(getattr(t, "ap")) and not isinstance(t, bass.AP) else t

    def __getitem__(self, idx):
        return V(self.ap()[idx], self, None)

    def v(self):
        return V(self.ap(), self, None)

    def part(self, key):
        if key not in self.state:
            w, r = self.state[None]
            self.state[key] = [w, list(r)]
        return V(self.ap(), self, key)

    def keys_for(self, key):
        if key is None:
            return list(self.state.keys())
        if isinstance(key, tuple):
            out = []
            for k in key:
                out += self.keys_for(k)
            return out
        if key not in self.state:
            w, r = self.state[None]
            self.state[key] = [w, list(r)]
        return [key]


class Prog:
    def __init__(self, nc):
        self.nc = nc
        self.eng = {"pe": nc.tensor, "act": nc.scalar, "dve": nc.vector, "pool": nc.gpsimd, "sp": nc.sync}
        self.esem = {}
        self.ecnt = {}
        self.etot = {}
        for e in self.eng:
            self.esem[e] = nc.alloc_semaphore(f"es_{e}_0")
            self.ecnt[e] = 0
            self.etot[e] = 0
        self.waited = {e: {} for e in self.eng}
        self.dsems = [nc.alloc_semaphore(f"dma_{i}") for i in range(N_DMA_SEMS)]
        self.dcnt = [0] * N_DMA_SEMS
        self.dnext = 0
        self.bsems = [nc.alloc_semaphore(f"bgdma_{i}") for i in range(8)]
        self.bcnt = [0] * 8
        self.bnext = 0
        self.all_sems = {}
        self.ninst = 0
        self.pe_sems = {id(self.esem["pe"])}
        self.npe = 0
        self.marks = []

    def _wait(self, e, ev):
        if ev is None:
            return
        sem, val = ev
        sid = id(sem)
        self.all_sems[sid] = sem
        if self.waited[e].get(sid, 0) >= val:
            return
        self.eng[e].wait_ge(sem, val)
        self.waited[e][sid] = val

    def _wait2(self, e, ev):
        if ev is not None and e == "pe" and id(ev[0]) in self.pe_sems:
            return
        self._wait(e, ev)

    def _bank(self, e, vs):
        for v in vs:
            if v.buf.psum and v.buf.bank_ev is not None and v.buf.bank_ev[1] != e:
                self._wait(e, v.buf.bank_ev[0])

    def _deps(self, e, reads, writes):
        self._bank(e, list(reads) + list(writes))
        for v in reads:
            for k in v.buf.keys_for(v.key):
                self._wait2(e, v.buf.state[k][0])
        for v in writes:
            for k in v.buf.keys_for(v.key):
                st = v.buf.state[k]
                self._wait2(e, st[0])
                for ev in st[1]:
                    self._wait2(e, ev)

    def _record(self, ev, reads, writes, e=None):
        for v in list(reads) + list(writes):
            if v.buf.psum:
                v.buf.bank_ev = (ev, e)
        for v in reads:
            for k in v.buf.keys_for(v.key):
                v.buf.state[k][1].append(ev)
        for v in writes:
            for k in v.buf.keys_for(v.key):
                v.buf.state[k][0] = ev
                v.buf.state[k][1] = []

    def _newev(self, e, inst):
        if self.ecnt[e] >= SEM_ROT:
            self.esem[e] = self.nc.alloc_semaphore(f"es_{e}_{self.etot[e]}")
            self.ecnt[e] = 0
            if e == "pe":
                self.pe_sems.add(id(self.esem[e]))
        self.ecnt[e] += 1
        self.etot[e] += 1
        inst.then_inc(self.esem[e], 1)
        ev = (self.esem[e], self.ecnt[e])
        self.waited[e][id(self.esem[e])] = 0 if id(self.esem[e]) not in self.waited[e] else self.waited[e][id(self.esem[e])]
        self.all_sems[id(self.esem[e])] = self.esem[e]
        self.last_ev = getattr(self, "last_ev", {})
        self.last_ev[e] = ev
        return ev

    def mark(self, name):
        self.marks.append((name, self.npe))

    def op(self, e, fn, reads, writes, nosync_same=False):
        if e == "pe":
            self.npe += 1
        self._deps(e, reads, writes)
        inst = fn()
        ev = self._newev(e, inst)
        self._record(ev, reads, writes, e)
        self.ninst += 1
        return inst

    def group(self, e, fns, reads, writes):
        self._deps(e, reads, writes)
        if e == "pe":
            self.npe += len(fns)
        inst = None
        for fn in fns:
            inst = fn()
            self.ninst += 1
        ev = self._newev(e, inst)
        self._record(ev, reads, writes, e)
        return inst

    def dma(self, q, out, in_, bg=False, **kw):
        if bg:
            i = self.bnext
            self.bnext = (self.bnext + 1) % len(self.bsems)
            sem = self.bsems[i]
            cnt = self.bcnt
        else:
            i = self.dnext
            self.dnext = (self.dnext + 1) % N_DMA_SEMS
            sem = self.dsems[i]
            cnt = self.dcnt
        if cnt[i] > 0:
            self._wait(q, (sem, cnt[i]))
        self._deps(q, [in_], [out])
        inst = self.eng[q].dma_start(out=out.ap, in_=in_.ap, **kw)
        inst.then_inc(sem, 16)
        cnt[i] += 16
        ev = (sem, cnt[i])
        self.all_sems[id(sem)] = sem
        self._record(ev, [in_], [out])
        self.ninst += 1
        return ev

    def drain(self):
        evs = []
        for e in self.eng:
            if self.ecnt[e] > 0:
                evs.append((self.esem[e], self.ecnt[e]))
        for i in range(N_DMA_SEMS):
            if self.dcnt[i] > 0:
                evs.append((self.dsems[i], self.dcnt[i]))
        for i in range(len(self.bsems)):
            if self.bcnt[i] > 0:
                evs.append((self.bsems[i], self.bcnt[i]))
        for e in self.eng:
            for ev in evs:
                self._wait(e, ev)

    def mm(self, out, lhsT, rhs, start=True, stop=True):
        if lhsT.ap.dtype == F32:
            self.npe += 1
        return self.op("pe", lambda: self.nc.tensor.matmul(out.ap, lhsT.ap, rhs.ap, start=start, stop=stop),
                       [lhsT, rhs] + ([] if start else [out]), [out])

    def mmg(self, out, pairs):
        n = len(pairs)
        if pairs[0][0].ap.dtype == F32:
            self.npe += n
        fns = []
        reads = []
        for j, (l, r) in enumerate(pairs):
            fns.append((lambda l=l, r=r, j=j: self.nc.tensor.matmul(out.ap, l.ap, r.ap, start=(j == 0), stop=(j == n - 1))))
            reads += [l, r]
        return self.group("pe", fns, reads, [out])

    def transpose(self, out, in_, ident):
        return self.op("pe", lambda: self.nc.tensor.transpose(out.ap, in_.ap, ident.ap), [in_, ident], [out])

    def act(self, out, in_, func, bias=None, scale=None, accum=None, e="act"):
        kw = {}
        reads = [in_]
        if bias is not None:
            if isinstance(bias, V):
                kw["bias"] = bias.ap
                reads.append(bias)
            else:
                kw["bias"] = bias
        if scale is not None:
            if isinstance(scale, V):
                kw["scale"] = scale.ap
                reads.append(scale)
            else:
                kw["scale"] = scale
        writes = [out]
        if accum is not None:
            kw["accum_out"] = accum.ap
            writes.append(accum)
        return self.op("act", lambda: self.nc.scalar.activation(out.ap, in_.ap, func, **kw), reads, writes)

    def tt(self, out, a, b, op, e="dve"):
        return self.op(e, lambda: self.eng[e].tensor_tensor(out.ap, a.ap, b.ap, op), [a, b], [out])

    def ts(self, out, a, s1, s2, op0, op1=None, accum=None, e="dve"):
        reads = [a]
        s1a = s1
        s2a = s2
        if isinstance(s1, V):
            reads.append(s1)
            s1a = s1.ap
        if isinstance(s2, V):
            reads.append(s2)
            s2a = s2.ap
        writes = [out]
        kw = {}
        if op1 is not None:
            kw["op1"] = op1
        if accum is not None:
            kw["accum_out"] = accum.ap
            writes.append(accum)
        return self.op(e, lambda: self.eng[e].tensor_scalar(out.ap, a.ap, s1a, s2a, op0, **kw), reads, writes)

    def stt(self, out, a, s, b, op0, op1, accum=None):
        reads = [a, b]
        sa = s
        if isinstance(s, V):
            reads.append(s)
            sa = s.ap
        writes = [out]
        kw = {}
        if accum is not None:
            kw["accum_out"] = accum.ap
            writes.append(accum)
        return self.op("dve", lambda: self.nc.vector.scalar_tensor_tensor(out.ap, a.ap, sa, b.ap, op0, op1, **kw), reads, writes)

    def copy(self, out, in_, e="dve"):
        if e == "act":
            return self.op("act", lambda: self.nc.scalar.copy(out.ap, in_.ap), [in_], [out])
        return self.op(e, lambda: self.eng[e].tensor_copy(out.ap, in_.ap), [in_], [out])

    def memset(self, out, val, e="dve"):
        return self.op(e, lambda: self.eng[e].memset(out.ap, val), [], [out])

    def reduce(self, out, in_, op, axis=None, e="dve"):
        axis = axis or AX.X
        return self.op(e, lambda: self.eng[e].tensor_reduce(out.ap, in_.ap, axis, op), [in_], [out])

    def recip(self, out, in_):
        return self.op("dve", lambda: self.nc.vector.reciprocal(out.ap, in_.ap), [in_], [out])


class Stage:
    uid = 0

    def __init__(self, P):
        self.P = P
        self.es = ExitStack()

    def __enter__(self):
        self.es.__enter__()
        return self

    def sb(self, name, shape, dtype=F32):
        Stage.uid += 1
        name = f"{name}_s{Stage.uid}"
        t = self.es.enter_context(self.P.nc.sbuf_tensor(name, list(shape), dtype))
        return Buf(name, t)

    def ps(self, name, shape, dtype=F32):
        Stage.uid += 1
        name = f"{name}_p{Stage.uid}"
        t = self.es.enter_context(self.P.nc.psum_tensor(name, list(shape), dtype))
        b = Buf(name, t)
        b.psum = True
        return b

    def __exit__(self, *a):
        self.P.drain()
        return self.es.__exit__(*a)

from concourse.bass_utils import run_bass_kernel_spmd
import ml_dtypes
import math

DM = 2048
T = 2048
TC = 256
NT = T + TC
NTILE = NT // 128
IN_W = 13504
IN_WP = 13568
OFF_NA = 0
OFF_RW = 3072
OFF_CQ = 6560
OFF_CKV = 7072
OFF_KR = 7328
OFF_G = 7360
TOKCH = [(0, 512), (512, 512), (1024, 512), (1536, 512), (2048, 256)]


class NS:
    pass


def dram(nc, name, shape, dtype, kind="Internal"):
    return Buf(name, nc.dram_tensor(name, list(shape), dtype, kind=kind))


def stage_mod(P, D, L):
    with Stage(P) as st:
        cT = st.sb("cT", [128, 16, 2], F32)
        cs = st.sb("cs", [128, 16, 2], F32)
        P.dma("sp", cT.v(), D.cT.v())
        P.act(cs.v(), cT.v(), AF.Silu)
        wb = [st.sb(f"wm{i}", [128, 16, 512], F32) for i in range(2)]
        bt = [st.sb(f"bm{i}", [2, 512], F32) for i in range(2)]
        ot = [st.sb(f"om{i}", [2, 512], F32) for i in range(2)]
        ps = [st.ps(f"pm{i}", [2, 512], F32) for i in range(2)]
        for j in range(24):
            w = wb[j % 2]
            for q4 in range(4):
                P.dma("sp", w[:, q4 * 4:(q4 + 1) * 4, :],
                      D.w_mod[L, q4 * 512:(q4 + 1) * 512, j * 512:(j + 1) * 512].re("(kc p) n -> p kc n", p=128))
            P.dma("sp", bt[j % 2].v(), D.b_mod[L:L + 1, j * 512:(j + 1) * 512].m(lambda a: a.broadcast_to([2, 512])))
            P.mmg(ps[j % 2].v(), [(cs[:, kc, :], w[:, kc, :]) for kc in range(16)])
            P.tt(ot[j % 2].v(), ps[j % 2].v(), bt[j % 2].v(), ALU.add)
            P.dma("sp", D.modv[L][:, j * 512:(j + 1) * 512], ot[j % 2].v())


def bc_row(v, n=128):
    return v.m(lambda a: a.broadcast_to([n, a.shape[-1]]))


def stage_norm(P, D, L, which, hT, st_outer):
    gsrc = D.norm1_g if which == 0 else D.norm2_g
    sh_i, sc_i = (0, 1) if which == 0 else (3, 4)
    with Stage(P) as st:
        g = st.sb("ng", [128, DM], F32)
        P.dma("sp", g.v(), bc_row(gsrc[L:L + 1, :]))
        A = []
        Bs = []
        for r in range(2):
            a = st.sb(f"nA{r}", [128, DM], F32)
            b = st.sb(f"nB{r}", [128, DM], F32)
            P.dma("sp", a.v(), bc_row(D.modv[L][r:r + 1, sc_i * DM:(sc_i + 1) * DM]))
            P.dma("sp", b.v(), bc_row(D.modv[L][r:r + 1, sh_i * DM:(sh_i + 1) * DM]))
            P.stt(a.v(), a.v(), 1.0, g.v(), ALU.add, ALU.mult)
            A.append(a)
            Bs.append(b)
        xt = [st.sb(f"nx{i}", [128, DM], F32) for i in range(2)]
        sq = st.sb("nsq", [128, DM], F32)
        hb = [st.sb(f"nh{i}", [128, DM], BF16) for i in range(2)]
        ss = [st.sb(f"nss{i}", [128, 1], F32) for i in range(2)]
        pt = [st.ps(f"npt{i}", [128, 16, 128], BF16) for i in range(2)]
        for t in range(NTILE):
            r = 0 if t < 16 else 1
            x = xt[t % 2]
            s = ss[t % 2]
            h = hb[t % 2]
            p = pt[t % 2]
            P.dma("sp", x.v(), D.xcur[t * 128:(t + 1) * 128, :])
            P.act(sq.v(), x.v(), AF.Square, accum=s.v())
            P.ts(s.v(), s.v(), 1.0 / DM, 1e-6, ALU.mult, ALU.add)
            P.act(s.v(), s.v(), AF.Sqrt)
            P.recip(s.v(), s.v())
            P.stt(sq.v(), x.v(), s.v(), A[r].v(), ALU.mult, ALU.mult)
            P.tt(h.v(), sq.v(), Bs[r].v(), ALU.add)
            for c in range(16):
                P.transpose(p[:, c, :], h[:, c * 128:(c + 1) * 128], D.ident_bf.v())
            P.copy(hT[:, :, t * 128:(t + 1) * 128], p.v(), e=("act" if t % 2 else "dve"))


def stage_win(P, D, L, hT):
    with Stage(P) as st:
        wb = [st.sb(f"ww{i}", [128, 16, 512], BF16) for i in range(2)]
        ob = [st.sb(f"wo{i}", [128, NT], BF16) for i in range(2)]
        ps = [st.ps(f"wp{i}", [128, 512], F32) for i in range(4)]
        nslab = (IN_W + 511) // 512
        pi = 0
        oi = 0
        for s in range(nslab):
            c0 = s * 512
            cw = min(512, IN_W - c0)
            w = wb[s % 2]
            for q4 in range(4):
                P.dma("pool", w[:, q4 * 4:(q4 + 1) * 4, :cw],
                      D.w_in[L, q4 * 512:(q4 + 1) * 512, c0:c0 + cw].re("(kc p) n -> p kc n", p=128))
            tokmajor = (2048 <= c0 < 3072)
            if tokmajor:
                for t in range(NTILE):
                    p = ps[pi % 4]
                    pi += 1
                    P.mmg(p.v(), [(hT[:, kc, t * 128:(t + 1) * 128], w[:, kc, :]) for kc in range(16)])
                    o = ob[oi % 2]
                    oi += 1
                    P.copy(o[:, :512], p.v(), e=("act" if t % 2 else "dve"))
                    P.dma("sp", D.utok[t * 128:(t + 1) * 128, c0 - 2048:c0 - 2048 + 512], o[:, :512])
                continue
            for blk in range((cw + 127) // 128):
                m = min(128, cw - blk * 128)
                o = ob[oi % 2]
                oi += 1
                for ci, (t0, tn) in enumerate(TOKCH):
                    p = ps[pi % 4]
                    pi += 1
                    P.mmg(p[:m, :tn], [(w[:, kc, blk * 128:blk * 128 + m], hT[:, kc, t0:t0 + tn]) for kc in range(16)])
                    P.copy(o[:m, t0:t0 + tn], p[:m, :tn], e=("act" if ci % 2 else "dve"))
                r0 = c0 + blk * 128
                P.dma("sp", D.uT[r0:r0 + m, :], o[:m, :])


def na_kr0(j):
    return min(max(2 * j - 4, 0), 22)


def na_pat(j):
    return 0 if j == 0 else 1 if j == 1 else 2 if j <= 13 else 3 if j == 14 else 4


def stage_na(P, D, L, need_ctx):
    with Stage(P) as st:
        qT = [st.sb(f"naq{i}", [64, NT], BF16) for i in range(2)]
        kT = [st.sb(f"nak{i}", [64, NT], BF16) for i in range(2)]
        va = [st.sb(f"nav{i}", [128, NTILE, 65], BF16) for i in range(2)]
        bias = [st.sb(f"nab{i}", [128, 5, 5, 128], F32) for i in range(2)]
        eloc = [st.sb(f"nae{i}", [128, 5, 128], F32) for i in range(2)]
        pT = [st.sb(f"nap{i}", [128, 7, 128], BF16) for i in range(2)]
        rec = [st.sb(f"nar{i}", [128, 1], F32) for i in range(2)]
        ytok = st.sb("nay", [128, NTILE, 64], BF16)
        yT = [st.sb(f"nayT{i}", [64, NT], BF16) for i in range(2)]
        ps_s = [st.ps(f"nps{i}", [128, 8, 128], F32) for i in range(2)]
        ps_o = [st.ps(f"npo{i}", [128, 65], F32) for i in range(2)]
        ps_t = st.ps("npt", [64, 8, 128], BF16)
        for i in range(2):
            P.memset(va[i][:, :, 64:65], 1.0)
        ntq = NTILE if need_ctx else 16
        u = 0
        for h in range(16):
            b = h % 2
            P.dma("sp", qT[b].v(), D.uT[h * 64:(h + 1) * 64, :])
            P.dma("sp", kT[b].v(), D.uT[1024 + h * 64:1024 + (h + 1) * 64, :])
            P.dma("sp", va[b][:, :, 0:64], D.utok[:, h * 64:(h + 1) * 64].re("(c p) d -> p c d", p=128))
            P.dma("sp", bias[b].v(), D.na_bias[L, h])
            for j in range(ntq):
                s = ps_s[u % 2]
                o = ps_o[u % 2]
                e_ = eloc[u % 2]
                p_ = pT[u % 2]
                r_ = rec[u % 2]
                u += 1
                q = qT[b][:, j * 128:(j + 1) * 128]
                if j < 16:
                    t0 = na_kr0(j) // 2
                    tiles = [t0 + c for c in range(5)] + [16, 17]
                else:
                    tiles = [16, 17]
                nk = len(tiles)
                P.group("pe", [(lambda c=c, tt_=tt_: P.nc.tensor.matmul(s[:, c, :].ap, kT[b][:, tt_ * 128:(tt_ + 1) * 128].ap, q.ap, start=True, stop=True))
                               for c, tt_ in enumerate(tiles)], [kT[b].v(), qT[b].v()], [s.v()])
                if j < 16:
                    P.stt(e_.v(), s[:, 0:5, :], 0.125, bias[b][:, na_pat(j), :, :], ALU.mult, ALU.add)
                    P.act(p_[:, 0:5, :], e_.v(), AF.Exp)
                    P.act(p_[:, 5:7, :], s[:, 5:7, :], AF.Exp, scale=0.125)
                else:
                    P.act(p_[:, 0:2, :], s[:, 0:2, :], AF.Exp, scale=0.125)
                P.mmg(o.v(), [(p_[:, c, :], va[b][:, tt_, :]) for c, tt_ in enumerate(tiles)])
                P.recip(r_.v(), o[:, 64:65])
                P.ts(ytok[:, j, :], o[:, 0:64], r_.v(), None, ALU.mult)
            for g0 in range(0, ntq, 8):
                gn = min(8, ntq - g0)
                for jj in range(gn):
                    P.transpose(ps_t[:, jj, :], ytok[:, g0 + jj, :], D.ident_bf.v())
                P.copy(yT[b][:, g0 * 128:(g0 + gn) * 128], ps_t[:, 0:gn, :], e="act")
            P.dma("sp", D.ys[0][h * 64:(h + 1) * 64, 0:ntq * 128], yT[b][:, 0:ntq * 128])


def stage_mla_prep(P, D, L, need_ctx):
    with Stage(P) as st:
        cq = st.sb("mcq", [128, 4, NT], BF16)
        ckv = st.sb("mckv", [128, 2, NT], BF16)
        kr = st.sb("mkr", [96, NT], BF16)
        krr = st.sb("mkrr", [96, NT], BF16)
        cs = st.sb("mcs", [96, NT], F32)
        sg = st.sb("msg", [96, NT], F32)
        perm = st.sb("mperm", [96, 96], BF16)
        ones = st.sb("mones", [128, 128], BF16)
        gq = st.sb("mgq", [128, 4], F32)
        gkv = st.sb("mgkv", [128, 2], F32)
        wq = st.sb("mwq", [128, 4, 1536], BF16)
        wkv = st.sb("mwkv", [128, 2, 2048], BF16)
        P.dma("sp", cq.v(), D.uT[OFF_CQ:OFF_CQ + 512, :].re("(c p) t -> p c t", p=128))
        P.dma("sp", ckv.v(), D.uT[OFF_CKV:OFF_CKV + 256, :].re("(c p) t -> p c t", p=128))
        P.memset(kr.v(), 0.0)
        P.dma("sp", kr[64:96, :], D.uT[OFF_KR:OFF_KR + 32, :])
        P.dma("sp", cs[64:96, :], D.rope_c.v())
        P.dma("sp", sg[64:96, :], D.rope_s.v())
        P.dma("pool", perm.v(), D.perm96.v())
        P.memset(ones.v(), 1.0)
        P.dma("sp", gq.v(), D.mla_q_norm_g[L].re("(c p) -> p c", p=128), allow_slow_non_contiguous=True)
        P.dma("sp", gkv.v(), D.mla_kv_norm_g[L].re("(c p) -> p c", p=128), allow_slow_non_contiguous=True)
        for kc in range(4):
            P.dma("pool", wq[:, kc, :], D.mla_w_uq[L, kc * 128:(kc + 1) * 128, :])
        for kc in range(2):
            P.dma("pool", wkv[:, kc, :], D.mla_w_ukv[L, kc * 128:(kc + 1) * 128, :])
        sq = st.sb("msq", [128, 4, 512], BF16)
        rs = st.sb("mrs", [128, 512], F32)
        ps = [st.ps(f"mps{i}", [128, 512], F32) for i in range(2)]
        psb = [st.ps(f"mpb{i}", [96, 512], F32) for i in range(2)]
        for (src, g, nblk) in ((cq, gq, 4), (ckv, gkv, 2)):
            for ci, (t0, tn) in enumerate(TOKCH):
                p = ps[ci % 2]
                P.act(sq[:, 0:nblk, :tn], src[:, :, t0:t0 + tn], AF.Square)
                P.mmg(p[:, :tn], [(ones.v(), sq[:, c, :tn]) for c in range(nblk)])
                P.ts(rs[:, :tn], p[:, :tn], 1.0 / (128 * nblk), 1e-6, ALU.mult, ALU.add)
                P.act(rs[:, :tn], rs[:, :tn], AF.Sqrt)
                P.recip(rs[:, :tn], rs[:, :tn])
                for c in range(nblk):
                    P.stt(src[:, c, t0:t0 + tn], src[:, c, t0:t0 + tn], g[:, c:c + 1], rs[:, :tn], ALU.mult, ALU.mult)
        tmp = st.sb("mtmp", [96, 512], F32)
        tmp2 = st.sb("mtmp2", [96, 512], F32)
        for ci, (t0, tn) in enumerate(TOKCH):
            p = psb[ci % 2]
            P.mm(p[:, :tn], perm.v(), kr[:, t0:t0 + tn])
            P.tt(tmp[64:96, :tn], p[64:96, :tn], sg[64:96, t0:t0 + tn], ALU.mult)
            P.tt(tmp2[64:96, :tn], kr[64:96, t0:t0 + tn], cs[64:96, t0:t0 + tn], ALU.mult)
            P.tt(krr[64:96, t0:t0 + tn], tmp[64:96, :tn], tmp2[64:96, :tn], ALU.add)
        for h in range(16):
            P.dma("sp", D.kmT[h, 64:96, :], krr[64:96, :])
        qh = [st.sb(f"mqh{i}", [96, NT], BF16) for i in range(2)]
        kh = [st.sb(f"mkh{i}", [64, NT], BF16) for i in range(2)]
        for h in range(16):
            q_ = qh[h % 2]
            k_ = kh[h % 2]
            for ci, (t0, tn) in enumerate(TOKCH):
                if t0 >= T and not need_ctx:
                    continue
                pa = ps[ci % 2]
                pb = psb[ci % 2]
                P.mmg(pa[:96, :tn], [(wq[:, kc, h * 96:(h + 1) * 96], cq[:, kc, t0:t0 + tn]) for kc in range(4)])
                P.copy(q_[:, t0:t0 + tn], pa[:96, :tn], e="act")
                P.mm(pb[:, :tn], perm.v(), q_[:, t0:t0 + tn])
                P.tt(tmp[64:96, :tn], pb[64:96, :tn], sg[64:96, t0:t0 + tn], ALU.mult)
                P.tt(tmp2[64:96, :tn], pa[64:96, :tn], cs[64:96, t0:t0 + tn], ALU.mult)
                P.tt(q_[64:96, t0:t0 + tn], tmp[64:96, :tn], tmp2[64:96, :tn], ALU.add)
            nq = NT if need_ctx else T
            P.dma("sp", D.qmT[h, :, 0:nq], q_[:, 0:nq])
            for ci, (t0, tn) in enumerate(TOKCH):
                pa = ps[ci % 2]
                P.mmg(pa[:64, :tn], [(wkv[:, kc, h * 128:h * 128 + 64], ckv[:, kc, t0:t0 + tn]) for kc in range(2)])
                P.copy(k_[:, t0:t0 + tn], pa[:64, :tn], e="act")
            P.dma("sp", D.kmT[h, 0:64, :], k_.v())
        vo = [st.sb(f"mvo{i}", [128, 1024], BF16) for i in range(2)]
        wv = wkv.v().re("p kc (h two d) -> p kc h two d", two=2, d=64)
        for t in range(NTILE):
            o = vo[t % 2]
            for half in range(2):
                pa = ps[half]
                P.mmg(pa.v().re("p (h d) -> p h d", d=64),
                      [(ckv[:, kc, t * 128:(t + 1) * 128], wv[:, kc, half * 8:(half + 1) * 8, 1, :]) for kc in range(2)])
                P.copy(o[:, half * 512:(half + 1) * 512], pa.v(), e=("act" if half else "dve"))
            P.dma("sp", D.vmtok[t * 128:(t + 1) * 128, :], o.v())


def stage_mla_attn(P, D, L, need_ctx):
    sc = 1.0 / math.sqrt(96.0)
    with Stage(P) as st:
        qT = [st.sb(f"aq{i}", [96, NT], BF16) for i in range(2)]
        kT = [st.sb(f"ak{i}", [96, NT], BF16) for i in range(2)]
        va = [st.sb(f"av{i}", [128, NTILE, 65], BF16) for i in range(2)]
        pT = [st.sb(f"ap{i}", [128, 512], BF16) for i in range(3)]
        rec = [st.sb(f"ar{i}", [128, 1], F32) for i in range(2)]
        ytok = st.sb("ay", [128, NTILE, 64], BF16)
        yT = [st.sb(f"ayT{i}", [64, NT], BF16) for i in range(2)]
        ps_s = [st.ps(f"aps{i}", [128, 512], F32) for i in range(2)]
        ps_o = [st.ps(f"apo{i}", [128, 65], F32) for i in range(4)]
        ps_t = st.ps("apt", [64, 8, 128], BF16)
        for i in range(2):
            P.memset(va[i][:, :, 64:65], 1.0)
        ntq = NTILE if need_ctx else 16
        u = 0
        for h in range(16):
            b = h % 2
            nq = NT if need_ctx else T
            P.dma("sp", qT[b][:, 0:nq], D.qmT[h, :, 0:nq])
            P.dma("sp", kT[b].v(), D.kmT[h])
            P.dma("sp", va[b][:, :, 0:64], D.vmtok[:, h * 64:(h + 1) * 64].re("(c p) d -> p c d", p=128))
            blocks = [(0, 512, list(range(18))), (512, 512, list(range(18))), (1024, 512, list(range(18))), (1536, 512, list(range(18)))]
            if need_ctx:
                blocks.append((2048, 256, [16, 17]))
            for (q0, qn, tiles) in blocks:
                nqi = qn // 128
                for ci, tt_ in enumerate(tiles):
                    s = ps_s[u % 2]
                    p_ = pT[u % 3]
                    u += 1
                    P.mm(s[:, :qn], kT[b][:, tt_ * 128:(tt_ + 1) * 128], qT[b][:, q0:q0 + qn])
                    P.act(p_[:, :qn], s[:, :qn], AF.Exp, scale=sc)
                    for qi in range(nqi):
                        P.mm(ps_o[qi].v(), p_[:, qi * 128:(qi + 1) * 128], va[b][:, tt_, :], start=(ci == 0), stop=(ci == len(tiles) - 1))
                for qi in range(nqi):
                    r_ = rec[qi % 2]
                    P.recip(r_.v(), ps_o[qi][:, 64:65])
                    P.ts(ytok[:, q0 // 128 + qi, :], ps_o[qi][:, 0:64], r_.v(), None, ALU.mult)
            for g0 in range(0, ntq, 8):
                gn = min(8, ntq - g0)
                for jj in range(gn):
                    P.transpose(ps_t[:, jj, :], ytok[:, g0 + jj, :], D.ident_bf.v())
                P.copy(yT[b][:, g0 * 128:(g0 + gn) * 128], ps_t[:, 0:gn, :], e="dve")
            P.dma("sp", D.ys[2][h * 64:(h + 1) * 64, 0:ntq * 128], yT[b][:, 0:ntq * 128])


def ek(a, b):
    return tuple(f"e{i}" for i in range(a, b))


def rw_shift(P, X, om, hm, out, tmp, tmp2, n_):
    for (a, n) in ((0, T), (T, TC)):
        P.tt(tmp[:n_, a + 1:a + n - 1], X[:n_, a:a + n - 2], X[:n_, a + 2:a + n], ALU.add, e="pool")
        P.copy(tmp[:n_, a:a + 1], X[:n_, a + 1:a + 2], e="pool")
        P.copy(tmp[:n_, a + n - 1:a + n], X[:n_, a + n - 2:a + n - 1], e="pool")
    P.ts(tmp2[:n_], X[:n_], om, None, ALU.mult)
    P.stt(out[:n_], tmp[:n_], hm, tmp2[:n_], ALU.mult, ALU.add)


def stage_rw(P, D, L, need_ctx, heads=range(16), dbgd=None, on_head=None):
    C0 = math.exp(-0.5)
    nc = P.nc
    NS_ = 4
    with Stage(P) as st:
        masks = [st.sb(f"rwm{i}", [128, 128], F32) for i in range(4)]
        for i in range(4):
            P.dma("sp", masks[i].v(), D.tri_masks[i])
        identf = st.sb("rwid", [128, 128], F32)
        P.dma("sp", identf.v(), D.ident_f.v())
        ones_f = st.sb("rwof", [64, 64], F32)
        P.memset(ones_f.v(), 1.0)
        rmask = st.sb("rwrm", [64, NTILE, 128], BF16)
        P.memset(rmask.v(), 1.0)
        P.memset(rmask[:, :, 0:1], 0.0)

        def col16(name, src):
            t = st.sb(name, [64, 16], F32)
            P.dma("sp", t.v(), src.re("(h p) -> p h", p=64), allow_slow_non_contiguous=True)
            return t

        def col1(name, src, n):
            t = st.sb(name, [n, 1], F32)
            P.dma("sp", t.v(), src.re("(p o) -> p o", o=1), allow_slow_non_contiguous=True)
            return t

        kk_c = col16("ckk", D.rw_k_k[L])
        ka_c = col16("cka", D.rw_k_a[L])
        rk_c = col16("crk", D.rw_r_k[L].re("h d -> (h d)"))
        gnw_c = col16("cgw", D.rw_gn_w[L])
        gnb_c = col16("cgb", D.rw_gn_b[L])
        w0_c = [col16(f"cw0{z}", D.rw_w0[L, z]) for z in range(2)]
        a0_c = [col16(f"ca0{z}", D.rw_a0[L, z]) for z in range(2)]
        omka = st.sb("comka", [64, 16], F32)
        P.ts(omka.v(), ka_c.v(), -1.0, 1.0, ALU.mult, ALU.add)
        mu = D.rw_shift_mu[L]
        mus = {}
        for nm, src, kind in (("r", mu[0:1024], 16), ("k", mu[1024:2048], 16), ("v", mu[2048:3072], 16),
                              ("wd", mu[3072:3200], 128), ("ad", mu[3200:3328], 128), ("gd", mu[3328:3456], 128), ("gd2", mu[3456:3488], 32)):
            m_ = col16("mu" + nm, src) if kind == 16 else col1("mu" + nm, src, kind)
            shp = [64, 16] if kind == 16 else [kind, 1]
            om = st.sb("om" + nm, shp, F32)
            hm = st.sb("hm" + nm, shp, F32)
            P.ts(om.v(), m_.v(), -1.0, 1.0, ALU.mult, ALU.add)
            P.ts(hm.v(), m_.v(), 0.5, None, ALU.mult)
            mus[nm] = (om, hm)

        wup = st.sb("rwwup", [128, 1024], BF16)
        aup = st.sb("rwaup", [128, 1024], BF16)
        gup = st.sb("rwgup", [128, 1024], BF16)
        gup2 = st.sb("rwgup2", [32, 1024], BF16)
        P.dma("pool", wup.v(), D.rw_w_up[L].re("z l c -> (z l) c"))
        P.dma("pool", aup.v(), D.rw_a_up[L].re("z l c -> (z l) c"))
        P.dma("pool", gup.v(), D.rw_g_up[L, 0:128, :])
        P.dma("pool", gup2.v(), D.rw_g_up[L, 128:160, :])

        tA = st.sb("rwtA", [128, NT], BF16)
        tB = st.sb("rwtB", [128, NT], BF16)
        xin = st.sb("rwxin", [128, NT], BF16)
        twd = st.sb("rwtwd", [128, NT], BF16)
        ads = st.sb("rwads", [128, NT], BF16)
        sgd = st.sb("rwsgd", [128, NT], BF16)
        sgd2 = st.sb("rwsgd2", [32, NT], BF16)
        base = OFF_RW
        for (dst, r0, n_, key, fn) in ((twd, 3072, 128, "wd", AF.Tanh), (ads, 3200, 128, "ad", AF.Copy),
                                       (sgd, 3328, 128, "gd", AF.Sigmoid), (sgd2, 3456, 32, "gd2", AF.Sigmoid)):
            P.dma("sp", xin[:n_, :], D.uT[base + r0:base + r0 + n_, :])
            rw_shift(P, xin, mus[key][0].v(), mus[key][1].v(), tA, tA, tB, n_)
            P.act(dst[:n_, :], tA[:n_, :], fn)

        bk = [st.ps(f"rwbk{i}", [128, 512], F32) for i in range(8)]
        r_s = st.sb("rwr", [64, NT], BF16)
        k_s = st.sb("rwk", [64, NT], BF16)
        v_s = st.sb("rwv", [64, NT], BF16)
        g_s = st.sb("rwg", [64, NT], BF16)
        kk = st.sb("rwkk", [64, NT], F32)
        f_sg = st.sb("rwsg", [64, NT], F32)
        f_a = st.sb("rwa", [64, NT], F32)
        f_t = st.sb("rwt", [64, NT], F32)
        f_L = st.sb("rwL", [64, NTILE, 128], F32)
        f_E = st.sb("rwE", [64, NTILE, 128], F32)
        l63 = st.sb("rwl63", [64, NTILE, 1], F32)
        eA = st.sb("rweA", [64, NTILE, 1], F32)
        eB = st.sb("rweB", [64, NTILE, 1], F32)
        scl = st.sb("rwscl", [64, NTILE], F32)
        rT = st.sb("rwrT", [64, NT], BF16)
        kapT = st.sb("rwkapT", [64, NT], BF16)
        ktT = st.sb("rwktT", [64, NT], BF16)
        bT = st.sb("rwbT", [64, NT], BF16)
        tokz = st.sb("rwtokz", [128, NTILE, 3, 64], BF16)
        vtok = st.sb("rwvtok", [128, NTILE, 64], BF16)
        MA = st.sb("rwMA", [64, NTILE, 192], BF16)
        MB = st.sb("rwMB", [128, NTILE, 192], BF16)
        yacc = st.sb("rwy", [64, NT], F32)
        Nsb = [st.sb(f"rwN{i}", [128, 128], F32) for i in range(NS_)]
        NTsb = [st.sb(f"rwNT{i}", [128, 128], F32) for i in range(NS_)]
        PP = [[st.sb(f"rwPP{i}_{j}", [128, 2, 128], F32) for j in range(2)] for i in range(NS_)]
        ZZ = [[st.sb(f"rwZ{i}_{j}", [128, 192], F32) for j in range(2)] for i in range(NS_)]
        rc = [st.sb(f"rwrc{i}", [128, 192], F32) for i in range(NS_)]
        akr = [st.sb(f"rwakr{i}", [128, 128], F32) for i in range(NS_)]
        ST = [st.sb(f"rwST{i}", [64, 64], BF16) for i in range(2)]
        idb = D.ident_bf

        for h in heads:
            hc = slice(h, h + 1)
            if on_head is not None:
                on_head(h)
            for (dst, r0, key) in ((r_s, 0, "r"), (k_s, 1024, "k"), (v_s, 2048, "v")):
                P.dma("sp", xin[:64, :], D.uT[base + r0 + h * 64:base + r0 + (h + 1) * 64, :])
                rw_shift(P, xin, mus[key][0][:, hc], mus[key][1][:, hc], dst, tA, tB, 64)
            for ci, (t0, tn) in enumerate(TOKCH):
                ps = bk[ci % 8]
                P.mmg(ps[:64, :tn], [(gup[:, h * 64:(h + 1) * 64], sgd[:, t0:t0 + tn]), (gup2[:32, h * 64:(h + 1) * 64], sgd2[:32, t0:t0 + tn])])
                P.copy(g_s[:, t0:t0 + tn], ps[:64, :tn], e="act")
            P.ts(f_t.v(), k_s.v(), kk_c[:, hc], None, ALU.mult)
            P.act(f_a.v(), f_t.v(), AF.Square)
            for ci, (t0, tn) in enumerate(TOKCH):
                ps = bk[(ci + 5) % 8]
                P.mm(ps[:64, :tn], ones_f.v(), f_a[:, t0:t0 + tn])
                P.act(f_sg[:, t0:t0 + tn], ps[:64, :tn], AF.Sqrt)
            P.ts(f_sg.v(), f_sg.v(), 1e-12, None, ALU.max)
            P.recip(f_sg.v(), f_sg.v())
            P.tt(kk.v(), f_t.v(), f_sg.v(), ALU.mult)
            for c in range(NTILE):
                pb = bk[c % 8].v().bc(BF16)
                P.transpose(pb[:, 0:64], v_s[:, c * 128:(c + 1) * 128], idb[0:64, 0:64])
                P.copy(vtok[:, c, :], pb[:, 0:64], e="act")

            for z in range(2):
                zs = slice(z * 64, (z + 1) * 64)
                mS, mST, mD = (masks[0], masks[1], masks[2]) if z == 0 else (masks[1], masks[0], masks[3])
                for ci, (t0, tn) in enumerate(TOKCH):
                    ps = bk[ci % 8]
                    P.mm(ps[:64, :tn], wup[zs, h * 64:(h + 1) * 64], twd[zs, t0:t0 + tn])
                    P.act(f_sg[:, t0:t0 + tn], ps[:64, :tn], AF.Sigmoid, bias=w0_c[z][:, hc])
                    ps2 = bk[(ci + 4) % 8]
                    P.mm(ps2[:64, :tn], aup[zs, h * 64:(h + 1) * 64], ads[zs, t0:t0 + tn])
                    P.act(f_a[:, t0:t0 + tn], ps2[:64, :tn], AF.Sigmoid, bias=a0_c[z][:, hc])
                P.ts(f_t.v(), f_a.v(), ka_c[:, hc], omka[:, hc], ALU.mult, ALU.add)
                P.tt(f_t.v(), f_t.v(), k_s.v(), ALU.mult)
                P.tt(f_a.v(), f_a.v(), kk.v(), ALU.mult)
                L2 = f_L.v().re("p c t -> p (c t)")
                P.op("dve", lambda: nc.vector.tensor_tensor_scan(L2.ap, rmask.v().re("p c t -> p (c t)").ap, f_sg.ap() , 0.0, ALU.mult, ALU.add),
                     [rmask.v(), f_sg.v()], [f_L.v()])
                P.copy(l63.v(), f_L[:, :, 63:64])
                P.tt(f_L.v(), f_L.v(), l63.v().m(lambda a: a.broadcast_to([64, NTILE, 128])), ALU.subtract)
                P.tt(f_sg.v(), L2, f_sg.v(), ALU.subtract)
                E2d = f_E.v().re("p c t -> p (c t)")
                P.act(E2d, L2, AF.Exp, scale=-C0)
                P.copy(eA.v(), f_E[:, :, 127:128])
                if z == 0:
                    P.tt(rT.v(), r_s.v(), E2d, ALU.mult)
                P.act(E2d, L2, AF.Exp, scale=C0)
                if z == 0:
                    P.tt(ktT.v(), f_t.v(), E2d, ALU.mult)
                    P.tt(bT.v(), f_a.v(), E2d, ALU.mult)
                else:
                    P.tt(kapT.v(), kk.v(), E2d, ALU.mult)
                P.act(E2d, f_sg.v(), AF.Exp, scale=-C0)
                if z == 0:
                    P.tt(kapT.v(), kk.v(), E2d, ALU.mult)
                else:
                    P.tt(ktT.v(), f_t.v(), E2d, ALU.mult)
                    P.tt(bT.v(), f_a.v(), E2d, ALU.mult)
                P.act(E2d, f_sg.v(), AF.Exp, scale=C0)
                P.copy(eB.v(), f_E[:, :, 0:1])
                if z == 1:
                    P.tt(rT.v(), r_s.v(), E2d, ALU.mult)
                eA2 = eA.v().re("p c o -> p (c o)")
                eB2 = eB.v().re("p c o -> p (c o)")
                P.memset(scl.v(), 1.0)
                if z == 0:
                    P.tt(scl[:, 0:15], eA2[:, 0:15], eB2[:, 1:16], ALU.mult)
                    P.tt(scl[:, 16:17], eA2[:, 16:17], eB2[:, 17:18], ALU.mult)
                    P.tt(scl[:, 17:18], eA2[:, 17:18], eB2[:, 0:1], ALU.mult)
                else:
                    P.tt(scl[:, 1:18], eB2[:, 1:18], eA2[:, 0:17], ALU.mult)
                for c in range(NTILE):
                    cs_ = slice(c * 128, (c + 1) * 128)
                    pb = bk[c % 8].v().bc(BF16)
                    P.transpose(pb[:, 0:64], kapT[:, cs_], idb[0:64, 0:64])
                    P.transpose(pb[:, 64:128], ktT[:, cs_], idb[0:64, 0:64])
                    P.transpose(pb[:, 128:192], bT[:, cs_], idb[0:64, 0:64])
                    P.copy(tokz[:, c, :, :], pb[:, 0:192].re("p (a d) -> p a d", a=3), e=("act" if c % 2 else "dve"))
                for c0 in range(0, NTILE, NS_):
                    units = list(range(c0, min(NTILE, c0 + NS_)))
                    for s, c in enumerate(units):
                        cs_ = slice(c * 128, (c + 1) * 128)
                        b = bk[s]
                        P.mm(b[:, 0:128].k(ek(0, 2)), bT[:, cs_], kapT[:, cs_])
                        P.mm(b[:, 128:256].k(ek(2, 4)), kapT[:, cs_], bT[:, cs_])
                        P.mm(b[:, 256:384].k(ek(4, 6)), kapT[:, cs_], ktT[:, cs_])
                        P.mm(b[:, 384:512].k(ek(6, 8)), bT[:, cs_], rT[:, cs_])
                        P.tt(Nsb[s].v(), b[:, 0:128].k(ek(0, 2)), mS.v(), ALU.mult)
                        P.tt(NTsb[s].v(), b[:, 128:256].k(ek(2, 4)), mST.v(), ALU.mult)
                        P.tt(ZZ[s][0][:, 64:192], b[:, 256:384].k(ek(4, 6)), mST.v(), ALU.mult)
                        P.tt(rc[s][:, 0:128], b[:, 384:512].k(ek(6, 8)), mD.v(), ALU.mult)
                        P.copy(ZZ[s][0][:, 0:64], tokz[:, c, 0, :], e="pool")
                        P.copy(rc[s][:, 128:192], tokz[:, c, 2, :], e="pool")
                    for s, c in enumerate(units):
                        b = bk[s]
                        P.mm(b[:, 256:448].k(ek(4, 7)), Nsb[s].v(), ZZ[s][0].v())
                        P.mm(b[:, 0:128].k(ek(0, 2)), NTsb[s].v(), Nsb[s].v())
                        P.mm(b[:, 128:256].k(ek(2, 4)), Nsb[s].v(), NTsb[s].v())
                        P.tt(ZZ[s][1].v(), ZZ[s][0].v(), b[:, 256:448].k(ek(4, 7)), ALU.subtract)
                        P.copy(PP[s][0].v(), b[:, 0:256].k(ek(0, 4)), e="act")
                    for j in range(1, 7):
                        for s, c in enumerate(units):
                            b = bk[s]
                            pc = PP[s][(j - 1) % 2]
                            pn = PP[s][j % 2]
                            zc = ZZ[s][j % 2]
                            zn = ZZ[s][(j + 1) % 2]
                            P.mm(b[:, 256:448].k(ek(4, 7)), pc[:, 0, :], zc.v())
                            if j < 6:
                                P.mm(b[:, 0:128].k(ek(0, 2)), pc[:, 1, :], pc[:, 0, :])
                                if j < 5:
                                    P.mm(b[:, 128:256].k(ek(2, 4)), pc[:, 0, :], pc[:, 1, :])
                            P.tt(zn.v(), zc.v(), b[:, 256:448].k(ek(4, 7)), ALU.add)
                            if j < 5:
                                P.copy(pn.v(), b[:, 0:256].k(ek(0, 4)), e="act")
                            elif j == 5:
                                P.copy(pn[:, 0, :], b[:, 0:128].k(ek(0, 2)), e="act")
                    for s, c in enumerate(units):
                        cs_ = slice(c * 128, (c + 1) * 128)
                        b = bk[s]
                        zf = ZZ[s][1]
                        P.mm(b[:64, 0:192].k(ek(0, 3)), zf[:, 0:64], rc[s].v())
                        P.mm(b[:, 192:384].k(ek(3, 6)), zf[:, 64:192], rc[s].v())
                        P.mm(b[:, 384:512].k(ek(6, 8)), ktT[:, cs_], rT[:, cs_])
                        P.tt(akr[s].v(), b[:, 384:512].k(ek(6, 8)), mD.v(), ALU.mult)
                        P.tt(MA[:, c, 0:128], rT[:, cs_], b[:64, 0:128].k(ek(0, 2)), ALU.subtract)
                        P.tt(MA[:, c, 128:192], identf[0:64, 0:64], b[:64, 128:192].k(ek(2, 3)), ALU.subtract)
                        P.tt(MB[:, c, 0:128], akr[s].v(), b[:, 192:320].k(ek(3, 5)), ALU.subtract)
                        P.tt(MB[:, c, 128:192], tokz[:, c, 1, :], b[:, 320:384].k(ek(5, 6)), ALU.subtract)
                order = ([16, 17] + list(range(16))) if z == 0 else ([17, 16] + list(range(15, -1, -1)))
                P.memset(ST[0].v(), 0.0)
                for i, c in enumerate(order):
                    cur = ST[i % 2]
                    nxt = ST[(i + 1) % 2]
                    bY = bk[(2 * i) % 8]
                    bS = bk[(2 * i + 1) % 8]
                    P.mmg(bY[:64, 0:128].k(ek(0, 2)), [(cur.v(), MA[:, c, 0:128]), (vtok[:, c, :], MB[:, c, 0:128])])
                    P.mmg(bS[:64, 0:64].k(ek(0, 1)), [(MA[:, c, 128:192], cur.v()), (MB[:, c, 128:192], vtok[:, c, :])])
                    if i < NTILE - 1:
                        P.ts(nxt.v(), bS[:64, 0:64].k(ek(0, 1)), scl[:, c:c + 1], None, ALU.mult)
                    ysl = yacc[:, c * 128:(c + 1) * 128]
                    if z == 0:
                        P.copy(ysl, bY[:64, 0:128].k(ek(0, 2)), e="act")
                    else:
                        P.tt(ysl, ysl, bY[:64, 0:128].k(ek(0, 2)), ALU.add)
            if dbgd is not None:
                P.dma("sp", dbgd[h * 64:(h + 1) * 64, :], yacc.v())
            E2o = f_E.v().re("p c t -> p (c t)")
            yo = rT
            L2o = f_L.v().re("p c t -> p (c t)")
            for ci, (t0, tn) in enumerate(TOKCH):
                ts_ = slice(t0, t0 + tn)
                b1, b2, b3 = bk[(3 * ci) % 8], bk[(3 * ci + 1) % 8], bk[(3 * ci + 2) % 8]
                P.act(L2o[:, 0:tn], yacc[:, ts_], AF.Square)
                P.mm(b1[:64, :tn], ones_f.v(), yacc[:, ts_])
                P.mm(b2[:64, :tn], ones_f.v(), L2o[:, 0:tn])
                P.stt(L2o[:, 512:512 + tn], r_s[:, ts_], rk_c[:, hc], k_s[:, ts_], ALU.mult, ALU.mult)
                P.mm(b3[:64, :tn], ones_f.v(), L2o[:, 512:512 + tn])
                mean = E2o[:, 0:tn]
                var = E2o[:, 512:512 + tn]
                dd = E2o[:, 1024:1024 + tn]
                P.ts(mean, b1[:64, :tn], 1.0 / 64, None, ALU.mult)
                P.tt(var, mean, mean, ALU.mult)
                P.stt(var, b2[:64, :tn], 1.0 / 64, var, ALU.mult, ALU.subtract)
                P.ts(var, var, 64e-5, None, ALU.add)
                P.act(var, var, AF.Sqrt)
                P.recip(var, var)
                P.tt(dd, yacc[:, ts_], mean, ALU.subtract)
                P.tt(dd, dd, var, ALU.mult)
                P.ts(dd, dd, gnw_c[:, hc], gnb_c[:, hc], ALU.mult, ALU.add)
                P.tt(var, b3[:64, :tn], v_s[:, ts_], ALU.mult)
                P.tt(dd, dd, var, ALU.add)
                P.tt(yo[:, ts_], dd, g_s[:, ts_], ALU.mult)
            P.dma("sp", D.ys[1][h * 64:(h + 1) * 64, :], yo.v())

def stage_merge(P, D, L, need_ctx):
    chunks = TOKCH if need_ctx else TOKCH[:4]
    ntile = NTILE if need_ctx else 16
    with Stage(P) as st:
        mT = st.sb("mgT", [128, 16, NT], BF16)
        with Stage(P) as sa:
            Y = [sa.sb(f"mgY{i}", [128, 24, 512], BF16) for i in range(2)]
            wb = [sa.sb(f"mgw{i}", [128, 3, 8, 128], BF16) for i in range(2)]
            gt = [sa.sb(f"mgg{i}", [128, 3, 512], BF16) for i in range(2)]
            sg = [sa.sb(f"mgs{i}", [128, 3, 512], F32) for i in range(2)]
            t0_ = sa.sb("mgt0", [128, 512], F32)
            t1_ = sa.sb("mgt1", [128, 512], F32)
            ps = [sa.ps(f"mgp{i}", [128, 512], F32) for i in range(6)]
            u = 0
            for ci, (t0, tn) in enumerate(chunks):
                y = Y[ci % 2]
                for z in range(3):
                    P.dma("sp", y[:, z * 8:(z + 1) * 8, :tn], D.ys[z][:, t0:t0 + tn].re("(kc p) t -> p kc t", p=128))
                for ob in range(16):
                    w = wb[u % 2]
                    g = gt[u % 2]
                    s_ = sg[u % 2]
                    pp = ps[(u % 2) * 3:(u % 2) * 3 + 3]
                    u += 1
                    for z in range(3):
                        P.dma("sp", w[:, z, :, :], D.wbr_bf[L][z][:, ob * 128:(ob + 1) * 128].re("(kc p) n -> p kc n", p=128))
                        r0 = OFF_G + z * DM + ob * 128
                        P.dma("sp", g[:, z, :tn], D.uT[r0:r0 + 128, t0:t0 + tn])
                    P.act(s_[:, :, :tn], g[:, :, :tn], AF.Sigmoid)
                    for z in range(3):
                        P.mmg(pp[z][:, :tn], [(w[:, z, kc, :], y[:, z * 8 + kc, :tn]) for kc in range(8)])
                    P.tt(t0_[:, :tn], pp[0][:, :tn], s_[:, 0, :tn], ALU.mult)
                    P.tt(t1_[:, :tn], pp[1][:, :tn], s_[:, 1, :tn], ALU.mult)
                    P.tt(t0_[:, :tn], t0_[:, :tn], t1_[:, :tn], ALU.add)
                    P.tt(t1_[:, :tn], pp[2][:, :tn], s_[:, 2, :tn], ALU.mult)
                    P.tt(mT[:, ob, t0:t0 + tn], t0_[:, :tn], t1_[:, :tn], ALU.add)
        with Stage(P) as sb_:
            wo = [sb_.sb(f"mow{i}", [128, 16, 512], BF16) for i in range(2)]
            g1 = [[sb_.sb(f"mog{i}_{r}", [128, 512], F32) for r in range(2)] for i in range(2)]
            xt = [sb_.sb(f"mox{i}", [128, 512], F32) for i in range(3)]
            tm = [sb_.sb(f"mot{i}", [128, 512], F32) for i in range(2)]
            ps = [sb_.ps(f"mop{i}", [128, 512], F32) for i in range(4)]
            u = 0
            for s in range(4):
                cs_ = slice(s * 512, (s + 1) * 512)
                w = wo[s % 2]
                P.dma("sp", w.v(), D.wout_bf[L][:, cs_].re("(kc p) n -> p kc n", p=128))
                for r in range(2):
                    P.dma("sp", g1[s % 2][r].v(), bc_row(D.modv[L][r:r + 1, 2 * DM + s * 512:2 * DM + (s + 1) * 512]))
                for t in range(ntile):
                    r = 0 if t < 16 else 1
                    x = xt[u % 3]
                    tmp = tm[u % 2]
                    p = ps[u % 4]
                    u += 1
                    P.dma("sp", x.v(), D.xcur[t * 128:(t + 1) * 128, cs_])
                    P.mmg(p.v(), [(mT[:, kc, t * 128:(t + 1) * 128], w[:, kc, :]) for kc in range(16)])
                    P.tt(tmp.v(), p.v(), g1[s % 2][r].v(), ALU.mult)
                    P.tt(x.v(), x.v(), tmp.v(), ALU.add)
                    P.dma("sp", D.xcur[t * 128:(t + 1) * 128, cs_], x.v())


def stage_norm2_router(P, D, L, need_ctx):
    ntile = NTILE if need_ctx else 16
    with Stage(P) as st:
        h2T = st.sb("h2T", [128, 16, NT], BF16)
        stage_norm(P, D, L, 1, h2T, st)
        P.dma("sp", D.h2Td.v().re("(c p) t -> p c t", p=128), h2T.v())
        rw = st.sb("rtw", [128, 16, 16], BF16)
        P.dma("pool", rw.v(), D.router_w.v().re("(kc p) e -> p kc e", p=128))
        rb = st.sb("rtb", [128, 16], F32)
        P.dma("sp", rb.v(), bc_row(D.router_bias.v().re("(o e) -> o e", o=1)))
        identf = st.sb("rtid", [128, 128], F32)
        P.dma("sp", identf.v(), D.ident_f.v())
        wT = st.sb("rtwT", [16, NT], F32)
        ps = [st.ps(f"rtp{i}", [128, 16], F32) for i in range(2)]
        pst = [st.ps(f"rtq{i}", [16, 128], F32) for i in range(2)]

        def tl(n, shape):
            return [st.sb(f"{n}{i}", shape, F32) for i in range(2)]
        sc, sel, eq, s2, m1, m2, grp, gm, geq, t1, oh1, t2, oh2, den = (tl("rsc", [128, 16]), tl("rsel", [128, 16]), tl("req", [128, 16]), tl("rs2", [128, 16]),
                                                                     tl("rm1", [128, 4]), tl("rm2", [128, 4]), tl("rgrp", [128, 4]), tl("rgm", [128, 1]), tl("rgeq", [128, 4]),
                                                                     tl("rt1", [128, 1]), tl("roh1", [128, 16]), tl("rt2", [128, 1]), tl("roh2", [128, 16]), tl("rden", [128, 1]))
        BIG = 1.0e9

        def g4(v):
            return v.re("p (g e) -> p g e", e=4)

        def b4(v):
            return v.m(lambda a: a.unsqueeze(2).broadcast_to([128, 4, 4]))

        def b16(v):
            return v.m(lambda a: a.broadcast_to([128, 16]))
        for t in range(ntile):
            i = t % 2
            P.mmg(ps[i].v(), [(h2T[:, kc, t * 128:(t + 1) * 128], rw[:, kc, :]) for kc in range(16)])
            P.act(sc[i].v(), ps[i].v(), AF.Sigmoid)
            P.tt(sel[i].v(), sc[i].v(), rb.v(), ALU.add)
            P.reduce(m1[i].v(), g4(sel[i].v()), ALU.max)
            P.tt(g4(eq[i].v()), g4(sel[i].v()), b4(m1[i].v()), ALU.is_equal)
            P.stt(s2[i].v(), eq[i].v(), -BIG, sel[i].v(), ALU.mult, ALU.add)
            P.reduce(m2[i].v(), g4(s2[i].v()), ALU.max)
            P.tt(grp[i].v(), m1[i].v(), m2[i].v(), ALU.add)
            P.reduce(gm[i].v(), grp[i].v(), ALU.max)
            P.ts(geq[i].v(), grp[i].v(), gm[i].v(), None, ALU.is_equal)
            P.ts(geq[i].v(), geq[i].v(), BIG, -BIG, ALU.mult, ALU.add)
            P.tt(g4(s2[i].v()), g4(sel[i].v()), b4(geq[i].v()), ALU.add)
            P.reduce(t1[i].v(), s2[i].v(), ALU.max)
            P.ts(oh1[i].v(), s2[i].v(), t1[i].v(), None, ALU.is_equal)
            P.stt(eq[i].v(), oh1[i].v(), -BIG, s2[i].v(), ALU.mult, ALU.add)
            P.reduce(t2[i].v(), eq[i].v(), ALU.max)
            P.ts(oh2[i].v(), eq[i].v(), t2[i].v(), None, ALU.is_equal)
            P.tt(oh1[i].v(), oh1[i].v(), oh2[i].v(), ALU.add)
            P.tt(oh1[i].v(), oh1[i].v(), sc[i].v(), ALU.mult)
            P.reduce(den[i].v(), oh1[i].v(), ALU.add)
            P.recip(den[i].v(), den[i].v())
            P.ts(oh1[i].v(), oh1[i].v(), den[i].v(), None, ALU.mult)
            P.transpose(pst[i].v(), oh1[i].v(), identf.v())
            P.copy(wT[:, t * 128:(t + 1) * 128], pst[i].v(), e="act")
        P.dma("sp", D.wgtT[:, 0:ntile * 128], wT[:, 0:ntile * 128])


def stage_moe(P, D, L, need_ctx):
    ntok = NT if need_ctx else T
    passes = []
    p0 = 0
    while p0 < ntok:
        pn = min(768, ntok - p0)
        passes.append((p0, pn))
        p0 += pn
    with Stage(P) as st:
        identf = st.sb("moid", [128, 128], F32)
        P.dma("sp", identf.v(), D.ident_f.v())
        selE = st.sb("mosel", [16, 16, 128], F32)
        P.dma("sp", selE.v(), D.selE.v())
        wT = st.sb("mowT", [16, NT], F32)
        P.dma("sp", wT[:, 0:ntok], D.wgtT[:, 0:ntok])
        g2c = [st.sb(f"mog2{r}", [128, 16], F32) for r in range(2)]
        for r in range(2):
            P.dma("sp", g2c[r].v(), D.modv[L][r, 5 * DM:6 * DM].re("(o p) -> p o", p=128), allow_slow_non_contiguous=True)
        h2 = st.sb("moh2", [128, 16, 768], BF16)
        facc = st.sb("mofacc", [128, 16, 768], F32)
        act_ = st.sb("moact", [128, 8, 768], BF16)
        wbc = st.sb("mowbc", [128, 768], F32)
        wgu = [st.sb(f"mowgu{i}", [128, 16, 2, 256], BF16) for i in range(3)]
        wd = [st.sb(f"mowd{i}", [128, 8, 512], BF16) for i in range(2)]
        sgt = [st.sb(f"mosg{i}", [128, 384], F32) for i in range(2)]
        tt_ = [st.sb(f"mott{i}", [128, 384], F32) for i in range(2)]
        xt = [st.sb(f"moxt{i}", [128, DM], F32) for i in range(2)]
        ps = [st.ps(f"mop{i}", [128, 512], F32) for i in range(8)]
        ug = 0
        ud = 0
        pi = 0
        for (p0, pn) in passes:
            chunks = [(c0, min(384, pn - c0)) for c0 in range(0, pn, 384)]
            P.dma("sp", h2[:, :, :pn], D.h2Td[:, p0:p0 + pn].re("(c p) t -> p c t", p=128))
            for e in range(16):
                for (c0, cn) in chunks:
                    p = ps[pi % 8]
                    pi += 1
                    P.mm(p[:, :cn], selE[:, e, :], wT[:, p0 + c0:p0 + c0 + cn])
                    P.copy(wbc[:, c0:c0 + cn], p[:, :cn], e="act")
                for s2_ in range(4):
                    w = wgu[ug % 3]
                    ug += 1
                    for gu in range(2):
                        P.dma("sp", w[:, :, gu, :],
                              D.wgu_bf[L][e][:, gu * 1024 + s2_ * 256:gu * 1024 + (s2_ + 1) * 256].re("(kc p) n -> p kc n", p=128))
                    for b2 in range(2):
                        blk = s2_ * 2 + b2
                        for ci, (c0, cn) in enumerate(chunks):
                            pg = ps[pi % 8]
                            pu = ps[(pi + 1) % 8]
                            pi += 2
                            P.mmg(pg[:, :cn], [(w[:, kc, 0, b2 * 128:(b2 + 1) * 128], h2[:, kc, c0:c0 + cn]) for kc in range(16)])
                            P.mmg(pu[:, :cn], [(w[:, kc, 1, b2 * 128:(b2 + 1) * 128], h2[:, kc, c0:c0 + cn]) for kc in range(16)])
                            sg_ = sgt[ci % 2]
                            t_ = tt_[ci % 2]
                            P.act(sg_[:, :cn], pg[:, :cn], AF.Silu)
                            P.tt(t_[:, :cn], pu[:, :cn], sg_[:, :cn], ALU.mult)
                            P.tt(act_[:, blk, c0:c0 + cn], t_[:, :cn], wbc[:, c0:c0 + cn], ALU.mult)
                for s4 in range(4):
                    w = wd[ud % 2]
                    ud += 1
                    P.dma("sp", w.v(), D.wd_bf[L][e][:, s4 * 512:(s4 + 1) * 512].re("(kc p) n -> p kc n", p=128))
                    for o4 in range(4):
                        ob = s4 * 4 + o4
                        for (c0, cn) in chunks:
                            p = ps[pi % 8]
                            pi += 1
                            P.mmg(p[:, :cn], [(w[:, kc, o4 * 128:(o4 + 1) * 128], act_[:, kc, c0:c0 + cn]) for kc in range(8)])
                            if e == 0:
                                P.copy(facc[:, ob, c0:c0 + cn], p[:, :cn], e="act")
                            else:
                                P.tt(facc[:, ob, c0:c0 + cn], facc[:, ob, c0:c0 + cn], p[:, :cn], ALU.add)
            for ob in range(16):
                lat = max(0, min(pn, T - p0))
                if lat > 0:
                    P.ts(facc[:, ob, 0:lat], facc[:, ob, 0:lat], g2c[0][:, ob:ob + 1], None, ALU.mult)
                if lat < pn:
                    P.ts(facc[:, ob, lat:pn], facc[:, ob, lat:pn], g2c[1][:, ob:ob + 1], None, ALU.mult)
            for ti in range(pn // 128):
                t = p0 // 128 + ti
                x = xt[ti % 2]
                P.dma("sp", x.v(), D.xcur[t * 128:(t + 1) * 128, :])
                for o4 in range(4):
                    p = ps[pi % 8]
                    pi += 1
                    for j in range(4):
                        ob = o4 * 4 + j
                        P.transpose(p[:, j * 128:(j + 1) * 128], facc[:, ob, ti * 128:(ti + 1) * 128], identf.v())
                    P.tt(x[:, o4 * 512:(o4 + 1) * 512], x[:, o4 * 512:(o4 + 1) * 512], p.v(), ALU.add)
                P.dma("sp", D.xcur[t * 128:(t + 1) * 128, :], x.v())


def stage_final(P, D):
    with Stage(P) as st:
        g = st.sb("fng", [128, DM], F32)
        P.dma("sp", g.v(), bc_row(D.final_norm_g.v().re("(o d) -> o d", o=1)))
        xt = [st.sb(f"fnx{i}", [128, DM], F32) for i in range(2)]
        sq = st.sb("fnsq", [128, DM], F32)
        ot = [st.sb(f"fno{i}", [128, DM], F32) for i in range(2)]
        ss = [st.sb(f"fns{i}", [128, 1], F32) for i in range(2)]
        for t in range(16):
            x = xt[t % 2]
            s = ss[t % 2]
            o = ot[t % 2]
            P.dma("sp", x.v(), D.xcur[t * 128:(t + 1) * 128, :])
            P.act(sq.v(), x.v(), AF.Square, accum=s.v())
            P.ts(s.v(), s.v(), 1.0 / DM, 1e-6, ALU.mult, ALU.add)
            P.act(s.v(), s.v(), AF.Sqrt)
            P.recip(s.v(), s.v())
            P.stt(o.v(), x.v(), s.v(), g.v(), ALU.mult, ALU.mult)
            P.dma("sp", D.out[t * 128:(t + 1) * 128, :], o.v())

ORDER = ["mod", "norm1", "win", "na", "mlaprep", "mla", "rw", "merge", "norm2", "moe"]


def build(L_list=(0, 1), upto=None, dbg=(), skip=(), rw_heads=range(16)):
    nc = bass.Bass("TRN2", target_bir_lowering=False)
    P = Prog(nc)
    D = NS()

    def inp(name, shape, dt=F32):
        b = dram(nc, name, shape, dt, kind="ExternalInput")
        setattr(D, name, b)
        return b

    inp("xin", [NT, DM]); inp("cT", [128, 16, 2])
    inp("w_mod", [2, DM, 6 * DM]); inp("b_mod", [2, 6 * DM])
    inp("norm1_g", [2, DM]); inp("norm2_g", [2, DM])
    inp("w_in", [2, DM, IN_W])
    inp("ident_f", [128, 128])
    inp("na_bias", [2, 16, 128, 5, 5, 128])
    inp("rope_c", [32, NT]); inp("rope_s", [32, NT]); inp("perm96", [96, 96])
    inp("mla_q_norm_g", [2, 512]); inp("mla_kv_norm_g", [2, 256])
    inp("mla_w_uq", [2, 512, 1536]); inp("mla_w_ukv", [2, 256, 2048])
    inp("tri_masks", [4, 128, 128])
    inp("rw_shift_mu", [2, 3488]); inp("rw_w0", [2, 2, 1024]); inp("rw_w_up", [2, 2, 64, 1024])
    inp("rw_a0", [2, 2, 1024]); inp("rw_a_up", [2, 2, 64, 1024]); inp("rw_g_up", [2, 160, 1024])
    inp("rw_k_k", [2, 1024]); inp("rw_k_a", [2, 1024]); inp("rw_r_k", [2, 16, 64])
    inp("rw_gn_w", [2, 1024]); inp("rw_gn_b", [2, 1024])
    inp("w_branch", [2, 3, 1024, DM]); inp("w_out", [2, DM, DM])
    inp("router_w", [DM, 16]); inp("router_bias", [16])
    inp("moe_w_gate_up", [2, 16, DM, 2048]); inp("moe_w_down", [2, 16, 1024, DM])
    inp("final_norm_g", [DM]); inp("selE", [16, 16, 128])
    D.out = dram(nc, "out", [T, DM], F32, kind="ExternalOutput")

    def scratch(name, shape, dt):
        kind = "ExternalOutput" if name in dbg else "Internal"
        b = dram(nc, name, shape, dt, kind=kind)
        setattr(D, name, b)
        return b

    scratch("xcur", [NT, DM], F32)
    D.modv = [scratch(f"modv{l}", [2, 6 * DM], F32) for l in range(2)]
    scratch("uT", [IN_WP, NT], BF16)
    scratch("utok", [NT, 1024], BF16)
    scratch("hTd", [DM, NT], BF16)
    D.ys = [scratch(f"ys{z}", [1024, NT], BF16) for z in range(3)]
    scratch("qmT", [16, 96, NT], BF16)
    scratch("kmT", [16, 96, NT], BF16)
    scratch("vmtok", [NT, 1024], BF16)
    scratch("yscan", [1024, NT], F32)
    scratch("h2Td", [DM, NT], BF16)
    D.wgu_bf = [[scratch(f"wgubf{l}_{e}", [DM, 2048], BF16) for e in range(16)] for l in range(2)]
    D.wd_bf = [[scratch(f"wdbf{l}_{e}", [1024, DM], BF16) for e in range(16)] for l in range(2)]
    D.wbr_bf = [[scratch(f"wbrbf{l}_{z}", [1024, DM], BF16) for z in range(3)] for l in range(2)]
    D.wout_bf = [scratch(f"woutbf{l}", [DM, DM], BF16) for l in range(2)]

    def emit_casts(L, h):
        for i in range(4):
            P.dma("pool", D.wgu_bf[L][h].part(i)[i * 512:(i + 1) * 512, :], D.moe_w_gate_up[L, h, i * 512:(i + 1) * 512, :], bg=True)
        for i in range(2):
            P.dma("pool", D.wd_bf[L][h].part(i)[i * 512:(i + 1) * 512, :], D.moe_w_down[L, h, i * 512:(i + 1) * 512, :], bg=True)
        if h < 3:
            for i in range(2):
                P.dma("pool", D.wbr_bf[L][h].part(i)[i * 512:(i + 1) * 512, :], D.w_branch[L, h, i * 512:(i + 1) * 512, :], bg=True)
        elif h < 7:
            i = h - 3
            P.dma("pool", D.wout_bf[L].part(i)[i * 512:(i + 1) * 512, :], D.w_out[L, i * 512:(i + 1) * 512, :], bg=True)
    scratch("wgtT", [16, NT], F32)

    def stop(name):
        return upto is not None and ORDER.index(name) >= ORDER.index(upto)

    with Stage(P) as st0:
        ident_bf = st0.sb("ident_bf", [128, 128], BF16)
        D.ident_bf = ident_bf
        P.dma("pool", ident_bf.v(), D.ident_f.v())
        for i in range(6):
            P.dma("sp", D.xcur[i * 384:(i + 1) * 384, :], D.xin[i * 384:(i + 1) * 384, :])
        for L in L_list:
            need_ctx = (L == 0)
            if "mod" not in skip:
                stage_mod(P, D, L)
                P.mark("stage_mod")
            if stop("mod"):
                break
            if "win" not in skip:
                with Stage(P) as stl:
                    hT = stl.sb("hT", [128, 16, NT], BF16)
                    stage_norm(P, D, L, 0, hT, stl)
                    P.mark("stage_norm")
                    if "hTd" in dbg:
                        P.dma("sp", D.hTd.v().re("(c p) t -> p c t", p=128), hT.v())
                    stage_win(P, D, L, hT)
                    P.mark("stage_win")
            if stop("win"):
                break
            if "na" not in skip:
                stage_na(P, D, L, need_ctx)
                P.mark("stage_na")
            if stop("na"):
                break
            if "mla" not in skip:
                stage_mla_prep(P, D, L, need_ctx)
                P.mark("stage_mla_prep")
                stage_mla_attn(P, D, L, need_ctx)
                P.mark("stage_mla_attn")
            if stop("mla"):
                break
            if "rw" not in skip:
                stage_rw(P, D, L, need_ctx, heads=rw_heads, dbgd=(D.yscan if "yscan" in dbg else None), on_head=(lambda h, L=L: emit_casts(L, h)))
                P.mark("stage_rw")
            if stop("rw"):
                break
            if "merge" not in skip:
                stage_merge(P, D, L, need_ctx)
                P.mark("stage_merge")
            if stop("merge"):
                break
            if "moe" not in skip:
                stage_norm2_router(P, D, L, need_ctx)
                P.mark("stage_norm2_router")
                stage_moe(P, D, L, need_ctx)
                P.mark("stage_moe")
            if stop("moe"):
                break
        else:
            stage_final(P, D)
            P.mark("stage_final")
        P.drain()
    print("ninst", P.ninst, {e: P.etot[e] for e in P.eng})
    build.marks = P.marks
    return nc


def host_consts():
    f32 = np.float32
    c = {}
    c["ident_f"] = np.eye(128, dtype=f32)
    nf = 8
    inv = (10000.0 ** (-np.arange(nf, dtype=np.float32) / nf)).astype(np.float32)
    pos = np.arange(T)
    row = (pos // 64).astype(np.float32)
    col = (pos % 64).astype(np.float32)
    rc = np.ones((32, NT), f32)
    rs = np.zeros((32, NT), f32)
    for d in range(32):
        p = row if d < 16 else col
        dd = d % 16
        f = dd % 8
        ang = (p * inv[f]).astype(np.float32)
        rc[d, :T] = np.cos(ang)
        rs[d, :T] = (-np.sin(ang)) if dd < 8 else np.sin(ang)
    c["rope_c"] = rc
    c["rope_s"] = rs
    pm = np.zeros((96, 96), f32)
    for d in range(32):
        dd = d % 16
        partner = (d + 8) if dd < 8 else (d - 8)
        pm[64 + partner, 64 + d] = 1.0
    c["perm96"] = pm
    i = np.arange(128)
    mu_ = (i[:, None] < i[None, :]).astype(f32)
    ml_ = (i[:, None] > i[None, :]).astype(f32)
    se = np.zeros((16, 16, 128), f32)
    for e in range(16):
        se[e, e, :] = 1.0
    c["selE"] = se
    c["tri_masks"] = np.stack([mu_, ml_, mu_ + np.eye(128, dtype=f32), ml_ + np.eye(128, dtype=f32)], 0)
    return c


def na_bias_table(rpb):
    Lr = rpb.shape[0]
    out = np.full((Lr, 16, 128, 5, 5, 128), -1e30, np.float32)
    jrep = [0, 1, 2, 14, 15]
    for pi, j in enumerate(jrep):
        kr0 = na_kr0(j)
        qtok = np.arange(128)
        qi = 2 * j + qtok // 64
        qj = qtok % 64
        r0 = np.clip(qi - 4, 0, 24)
        cstart = np.clip(qj - 8, 0, 48)
        for c in range(5):
            ktok = np.arange(128)
            ar = kr0 + 2 * c + ktok // 64
            kc = ktok % 64
            rv = (ar[:, None] >= r0[None, :]) & (ar[:, None] < r0[None, :] + 8)
            cv = (kc[:, None] >= cstart[None, :]) & (kc[:, None] < cstart[None, :] + 16)
            ridx = np.clip(ar[:, None] - qi[None, :] + 7, 0, 14)
            cidx = np.clip(kc[:, None] - qj[None, :] + 15, 0, 30)
            val = rpb[:, :, ridx, cidx]
            out[:, :, :, pi, c, :] = np.where((rv & cv)[None, None], val, np.float32(-1e30))
    return out


def make_inputs(inputs, b, consts, nab):
    f32 = np.float32
    x = np.asarray(inputs["x"][b], f32)
    ctx = np.asarray(inputs["ctx"][b], f32)
    d = dict(consts)
    d["xin"] = np.ascontiguousarray(np.concatenate([x, ctx], axis=0))
    cc = np.stack([np.asarray(inputs["c"][b], f32), np.asarray(inputs["c_ctx"], f32)], axis=0)
    d["cT"] = np.ascontiguousarray(cc.reshape(2, 16, 128).transpose(2, 1, 0))
    for k in ["w_mod", "b_mod", "norm1_g", "norm2_g", "w_in", "mla_q_norm_g", "mla_kv_norm_g", "mla_w_uq", "mla_w_ukv",
              "rw_shift_mu", "rw_w0", "rw_w_up", "rw_a0", "rw_a_up", "rw_g_up", "rw_k_k", "rw_k_a", "rw_r_k", "rw_gn_w", "rw_gn_b",
              "w_branch", "w_out", "router_w", "router_bias", "moe_w_gate_up", "moe_w_down", "final_norm_g"]:
        d[k] = np.asarray(inputs[k], f32)
    d["na_bias"] = nab
    return d


def kernel(**inputs):
    nc = build()
    consts = host_consts()
    nab = na_bias_table(np.asarray(inputs["na_rpb"], np.float32))
    in_maps = [make_inputs(inputs, b, consts, nab) for b in range(8)]
    res = run_bass_kernel_spmd(nc, in_maps, core_ids=list(range(8)))
    return np.stack([np.asarray(r["out"], np.float32) for r in res.results], axis=0)
```

```python
import numpy as np
import concourse.bass as bass
import concourse.mybir as mybir
from contextlib import ExitStack

F32 = mybir.dt.float32
BF16 = mybir.dt.bfloat16
AF = mybir.ActivationFunctionType
ALU = mybir.AluOpType
AX = mybir.AxisListType

SEM_ROT = 30000
N_DMA_SEMS = 40


class V:
    __slots__ = ("ap", "buf", "key")

    def __init__(self, ap, buf, key=None):
        self.ap = ap
        self.buf = buf
        self.key = key

    def __getitem__(self, idx):
        return V(self.ap[idx], self.buf, self.key)

    def m(self, fn):
        return V(fn(self.ap), self.buf, self.key)

    def re(self, s, **kw):
        return V(self.ap.rearrange(s, **kw), self.buf, self.key)

    def bc(self, dt):
        return V(self.ap.bitcast(dt), self.buf, self.key)

    def k(self, key):
        return V(self.ap, self.buf, key)


class Buf:
    def __init__(self, name, t):
        self.name = name
        self.t = t
        self.state = {None: [None, []]}
        self.psum = False
        self.bank_ev = None

    def ap(self):
        t = self.t
        return t.ap() if hasattr(t, "ap") and callable(getattr(t, "ap")) and not isinstance(t, bass.AP) else t

    def __getitem__(self, idx):
        return V(self.ap()[idx], self, None)

    def v(self):
        return V(self.ap(), self, None)

    def part(self, key):
        if key not in self.state:
            w, r = self.state[None]
            self.state[key] = [w, list(r)]
        return V(self.ap(), self, key)

    def keys_for(self, key):
        if key is None:
            return list(self.state.keys())
        if isinstance(key, tuple):
            out = []
            for k in key:
                out += self.keys_for(k)
            return out
        if key not in self.state:
            w, r = self.state[None]
            self.state[key] = [w, list(r)]
        return [key]


class Prog:
    def __init__(self, nc):
        self.nc = nc
        self.eng = {"pe": nc.tensor, "act": nc.scalar, "dve": nc.vector, "pool": nc.gpsimd, "sp": nc.sync}
        self.esem = {}
        self.ecnt = {}
        self.etot = {}
        for e in self.eng:
            self.esem[e] = nc.alloc_semaphore(f"es_{e}_0")
            self.ecnt[e] = 0
            self.etot[e] = 0
        self.waited = {e: {} for e in self.eng}
        self.dsems = [nc.alloc_semaphore(f"dma_{i}") for i in range(N_DMA_SEMS)]
        self.dcnt = [0] * N_DMA_SEMS
        self.dnext = 0
        self.bsems = [nc.alloc_semaphore(f"bgdma_{i}") for i in range(8)]
        self.bcnt = [0] * 8
        self.bnext = 0
        self.all_sems = {}
        self.ninst = 0
        self.pe_sems = {id(self.esem["pe"])}
        self.npe = 0
        self.marks = []

    def _wait(self, e, ev):
        if ev is None:
            return
        sem, val = ev
        sid = id(sem)
        self.all_sems[sid] = sem
        if self.waited[e].get(sid, 0) >= val:
            return
        self.eng[e].wait_ge(sem, val)
        self.waited[e][sid] = val

    def _wait2(self, e, ev):
        if ev is not None and e == "pe" and id(ev[0]) in self.pe_sems:
            return
        self._wait(e, ev)

    def _bank(self, e, vs):
        for v in vs:
            if v.buf.psum and v.buf.bank_ev is not None and v.buf.bank_ev[1] != e:
                self._wait(e, v.buf.bank_ev[0])

    def _deps(self, e, reads, writes):
        self._bank(e, list(reads) + list(writes))
        for v in reads:
            for k in v.buf.keys_for(v.key):
                self._wait2(e, v.buf.state[k][0])
        for v in writes:
            for k in v.buf.keys_for(v.key):
                st = v.buf.state[k]
                self._wait2(e, st[0])
                for ev in st[1]:
                    self._wait2(e, ev)

    def _record(self, ev, reads, writes, e=None):
        for v in list(reads) + list(writes):
            if v.buf.psum:
                v.buf.bank_ev = (ev, e)
        for v in reads:
            for k in v.buf.keys_for(v.key):
                v.buf.state[k][1].append(ev)
        for v in writes:
            for k in v.buf.keys_for(v.key):
                v.buf.state[k][0] = ev
                v.buf.state[k][1] = []

    def _newev(self, e, inst):
        if self.ecnt[e] >= SEM_ROT:
            self.esem[e] = self.nc.alloc_semaphore(f"es_{e}_{self.etot[e]}")
            self.ecnt[e] = 0
            if e == "pe":
                self.pe_sems.add(id(self.esem[e]))
        self.ecnt[e] += 1
        self.etot[e] += 1
        inst.then_inc(self.esem[e], 1)
        ev = (self.esem[e], self.ecnt[e])
        self.waited[e][id(self.esem[e])] = 0 if id(self.esem[e]) not in self.waited[e] else self.waited[e][id(self.esem[e])]
        self.all_sems[id(self.esem[e])] = self.esem[e]
        self.last_ev = getattr(self, "last_ev", {})
        self.last_ev[e] = ev
        return ev

    def mark(self, name):
        self.marks.append((name, self.npe))

    def op(self, e, fn, reads, writes, nosync_same=False):
        if e == "pe":
            self.npe += 1
        self._deps(e, reads, writes)
        inst = fn()
        ev = self._newev(e, inst)
        self._record(ev, reads, writes, e)
        self.ninst += 1
        return inst

    def group(self, e, fns, reads, writes):
        self._deps(e, reads, writes)
        if e == "pe":
            self.npe += len(fns)
        inst = None
        for fn in fns:
            inst = fn()
            self.ninst += 1
        ev = self._newev(e, inst)
        self._record(ev, reads, writes, e)
        return inst

    def dma(self, q, out, in_, bg=False, **kw):
        if bg:
            i = self.bnext
            self.bnext = (self.bnext + 1) % len(self.bsems)
            sem = self.bsems[i]
            cnt = self.bcnt
        else:
            i = self.dnext
            self.dnext = (self.dnext + 1) % N_DMA_SEMS
            sem = self.dsems[i]
            cnt = self.dcnt
        if cnt[i] > 0:
            self._wait(q, (sem, cnt[i]))
        self._deps(q, [in_], [out])
        inst = self.eng[q].dma_start(out=out.ap, in_=in_.ap, **kw)
        inst.then_inc(sem, 16)
        cnt[i] += 16
        ev = (sem, cnt[i])
        self.all_sems[id(sem)] = sem
        self._record(ev, [in_], [out])
        self.ninst += 1
        return ev

    def drain(self):
        evs = []
        for e in self.eng:
            if self.ecnt[e] > 0:
                evs.append((self.esem[e], self.ecnt[e]))
        for i in range(N_DMA_SEMS):
            if self.dcnt[i] > 0:
                evs.append((self.dsems[i], self.dcnt[i]))
        for i in range(len(self.bsems)):
            if self.bcnt[i] > 0:
                evs.append((self.bsems[i], self.bcnt[i]))
        for e in self.eng:
            for ev in evs:
                self._wait(e, ev)

    def mm(self, out, lhsT, rhs, start=True, stop=True):
        if lhsT.ap.dtype == F32:
            self.npe += 1
        return self.op("pe", lambda: self.nc.tensor.matmul(out.ap, lhsT.ap, rhs.ap, start=start, stop=stop),
                       [lhsT, rhs] + ([] if start else [out]), [out])

    def mmg(self, out, pairs):
        n = len(pairs)
        if pairs[0][0].ap.dtype == F32:
            self.npe += n
        fns = []
        reads = []
        for j, (l, r) in enumerate(pairs):
            fns.append((lambda l=l, r=r, j=j: self.nc.tensor.matmul(out.ap, l.ap, r.ap, start=(j == 0), stop=(j == n - 1))))
            reads += [l, r]
        return self.group("pe", fns, reads, [out])

    def transpose(self, out, in_, ident):
        return self.op("pe", lambda: self.nc.tensor.transpose(out.ap, in_.ap, ident.ap), [in_, ident], [out])

    def act(self, out, in_, func, bias=None, scale=None, accum=None, e="act"):
        kw = {}
        reads = [in_]
        if bias is not None:
            if isinstance(bias, V):
                kw["bias"] = bias.ap
                reads.append(bias)
            else:
                kw["bias"] = bias
        if scale is not None:
            if isinstance(scale, V):
                kw["scale"] = scale.ap
                reads.append(scale)
            else:
                kw["scale"] = scale
        writes = [out]
        if accum is not None:
            kw["accum_out"] = accum.ap
            writes.append(accum)
        return self.op("act", lambda: self.nc.scalar.activation(out.ap, in_.ap, func, **kw), reads, writes)

    def tt(self, out, a, b, op, e="dve"):
        return self.op(e, lambda: self.eng[e].tensor_tensor(out.ap, a.ap, b.ap, op), [a, b], [out])

    def ts(self, out, a, s1, s2, op0, op1=None, accum=None, e="dve"):
        reads = [a]
        s1a = s1
        s2a = s2
        if isinstance(s1, V):
            reads.append(s1)
            s1a = s1.ap
        if isinstance(s2, V):
            reads.append(s2)
            s2a = s2.ap
        writes = [out]
        kw = {}
        if op1 is not None:
            kw["op1"] = op1
        if accum is not None:
            kw["accum_out"] = accum.ap
            writes.append(accum)
        return self.op(e, lambda: self.eng[e].tensor_scalar(out.ap, a.ap, s1a, s2a, op0, **kw), reads, writes)

    def stt(self, out, a, s, b, op0, op1, accum=None):
        reads = [a, b]
        sa = s
        if isinstance(s, V):
            reads.append(s)
            sa = s.ap
        writes = [out]
        kw = {}
        if accum is not None:
            kw["accum_out"] = accum.ap
            writes.append(accum)
        return self.op("dve", lambda: self.nc.vector.scalar_tensor_tensor(out.ap, a.ap, sa, b.ap, op0, op1, **kw), reads, writes)

    def copy(self, out, in_, e="dve"):
        if e == "act":
            return self.op("act", lambda: self.nc.scalar.copy(out.ap, in_.ap), [in_], [out])
        return self.op(e, lambda: self.eng[e].tensor_copy(out.ap, in_.ap), [in_], [out])

    def memset(self, out, val, e="dve"):
        return self.op(e, lambda: self.eng[e].memset(out.ap, val), [], [out])

    def reduce(self, out, in_, op, axis=None, e="dve"):
        axis = axis or AX.X
        return self.op(e, lambda: self.eng[e].tensor_reduce(out.ap, in_.ap, axis, op), [in_], [out])

    def recip(self, out, in_):
        return self.op("dve", lambda: self.nc.vector.reciprocal(out.ap, in_.ap), [in_], [out])


class Stage:
    uid = 0

    def __init__(self, P):
        self.P = P
        self.es = ExitStack()

    def __enter__(self):
        self.es.__enter__()
        return self

    def sb(self, name, shape, dtype=F32):
        Stage.uid += 1
        name = f"{name}_s{Stage.uid}"
        t = self.es.enter_context(self.P.nc.sbuf_tensor(name, list(shape), dtype))
        return Buf(name, t)

    def ps(self, name, shape, dtype=F32):
        Stage.uid += 1
        name = f"{name}_p{Stage.uid}"
        t = self.es.enter_context(self.P.nc.psum_tensor(name, list(shape), dtype))
        b = Buf(name, t)
        b.psum = True
        return b

    def __exit__(self, *a):
        self.P.drain()
        return self.es.__exit__(*a)

from concourse.bass_utils import run_bass_kernel_spmd
import ml_dtypes
import math

DM = 2048
T = 2048
TC = 256
NT = T + TC
NTILE = NT // 128
IN_W = 13504
IN_WP = 13568
OFF_NA = 0
OFF_RW = 3072
OFF_CQ = 6560
OFF_CKV = 7072
OFF_KR = 7328
OFF_G = 7360
TOKCH = [(0, 512), (512, 512), (1024, 512), (1536, 512), (2048, 256)]


class NS:
    pass


def dram(nc, name, shape, dtype, kind="Internal"):
    return Buf(name, nc.dram_tensor(name, list(shape), dtype, kind=kind))


def stage_mod(P, D, L):
    with Stage(P) as st:
        cT = st.sb("cT", [128, 16, 2], F32)
        cs = st.sb("cs", [128, 16, 2], F32)
        P.dma("sp", cT.v(), D.cT.v())
        P.act(cs.v(), cT.v(), AF.Silu)
        wb = [st.sb(f"wm{i}", [128, 16, 512], F32) for i in range(3)]
        bt = [st.sb(f"bm{i}", [2, 512], F32) for i in range(2)]
        ot = [st.sb(f"om{i}", [2, 512], F32) for i in range(2)]
        ps = [st.ps(f"pm{i}", [2, 512], F32) for i in range(2)]
        for j in range(24):
            w = wb[j % 3]
            for q4 in range(4):
                P.dma(("sp" if q4 % 2 == 0 else "act"), w[:, q4 * 4:(q4 + 1) * 4, :],
                      D.w_mod[L, q4 * 512:(q4 + 1) * 512, j * 512:(j + 1) * 512].re("(kc p) n -> p kc n", p=128))
            P.dma("sp", bt[j % 2].v(), D.b_mod[L:L + 1, j * 512:(j + 1) * 512].m(lambda a: a.broadcast_to([2, 512])))
            P.mmg(ps[j % 2].v(), [(cs[:, kc, :], w[:, kc, :]) for kc in range(16)])
            P.tt(ot[j % 2].v(), ps[j % 2].v(), bt[j % 2].v(), ALU.add)
            P.dma("sp", D.modv[L][:, j * 512:(j + 1) * 512], ot[j % 2].v())


def bc_row(v, n=128):
    return v.m(lambda a: a.broadcast_to([n, a.shape[-1]]))


def stage_norm(P, D, L, which, hT, st_outer):
    gsrc = D.norm1_g if which == 0 else D.norm2_g
    sh_i, sc_i = (0, 1) if which == 0 else (3, 4)
    with Stage(P) as st:
        g = st.sb("ng", [128, DM], F32)
        P.dma("sp", g.v(), bc_row(gsrc[L:L + 1, :]))
        A = []
        Bs = []
        for r in range(2):
            a = st.sb(f"nA{r}", [128, DM], F32)
            b = st.sb(f"nB{r}", [128, DM], F32)
            P.dma("sp", a.v(), bc_row(D.modv[L][r:r + 1, sc_i * DM:(sc_i + 1) * DM]))
            P.dma("sp", b.v(), bc_row(D.modv[L][r:r + 1, sh_i * DM:(sh_i + 1) * DM]))
            P.stt(a.v(), a.v(), 1.0, g.v(), ALU.add, ALU.mult)
            A.append(a)
            Bs.append(b)
        xt = [st.sb(f"nx{i}", [128, DM], F32) for i in range(2)]
        sq = st.sb("nsq", [128, DM], F32)
        hb = [st.sb(f"nh{i}", [128, DM], BF16) for i in range(2)]
        ss = [st.sb(f"nss{i}", [128, 1], F32) for i in range(2)]
        pt = [st.ps(f"npt{i}", [128, 16, 128], BF16) for i in range(2)]
        for t in range(NTILE):
            r = 0 if t < 16 else 1
            x = xt[t % 2]
            s = ss[t % 2]
            h = hb[t % 2]
            p = pt[t % 2]
            P.dma("sp", x.v(), D.xcur[t * 128:(t + 1) * 128, :])
            P.act(sq.v(), x.v(), AF.Square, accum=s.v())
            P.ts(s.v(), s.v(), 1.0 / DM, 1e-6, ALU.mult, ALU.add)
            P.act(s.v(), s.v(), AF.Sqrt)
            P.recip(s.v(), s.v())
            P.stt(sq.v(), x.v(), s.v(), A[r].v(), ALU.mult, ALU.mult)
            P.tt(h.v(), sq.v(), Bs[r].v(), ALU.add)
            for c in range(16):
                P.transpose(p[:, c, :], h[:, c * 128:(c + 1) * 128], D.ident_bf.v())
            P.copy(hT[:, :, t * 128:(t + 1) * 128], p.v(), e=("act" if t % 2 else "dve"))


def stage_win(P, D, L, hT):
    with Stage(P) as st:
        wb = [st.sb(f"ww{i}", [128, 16, 512], BF16) for i in range(2)]
        ob = [st.sb(f"wo{i}", [128, NT], BF16) for i in range(2)]
        ps = [st.ps(f"wp{i}", [128, 512], F32) for i in range(4)]
        nslab = (IN_W + 511) // 512
        pi = 0
        oi = 0
        for s in range(nslab):
            c0 = s * 512
            cw = min(512, IN_W - c0)
            w = wb[s % 2]
            for q4 in range(4):
                P.dma("pool", w[:, q4 * 4:(q4 + 1) * 4, :cw],
                      D.w_in[L, q4 * 512:(q4 + 1) * 512, c0:c0 + cw].re("(kc p) n -> p kc n", p=128))
            tokmajor = (2048 <= c0 < 3072)
            if tokmajor:
                for t in range(NTILE):
                    p = ps[pi % 4]
                    pi += 1
                    P.mmg(p.v(), [(hT[:, kc, t * 128:(t + 1) * 128], w[:, kc, :]) for kc in range(16)])
                    o = ob[oi % 2]
                    oi += 1
                    P.copy(o[:, :512], p.v(), e=("act" if t % 2 else "dve"))
                    P.dma("sp", D.utok[t * 128:(t + 1) * 128, c0 - 2048:c0 - 2048 + 512], o[:, :512])
                continue
            for blk in range((cw + 127) // 128):
                m = min(128, cw - blk * 128)
                o = ob[oi % 2]
                oi += 1
                for ci, (t0, tn) in enumerate(TOKCH):
                    p = ps[pi % 4]
                    pi += 1
                    P.mmg(p[:m, :tn], [(w[:, kc, blk * 128:blk * 128 + m], hT[:, kc, t0:t0 + tn]) for kc in range(16)])
                    P.copy(o[:m, t0:t0 + tn], p[:m, :tn], e=("act" if ci % 2 else "dve"))
                r0 = c0 + blk * 128
                P.dma("sp", D.uT[r0:r0 + m, :], o[:m, :])


def na_kr0(j):
    return min(max(2 * j - 4, 0), 22)


def na_pat(j):
    return 0 if j == 0 else 1 if j == 1 else 2 if j <= 13 else 3 if j == 14 else 4


def stage_na(P, D, L, need_ctx):
    with Stage(P) as st:
        qT = [st.sb(f"naq{i}", [64, NT], BF16) for i in range(2)]
        kT = [st.sb(f"nak{i}", [64, NT], BF16) for i in range(2)]
        va = [st.sb(f"nav{i}", [128, NTILE, 65], BF16) for i in range(2)]
        bias = [st.sb(f"nab{i}", [128, 5, 5, 128], F32) for i in range(2)]
        eloc = [st.sb(f"nae{i}", [128, 5, 128], F32) for i in range(2)]
        pT = [st.sb(f"nap{i}", [128, 7, 128], BF16) for i in range(2)]
        rec = [st.sb(f"nar{i}", [128, 1], F32) for i in range(2)]
        ytok = st.sb("nay", [128, NTILE, 64], BF16)
        yT = [st.sb(f"nayT{i}", [64, NT], BF16) for i in range(2)]
        ps_s = [st.ps(f"nps{i}", [128, 8, 128], F32) for i in range(2)]
        ps_o = [st.ps(f"npo{i}", [128, 65], F32) for i in range(2)]
        ps_t = st.ps("npt", [64, 8, 128], BF16)
        for i in range(2):
            P.memset(va[i][:, :, 64:65], 1.0)
        ntq = NTILE if need_ctx else 16
        u = 0
        for h in range(16):
            b = h % 2
            P.dma("sp", qT[b].v(), D.uT[h * 64:(h + 1) * 64, :])
            P.dma("sp", kT[b].v(), D.uT[1024 + h * 64:1024 + (h + 1) * 64, :])
            P.dma("sp", va[b][:, :, 0:64], D.utok[:, h * 64:(h + 1) * 64].re("(c p) d -> p c d", p=128))
            P.dma("sp", bias[b].v(), D.na_bias[L, h])
            def na_tiles(j):
                if j < 16:
                    t0 = na_kr0(j) // 2
                    return [t0 + c for c in range(5)] + [16, 17]
                return [16, 17]

            def na_S(j, uu):
                s = ps_s[uu % 2]
                q = qT[b][:, j * 128:(j + 1) * 128]
                P.group("pe", [(lambda c=c, tt_=tt_: P.nc.tensor.matmul(s[:, c, :].ap, kT[b][:, tt_ * 128:(tt_ + 1) * 128].ap, q.ap, start=True, stop=True))
                               for c, tt_ in enumerate(na_tiles(j))], [kT[b].v(), qT[b].v()], [s.v()])
            def na_soft(j, uu):
                s = ps_s[uu % 2]
                e_ = eloc[uu % 2]
                p_ = pT[uu % 2]
                if j < 16:
                    P.stt(e_.v(), s[:, 0:5, :], 0.125, bias[b][:, na_pat(j), :, :], ALU.mult, ALU.add)
                    P.act(p_[:, 0:5, :], e_.v(), AF.Exp)
                    P.act(p_[:, 5:7, :], s[:, 5:7, :], AF.Exp, scale=0.125)
                else:
                    P.act(p_[:, 0:2, :], s[:, 0:2, :], AF.Exp, scale=0.125)
            na_S(0, u)
            na_soft(0, u)
            for j in range(ntq):
                o = ps_o[u % 2]
                p_ = pT[u % 2]
                r_ = rec[u % 2]
                if j + 1 < ntq:
                    na_S(j + 1, u + 1)
                    na_soft(j + 1, u + 1)
                u += 1
                tiles = na_tiles(j)
                P.mmg(o.v(), [(p_[:, c, :], va[b][:, tt_, :]) for c, tt_ in enumerate(tiles)])
                P.recip(r_.v(), o[:, 64:65])
                P.ts(ytok[:, j, :], o[:, 0:64], r_.v(), None, ALU.mult)
            for g0 in range(0, ntq, 8):
                gn = min(8, ntq - g0)
                for jj in range(gn):
                    P.transpose(ps_t[:, jj, :], ytok[:, g0 + jj, :], D.ident_bf.v())
                P.copy(yT[b][:, g0 * 128:(g0 + gn) * 128], ps_t[:, 0:gn, :], e="act")
            P.dma("sp", D.ys[0][h * 64:(h + 1) * 64, 0:ntq * 128], yT[b][:, 0:ntq * 128])


def stage_mla_prep(P, D, L, need_ctx):
    with Stage(P) as st:
        cq = st.sb("mcq", [128, 4, NT], BF16)
        ckv = st.sb("mckv", [128, 2, NT], BF16)
        kr = st.sb("mkr", [96, NT], BF16)
        krr = st.sb("mkrr", [96, NT], BF16)
        cs = st.sb("mcs", [96, NT], F32)
        sg = st.sb("msg", [96, NT], F32)
        perm = st.sb("mperm", [96, 96], BF16)
        ones = st.sb("mones", [128, 128], BF16)
        gq = st.sb("mgq", [128, 4], F32)
        gkv = st.sb("mgkv", [128, 2], F32)
        wq = st.sb("mwq", [128, 4, 1536], BF16)
        wkv = st.sb("mwkv", [128, 2, 2048], BF16)
        P.dma("sp", cq.v(), D.uT[OFF_CQ:OFF_CQ + 512, :].re("(c p) t -> p c t", p=128))
        P.dma("sp", ckv.v(), D.uT[OFF_CKV:OFF_CKV + 256, :].re("(c p) t -> p c t", p=128))
        P.memset(kr.v(), 0.0)
        P.dma("sp", kr[64:96, :], D.uT[OFF_KR:OFF_KR + 32, :])
        P.dma("sp", cs[64:96, :], D.rope_c.v())
        P.dma("sp", sg[64:96, :], D.rope_s.v())
        P.dma("pool", perm.v(), D.perm96.v())
        P.memset(ones.v(), 1.0)
        P.dma("sp", gq.v(), D.mla_q_norm_g[L].re("(c p) -> p c", p=128), allow_slow_non_contiguous=True)
        P.dma("sp", gkv.v(), D.mla_kv_norm_g[L].re("(c p) -> p c", p=128), allow_slow_non_contiguous=True)
        for kc in range(4):
            P.dma("pool", wq[:, kc, :], D.mla_w_uq[L, kc * 128:(kc + 1) * 128, :])
        for kc in range(2):
            P.dma("pool", wkv[:, kc, :], D.mla_w_ukv[L, kc * 128:(kc + 1) * 128, :])
        sq = st.sb("msq", [128, 4, 512], BF16)
        rs = st.sb("mrs", [128, 512], F32)
        ps = [st.ps(f"mps{i}", [128, 512], F32) for i in range(2)]
        psb = [st.ps(f"mpb{i}", [96, 512], F32) for i in range(2)]
        for (src, g, nblk) in ((cq, gq, 4), (ckv, gkv, 2)):
            for ci, (t0, tn) in enumerate(TOKCH):
                p = ps[ci % 2]
                P.act(sq[:, 0:nblk, :tn], src[:, :, t0:t0 + tn], AF.Square)
                P.mmg(p[:, :tn], [(ones.v(), sq[:, c, :tn]) for c in range(nblk)])
                P.ts(rs[:, :tn], p[:, :tn], 1.0 / (128 * nblk), 1e-6, ALU.mult, ALU.add)
                P.act(rs[:, :tn], rs[:, :tn], AF.Sqrt)
                P.recip(rs[:, :tn], rs[:, :tn])
                for c in range(nblk):
                    P.stt(src[:, c, t0:t0 + tn], src[:, c, t0:t0 + tn], g[:, c:c + 1], rs[:, :tn], ALU.mult, ALU.mult)
        tmp = st.sb("mtmp", [96, 512], F32)
        tmp2 = st.sb("mtmp2", [96, 512], F32)
        for ci, (t0, tn) in enumerate(TOKCH):
            p = psb[ci % 2]
            P.mm(p[:, :tn], perm.v(), kr[:, t0:t0 + tn])
            P.tt(tmp[64:96, :tn], p[64:96, :tn], sg[64:96, t0:t0 + tn], ALU.mult)
            P.tt(tmp2[64:96, :tn], kr[64:96, t0:t0 + tn], cs[64:96, t0:t0 + tn], ALU.mult)
            P.tt(krr[64:96, t0:t0 + tn], tmp[64:96, :tn], tmp2[64:96, :tn], ALU.add)
        for h in range(16):
            P.dma("sp", D.kmT[h, 64:96, :], krr[64:96, :])
        qh = [st.sb(f"mqh{i}", [96, NT], BF16) for i in range(2)]
        kh = [st.sb(f"mkh{i}", [64, NT], BF16) for i in range(2)]
        for h in range(16):
            q_ = qh[h % 2]
            k_ = kh[h % 2]
            for ci, (t0, tn) in enumerate(TOKCH):
                if t0 >= T and not need_ctx:
                    continue
                pa = ps[ci % 2]
                pb = psb[ci % 2]
                P.mmg(pa[:96, :tn], [(wq[:, kc, h * 96:(h + 1) * 96], cq[:, kc, t0:t0 + tn]) for kc in range(4)])
                P.copy(q_[:, t0:t0 + tn], pa[:96, :tn], e="act")
                P.mm(pb[:, :tn], perm.v(), q_[:, t0:t0 + tn])
                P.tt(tmp[64:96, :tn], pb[64:96, :tn], sg[64:96, t0:t0 + tn], ALU.mult)
                P.tt(tmp2[64:96, :tn], pa[64:96, :tn], cs[64:96, t0:t0 + tn], ALU.mult)
                P.tt(q_[64:96, t0:t0 + tn], tmp[64:96, :tn], tmp2[64:96, :tn], ALU.add)
            nq = NT if need_ctx else T
            P.dma("sp", D.qmT[h, :, 0:nq], q_[:, 0:nq])
            for ci, (t0, tn) in enumerate(TOKCH):
                pa = ps[ci % 2]
                P.mmg(pa[:64, :tn], [(wkv[:, kc, h * 128:h * 128 + 64], ckv[:, kc, t0:t0 + tn]) for kc in range(2)])
                P.copy(k_[:, t0:t0 + tn], pa[:64, :tn], e="act")
            P.dma("sp", D.kmT[h, 0:64, :], k_.v())
        vo = [st.sb(f"mvo{i}", [128, 1024], BF16) for i in range(2)]
        wv = wkv.v().re("p kc (h two d) -> p kc h two d", two=2, d=64)
        for t in range(NTILE):
            o = vo[t % 2]
            for half in range(2):
                pa = ps[half]
                P.mmg(pa.v().re("p (h d) -> p h d", d=64),
                      [(ckv[:, kc, t * 128:(t + 1) * 128], wv[:, kc, half * 8:(half + 1) * 8, 1, :]) for kc in range(2)])
                P.copy(o[:, half * 512:(half + 1) * 512], pa.v(), e=("act" if half else "dve"))
            P.dma("sp", D.vmtok[t * 128:(t + 1) * 128, :], o.v())


def stage_mla_attn(P, D, L, need_ctx):
    sc = 1.0 / math.sqrt(96.0)
    with Stage(P) as st:
        qT = [st.sb(f"aq{i}", [96, NT], BF16) for i in range(2)]
        kT = [st.sb(f"ak{i}", [96, NT], BF16) for i in range(2)]
        va = [st.sb(f"av{i}", [128, NTILE, 65], BF16) for i in range(2)]
        pT = [st.sb(f"ap{i}", [128, 512], BF16) for i in range(3)]
        rec = [st.sb(f"ar{i}", [128, 1], F32) for i in range(4)]
        ytok = st.sb("ay", [128, NTILE, 64], BF16)
        yT = [st.sb(f"ayT{i}", [64, NT], BF16) for i in range(2)]
        ps_s = [st.ps(f"aps{i}", [128, 512], F32) for i in range(2)]
        ps_o = [st.ps(f"apo{i}", [128, 65], F32) for i in range(4)]
        ps_t = st.ps("apt", [64, 8, 128], BF16)
        for i in range(2):
            P.memset(va[i][:, :, 64:65], 1.0)
        ntq = NTILE if need_ctx else 16
        u = 0
        for h in range(16):
            b = h % 2
            nq = NT if need_ctx else T
            P.dma("sp", qT[b][:, 0:nq], D.qmT[h, :, 0:nq])
            P.dma("sp", kT[b].v(), D.kmT[h])
            P.dma("sp", va[b][:, :, 0:64], D.vmtok[:, h * 64:(h + 1) * 64].re("(c p) d -> p c d", p=128))
            blocks = [(0, 512, list(range(18))), (512, 512, list(range(18))), (1024, 512, list(range(18))), (1536, 512, list(range(18)))]
            if need_ctx:
                blocks.append((2048, 256, [16, 17]))
            items = []
            for (q0, qn, tiles) in blocks:
                for ci, tt_ in enumerate(tiles):
                    items.append((q0, qn, tt_, ci, len(tiles)))

            def mla_S(it, uu):
                q0, qn, tt_, ci, nt_ = it
                P.mm(ps_s[uu % 2][:, :qn], kT[b][:, tt_ * 128:(tt_ + 1) * 128], qT[b][:, q0:q0 + qn])
            mla_S(items[0], u)
            for ii, it in enumerate(items):
                q0, qn, tt_, ci, nt_ = it
                nqi = qn // 128
                s = ps_s[u % 2]
                p_ = pT[u % 3]
                if ii + 1 < len(items):
                    mla_S(items[ii + 1], u + 1)
                u += 1
                P.act(p_[:, :qn], s[:, :qn], AF.Exp, scale=sc)
                for qi in range(nqi):
                    P.mm(ps_o[qi].v(), p_[:, qi * 128:(qi + 1) * 128], va[b][:, tt_, :], start=(ci == 0), stop=(ci == nt_ - 1))
                if ci == nt_ - 1:
                    for qi in range(nqi):
                        P.recip(rec[qi].v(), ps_o[qi][:, 64:65])
                    for qi in range(nqi):
                        P.ts(ytok[:, q0 // 128 + qi, :], ps_o[qi][:, 0:64], rec[qi].v(), None, ALU.mult)
            for g0 in range(0, ntq, 8):
                gn = min(8, ntq - g0)
                for jj in range(gn):
                    P.transpose(ps_t[:, jj, :], ytok[:, g0 + jj, :], D.ident_bf.v())
                P.copy(yT[b][:, g0 * 128:(g0 + gn) * 128], ps_t[:, 0:gn, :], e="dve")
            P.dma("sp", D.ys[2][h * 64:(h + 1) * 64, 0:ntq * 128], yT[b][:, 0:ntq * 128])


def ek(a, b):
    return tuple(f"e{i}" for i in range(a, b))


def rw_shift(P, X, om, hm, out, tmp, tmp2, n_):
    for (a, n) in ((0, T), (T, TC)):
        P.tt(tmp[:n_, a + 1:a + n - 1], X[:n_, a:a + n - 2], X[:n_, a + 2:a + n], ALU.add, e="pool")
        P.copy(tmp[:n_, a:a + 1], X[:n_, a + 1:a + 2], e="pool")
        P.copy(tmp[:n_, a + n - 1:a + n], X[:n_, a + n - 2:a + n - 1], e="pool")
    P.ts(tmp2[:n_], X[:n_], om, None, ALU.mult)
    P.stt(out[:n_], tmp[:n_], hm, tmp2[:n_], ALU.mult, ALU.add)


def stage_rw(P, D, L, need_ctx, heads=range(16), dbgd=None, on_head=None):
    C0 = math.exp(-0.5)
    nc = P.nc
    NS_ = 4
    with Stage(P) as st:
        masks = [st.sb(f"rwm{i}", [128, 128], F32) for i in range(4)]
        for i in range(4):
            P.dma("sp", masks[i].v(), D.tri_masks[i])
        identf = st.sb("rwid", [128, 128], F32)
        P.dma("sp", identf.v(), D.ident_f.v())
        ones_f = st.sb("rwof", [64, 64], F32)
        P.memset(ones_f.v(), 1.0)
        rmask = st.sb("rwrm", [64, NTILE, 128], BF16)
        P.memset(rmask.v(), 1.0)
        P.memset(rmask[:, :, 0:1], 0.0)

        def col16(name, src):
            t = st.sb(name, [64, 16], F32)
            P.dma("sp", t.v(), src.re("(h p) -> p h", p=64), allow_slow_non_contiguous=True)
            return t

        def col1(name, src, n):
            t = st.sb(name, [n, 1], F32)
            P.dma("sp", t.v(), src.re("(p o) -> p o", o=1), allow_slow_non_contiguous=True)
            return t

        kk_c = col16("ckk", D.rw_k_k[L])
        ka_c = col16("cka", D.rw_k_a[L])
        rk_c = col16("crk", D.rw_r_k[L].re("h d -> (h d)"))
        gnw_c = col16("cgw", D.rw_gn_w[L])
        gnb_c = col16("cgb", D.rw_gn_b[L])
        w0_c = [col16(f"cw0{z}", D.rw_w0[L, z]) for z in range(2)]
        a0_c = [col16(f"ca0{z}", D.rw_a0[L, z]) for z in range(2)]
        omka = st.sb("comka", [64, 16], F32)
        P.ts(omka.v(), ka_c.v(), -1.0, 1.0, ALU.mult, ALU.add)
        mu = D.rw_shift_mu[L]
        mus = {}
        for nm, src, kind in (("r", mu[0:1024], 16), ("k", mu[1024:2048], 16), ("v", mu[2048:3072], 16),
                              ("wd", mu[3072:3200], 128), ("ad", mu[3200:3328], 128), ("gd", mu[3328:3456], 128), ("gd2", mu[3456:3488], 32)):
            m_ = col16("mu" + nm, src) if kind == 16 else col1("mu" + nm, src, kind)
            shp = [64, 16] if kind == 16 else [kind, 1]
            om = st.sb("om" + nm, shp, F32)
            hm = st.sb("hm" + nm, shp, F32)
            P.ts(om.v(), m_.v(), -1.0, 1.0, ALU.mult, ALU.add)
            P.ts(hm.v(), m_.v(), 0.5, None, ALU.mult)
            mus[nm] = (om, hm)

        wup = st.sb("rwwup", [128, 1024], BF16)
        aup = st.sb("rwaup", [128, 1024], BF16)
        gup = st.sb("rwgup", [128, 1024], BF16)
        gup2 = st.sb("rwgup2", [32, 1024], BF16)
        P.dma("pool", wup.v(), D.rw_w_up[L].re("z l c -> (z l) c"))
        P.dma("pool", aup.v(), D.rw_a_up[L].re("z l c -> (z l) c"))
        P.dma("pool", gup.v(), D.rw_g_up[L, 0:128, :])
        P.dma("pool", gup2.v(), D.rw_g_up[L, 128:160, :])

        tA = st.sb("rwtA", [128, NT], BF16)
        tB = st.sb("rwtB", [128, NT], BF16)
        xin = st.sb("rwxin", [128, NT], BF16)
        twd = st.sb("rwtwd", [128, NT], BF16)
        ads = st.sb("rwads", [128, NT], BF16)
        sgd = st.sb("rwsgd", [128, NT], BF16)
        sgd2 = st.sb("rwsgd2", [32, NT], BF16)
        base = OFF_RW
        for (dst, r0, n_, key, fn) in ((twd, 3072, 128, "wd", AF.Tanh), (ads, 3200, 128, "ad", AF.Copy),
                                       (sgd, 3328, 128, "gd", AF.Sigmoid), (sgd2, 3456, 32, "gd2", AF.Sigmoid)):
            P.dma("sp", xin[:n_, :], D.uT[base + r0:base + r0 + n_, :])
            rw_shift(P, xin, mus[key][0].v(), mus[key][1].v(), tA, tA, tB, n_)
            P.act(dst[:n_, :], tA[:n_, :], fn)

        bk = [st.ps(f"rwbk{i}", [128, 512], F32) for i in range(8)]
        r_s = st.sb("rwr", [64, NT], BF16)
        k_s = st.sb("rwk", [64, NT], BF16)
        v_s = st.sb("rwv", [64, NT], BF16)
        g_s = st.sb("rwg", [64, NT], BF16)
        kk = st.sb("rwkk", [64, NT], F32)
        f_sg = st.sb("rwsg", [64, NT], F32)
        f_a = st.sb("rwa", [64, NT], F32)
        f_t = st.sb("rwt", [64, NT], F32)
        f_L = st.sb("rwL", [64, NTILE, 128], F32)
        f_E = st.sb("rwE", [64, NTILE, 128], F32)
        l63 = st.sb("rwl63", [64, NTILE, 1], F32)
        eA = st.sb("rweA", [64, NTILE, 1], F32)
        eB = st.sb("rweB", [64, NTILE, 1], F32)
        scl = st.sb("rwscl", [64, NTILE], F32)
        rT = st.sb("rwrT", [64, NT], BF16)
        kapT = st.sb("rwkapT", [64, NT], BF16)
        ktT = st.sb("rwktT", [64, NT], BF16)
        bT = st.sb("rwbT", [64, NT], BF16)
        tokz = st.sb("rwtokz", [128, NTILE, 3, 64], BF16)
        vtok = st.sb("rwvtok", [128, NTILE, 64], BF16)
        MA = st.sb("rwMA", [64, NTILE, 192], BF16)
        MB = st.sb("rwMB", [128, NTILE, 192], BF16)
        yacc = st.sb("rwy", [64, NT], F32)
        Nsb = [st.sb(f"rwN{i}", [128, 128], F32) for i in range(NS_)]
        NTsb = [st.sb(f"rwNT{i}", [128, 128], F32) for i in range(NS_)]
        PP = [[st.sb(f"rwPP{i}_{j}", [128, 2, 128], F32) for j in range(2)] for i in range(NS_)]
        ZZ = [[st.sb(f"rwZ{i}_{j}", [128, 192], F32) for j in range(2)] for i in range(NS_)]
        rc = [st.sb(f"rwrc{i}", [128, 192], F32) for i in range(NS_)]
        akr = [st.sb(f"rwakr{i}", [128, 128], F32) for i in range(NS_)]
        ST = [st.sb(f"rwST{i}", [64, 64], BF16) for i in range(2)]
        idb = D.ident_bf

        for h in heads:
            hc = slice(h, h + 1)
            if on_head is not None:
                on_head(h)
            for (dst, r0, key) in ((r_s, 0, "r"), (k_s, 1024, "k"), (v_s, 2048, "v")):
                P.dma("sp", xin[:64, :], D.uT[base + r0 + h * 64:base + r0 + (h + 1) * 64, :])
                rw_shift(P, xin, mus[key][0][:, hc], mus[key][1][:, hc], dst, tA, tB, 64)
            for ci, (t0, tn) in enumerate(TOKCH):
                ps = bk[ci % 8]
                P.mmg(ps[:64, :tn], [(gup[:, h * 64:(h + 1) * 64], sgd[:, t0:t0 + tn]), (gup2[:32, h * 64:(h + 1) * 64], sgd2[:32, t0:t0 + tn])])
                P.copy(g_s[:, t0:t0 + tn], ps[:64, :tn], e="act")
            P.ts(f_t.v(), k_s.v(), kk_c[:, hc], None, ALU.mult)
            P.act(f_a.v(), f_t.v(), AF.Square)
            for ci, (t0, tn) in enumerate(TOKCH):
                ps = bk[(ci + 5) % 8]
                P.mm(ps[:64, :tn], ones_f.v(), f_a[:, t0:t0 + tn])
                P.act(f_sg[:, t0:t0 + tn], ps[:64, :tn], AF.Sqrt)
            P.ts(f_sg.v(), f_sg.v(), 1e-12, None, ALU.max)
            P.recip(f_sg.v(), f_sg.v())
            P.tt(kk.v(), f_t.v(), f_sg.v(), ALU.mult)
            for c in range(NTILE):
                pb = bk[c % 8].v().bc(BF16)
                P.transpose(pb[:, 0:64], v_s[:, c * 128:(c + 1) * 128], idb[0:64, 0:64])
                P.copy(vtok[:, c, :], pb[:, 0:64], e="act")

            for z in range(2):
                zs = slice(z * 64, (z + 1) * 64)
                mS, mST, mD = (masks[0], masks[1], masks[2]) if z == 0 else (masks[1], masks[0], masks[3])
                for ci, (t0, tn) in enumerate(TOKCH):
                    ps = bk[ci % 8]
                    P.mm(ps[:64, :tn], wup[zs, h * 64:(h + 1) * 64], twd[zs, t0:t0 + tn])
                    P.act(f_sg[:, t0:t0 + tn], ps[:64, :tn], AF.Sigmoid, bias=w0_c[z][:, hc])
                    ps2 = bk[(ci + 4) % 8]
                    P.mm(ps2[:64, :tn], aup[zs, h * 64:(h + 1) * 64], ads[zs, t0:t0 + tn])
                    P.act(f_a[:, t0:t0 + tn], ps2[:64, :tn], AF.Sigmoid, bias=a0_c[z][:, hc])
                P.ts(f_t.v(), f_a.v(), ka_c[:, hc], omka[:, hc], ALU.mult, ALU.add)
                P.tt(f_t.v(), f_t.v(), k_s.v(), ALU.mult)
                P.tt(f_a.v(), f_a.v(), kk.v(), ALU.mult)
                L2 = f_L.v().re("p c t -> p (c t)")
                P.op("dve", lambda: nc.vector.tensor_tensor_scan(L2.ap, rmask.v().re("p c t -> p (c t)").ap, f_sg.ap() , 0.0, ALU.mult, ALU.add),
                     [rmask.v(), f_sg.v()], [f_L.v()])
                P.copy(l63.v(), f_L[:, :, 63:64])
                P.tt(f_L.v(), f_L.v(), l63.v().m(lambda a: a.broadcast_to([64, NTILE, 128])), ALU.subtract)
                P.tt(f_sg.v(), L2, f_sg.v(), ALU.subtract)
                E2d = f_E.v().re("p c t -> p (c t)")
                P.act(E2d, L2, AF.Exp, scale=-C0)
                P.copy(eA.v(), f_E[:, :, 127:128])
                if z == 0:
                    P.tt(rT.v(), r_s.v(), E2d, ALU.mult)
                P.act(E2d, L2, AF.Exp, scale=C0)
                if z == 0:
                    P.tt(ktT.v(), f_t.v(), E2d, ALU.mult)
                    P.tt(bT.v(), f_a.v(), E2d, ALU.mult)
                else:
                    P.tt(kapT.v(), kk.v(), E2d, ALU.mult)
                P.act(E2d, f_sg.v(), AF.Exp, scale=-C0)
                if z == 0:
                    P.tt(kapT.v(), kk.v(), E2d, ALU.mult)
                else:
                    P.tt(ktT.v(), f_t.v(), E2d, ALU.mult)
                    P.tt(bT.v(), f_a.v(), E2d, ALU.mult)
                P.act(E2d, f_sg.v(), AF.Exp, scale=C0)
                P.copy(eB.v(), f_E[:, :, 0:1])
                if z == 1:
                    P.tt(rT.v(), r_s.v(), E2d, ALU.mult)
                eA2 = eA.v().re("p c o -> p (c o)")
                eB2 = eB.v().re("p c o -> p (c o)")
                P.memset(scl.v(), 1.0)
                if z == 0:
                    P.tt(scl[:, 0:15], eA2[:, 0:15], eB2[:, 1:16], ALU.mult)
                    P.tt(scl[:, 16:17], eA2[:, 16:17], eB2[:, 17:18], ALU.mult)
                    P.tt(scl[:, 17:18], eA2[:, 17:18], eB2[:, 0:1], ALU.mult)
                else:
                    P.tt(scl[:, 1:18], eB2[:, 1:18], eA2[:, 0:17], ALU.mult)
                for c in range(NTILE):
                    cs_ = slice(c * 128, (c + 1) * 128)
                    pb = bk[c % 8].v().bc(BF16)
                    P.transpose(pb[:, 0:64], kapT[:, cs_], idb[0:64, 0:64])
                    P.transpose(pb[:, 64:128], ktT[:, cs_], idb[0:64, 0:64])
                    P.transpose(pb[:, 128:192], bT[:, cs_], idb[0:64, 0:64])
                    P.copy(tokz[:, c, :, :], pb[:, 0:192].re("p (a d) -> p a d", a=3), e=("act" if c % 2 else "dve"))
                for c0 in range(0, NTILE, NS_):
                    units = list(range(c0, min(NTILE, c0 + NS_)))
                    for s, c in enumerate(units):
                        cs_ = slice(c * 128, (c + 1) * 128)
                        b = bk[s]
                        P.mm(b[:, 0:128].k(ek(0, 2)), bT[:, cs_], kapT[:, cs_])
                        P.mm(b[:, 128:256].k(ek(2, 4)), kapT[:, cs_], bT[:, cs_])
                        P.mm(b[:, 256:384].k(ek(4, 6)), kapT[:, cs_], ktT[:, cs_])
                        P.mm(b[:, 384:512].k(ek(6, 8)), bT[:, cs_], rT[:, cs_])
                        P.tt(Nsb[s].v(), b[:, 0:128].k(ek(0, 2)), mS.v(), ALU.mult)
                        P.tt(NTsb[s].v(), b[:, 128:256].k(ek(2, 4)), mST.v(), ALU.mult)
                        P.tt(ZZ[s][0][:, 64:192], b[:, 256:384].k(ek(4, 6)), mST.v(), ALU.mult)
                        P.tt(rc[s][:, 0:128], b[:, 384:512].k(ek(6, 8)), mD.v(), ALU.mult)
                        P.copy(ZZ[s][0][:, 0:64], tokz[:, c, 0, :], e="pool")
                        P.copy(rc[s][:, 128:192], tokz[:, c, 2, :], e="pool")
                    for s, c in enumerate(units):
                        b = bk[s]
                        P.mm(b[:, 256:448].k(ek(4, 7)), Nsb[s].v(), ZZ[s][0].v())
                        P.mm(b[:, 0:128].k(ek(0, 2)), NTsb[s].v(), Nsb[s].v())
                        P.mm(b[:, 128:256].k(ek(2, 4)), Nsb[s].v(), NTsb[s].v())
                        P.tt(ZZ[s][1].v(), ZZ[s][0].v(), b[:, 256:448].k(ek(4, 7)), ALU.subtract)
                        P.copy(PP[s][0].v(), b[:, 0:256].k(ek(0, 4)), e="act")
                    for j in range(1, 7):
                        for s, c in enumerate(units):
                            b = bk[s]
                            pc = PP[s][(j - 1) % 2]
                            pn = PP[s][j % 2]
                            zc = ZZ[s][j % 2]
                            zn = ZZ[s][(j + 1) % 2]
                            P.mm(b[:, 256:448].k(ek(4, 7)), pc[:, 0, :], zc.v())
                            if j < 6:
                                P.mm(b[:, 0:128].k(ek(0, 2)), pc[:, 1, :], pc[:, 0, :])
                                if j < 5:
                                    P.mm(b[:, 128:256].k(ek(2, 4)), pc[:, 0, :], pc[:, 1, :])
                            P.tt(zn.v(), zc.v(), b[:, 256:448].k(ek(4, 7)), ALU.add)
                            if j < 5:
                                P.copy(pn.v(), b[:, 0:256].k(ek(0, 4)), e="act")
                            elif j == 5:
                                P.copy(pn[:, 0, :], b[:, 0:128].k(ek(0, 2)), e="act")
                    for s, c in enumerate(units):
                        cs_ = slice(c * 128, (c + 1) * 128)
                        b = bk[s]
                        zf = ZZ[s][1]
                        P.mm(b[:64, 0:192].k(ek(0, 3)), zf[:, 0:64], rc[s].v())
                        P.mm(b[:, 192:384].k(ek(3, 6)), zf[:, 64:192], rc[s].v())
                        P.mm(b[:, 384:512].k(ek(6, 8)), ktT[:, cs_], rT[:, cs_])
                        P.tt(akr[s].v(), b[:, 384:512].k(ek(6, 8)), mD.v(), ALU.mult)
                        P.tt(MA[:, c, 0:128], rT[:, cs_], b[:64, 0:128].k(ek(0, 2)), ALU.subtract)
                        P.tt(MA[:, c, 128:192], identf[0:64, 0:64], b[:64, 128:192].k(ek(2, 3)), ALU.subtract)
                        P.tt(MB[:, c, 0:128], akr[s].v(), b[:, 192:320].k(ek(3, 5)), ALU.subtract)
                        P.tt(MB[:, c, 128:192], tokz[:, c, 1, :], b[:, 320:384].k(ek(5, 6)), ALU.subtract)
                order = ([16, 17] + list(range(16))) if z == 0 else ([17, 16] + list(range(15, -1, -1)))
                P.memset(ST[0].v(), 0.0)
                for i, c in enumerate(order):
                    cur = ST[i % 2]
                    nxt = ST[(i + 1) % 2]
                    bY = bk[(2 * i) % 8]
                    bS = bk[(2 * i + 1) % 8]
                    P.mmg(bY[:64, 0:128].k(ek(0, 2)), [(cur.v(), MA[:, c, 0:128]), (vtok[:, c, :], MB[:, c, 0:128])])
                    P.mmg(bS[:64, 0:64].k(ek(0, 1)), [(MA[:, c, 128:192], cur.v()), (MB[:, c, 128:192], vtok[:, c, :])])
                    if i < NTILE - 1:
                        P.ts(nxt.v(), bS[:64, 0:64].k(ek(0, 1)), scl[:, c:c + 1], None, ALU.mult)
                    ysl = yacc[:, c * 128:(c + 1) * 128]
                    if z == 0:
                        P.copy(ysl, bY[:64, 0:128].k(ek(0, 2)), e="act")
                    else:
                        P.tt(ysl, ysl, bY[:64, 0:128].k(ek(0, 2)), ALU.add)
            if dbgd is not None:
                P.dma("sp", dbgd[h * 64:(h + 1) * 64, :], yacc.v())
            E2o = f_E.v().re("p c t -> p (c t)")
            yo = rT
            L2o = f_L.v().re("p c t -> p (c t)")
            for ci, (t0, tn) in enumerate(TOKCH):
                ts_ = slice(t0, t0 + tn)
                b1, b2, b3 = bk[(3 * ci) % 8], bk[(3 * ci + 1) % 8], bk[(3 * ci + 2) % 8]
                P.act(L2o[:, 0:tn], yacc[:, ts_], AF.Square)
                P.mm(b1[:64, :tn], ones_f.v(), yacc[:, ts_])
                P.mm(b2[:64, :tn], ones_f.v(), L2o[:, 0:tn])
                P.stt(L2o[:, 512:512 + tn], r_s[:, ts_], rk_c[:, hc], k_s[:, ts_], ALU.mult, ALU.mult)
                P.mm(b3[:64, :tn], ones_f.v(), L2o[:, 512:512 + tn])
                mean = E2o[:, 0:tn]
                var = E2o[:, 512:512 + tn]
                dd = E2o[:, 1024:1024 + tn]
                P.ts(mean, b1[:64, :tn], 1.0 / 64, None, ALU.mult)
                P.tt(var, mean, mean, ALU.mult)
                P.stt(var, b2[:64, :tn], 1.0 / 64, var, ALU.mult, ALU.subtract)
                P.ts(var, var, 64e-5, None, ALU.add)
                P.act(var, var, AF.Sqrt)
                P.recip(var, var)
                P.tt(dd, yacc[:, ts_], mean, ALU.subtract)
                P.tt(dd, dd, var, ALU.mult)
                P.ts(dd, dd, gnw_c[:, hc], gnb_c[:, hc], ALU.mult, ALU.add)
                P.tt(var, b3[:64, :tn], v_s[:, ts_], ALU.mult)
                P.tt(dd, dd, var, ALU.add)
                P.tt(yo[:, ts_], dd, g_s[:, ts_], ALU.mult)
            P.dma("sp", D.ys[1][h * 64:(h + 1) * 64, :], yo.v())

def stage_merge(P, D, L, need_ctx):
    chunks = TOKCH if need_ctx else TOKCH[:4]
    ntile = NTILE if need_ctx else 16
    with Stage(P) as st:
        mT = st.sb("mgT", [128, 16, NT], BF16)
        with Stage(P) as sa:
            Y = [sa.sb(f"mgY{i}", [128, 24, 512], BF16) for i in range(2)]
            wb = [sa.sb(f"mgw{i}", [128, 3, 8, 128], BF16) for i in range(2)]
            gt = [sa.sb(f"mgg{i}", [128, 3, 512], BF16) for i in range(2)]
            sg = [sa.sb(f"mgs{i}", [128, 3, 512], F32) for i in range(2)]
            t0_ = sa.sb("mgt0", [128, 512], F32)
            t1_ = sa.sb("mgt1", [128, 512], F32)
            ps = [sa.ps(f"mgp{i}", [128, 512], F32) for i in range(6)]
            u = 0
            for ci, (t0, tn) in enumerate(chunks):
                y = Y[ci % 2]
                for z in range(3):
                    P.dma("sp", y[:, z * 8:(z + 1) * 8, :tn], D.ys[z][:, t0:t0 + tn].re("(kc p) t -> p kc t", p=128))
                for ob in range(16):
                    w = wb[u % 2]
                    g = gt[u % 2]
                    s_ = sg[u % 2]
                    pp = ps[(u % 2) * 3:(u % 2) * 3 + 3]
                    u += 1
                    for z in range(3):
                        P.dma("sp", w[:, z, :, :], D.wbr_bf[L][z][:, ob * 128:(ob + 1) * 128].re("(kc p) n -> p kc n", p=128))
                        r0 = OFF_G + z * DM + ob * 128
                        P.dma("sp", g[:, z, :tn], D.uT[r0:r0 + 128, t0:t0 + tn])
                    P.act(s_[:, :, :tn], g[:, :, :tn], AF.Sigmoid)
                    for z in range(3):
                        P.mmg(pp[z][:, :tn], [(w[:, z, kc, :], y[:, z * 8 + kc, :tn]) for kc in range(8)])
                    P.tt(t0_[:, :tn], pp[0][:, :tn], s_[:, 0, :tn], ALU.mult)
                    P.tt(t1_[:, :tn], pp[1][:, :tn], s_[:, 1, :tn], ALU.mult)
                    P.tt(t0_[:, :tn], t0_[:, :tn], t1_[:, :tn], ALU.add)
                    P.tt(t1_[:, :tn], pp[2][:, :tn], s_[:, 2, :tn], ALU.mult)
                    P.tt(mT[:, ob, t0:t0 + tn], t0_[:, :tn], t1_[:, :tn], ALU.add)
        with Stage(P) as sb_:
            wo = [sb_.sb(f"mow{i}", [128, 16, 512], BF16) for i in range(2)]
            g1 = [[sb_.sb(f"mog{i}_{r}", [128, 512], F32) for r in range(2)] for i in range(2)]
            xt = [sb_.sb(f"mox{i}", [128, 512], F32) for i in range(3)]
            tm = [sb_.sb(f"mot{i}", [128, 512], F32) for i in range(2)]
            ps = [sb_.ps(f"mop{i}", [128, 512], F32) for i in range(4)]
            u = 0
            for s in range(4):
                cs_ = slice(s * 512, (s + 1) * 512)
                w = wo[s % 2]
                P.dma("sp", w.v(), D.wout_bf[L][:, cs_].re("(kc p) n -> p kc n", p=128))
                for r in range(2):
                    P.dma("sp", g1[s % 2][r].v(), bc_row(D.modv[L][r:r + 1, 2 * DM + s * 512:2 * DM + (s + 1) * 512]))
                for t in range(ntile):
                    r = 0 if t < 16 else 1
                    x = xt[u % 3]
                    tmp = tm[u % 2]
                    p = ps[u % 4]
                    u += 1
                    P.dma("sp", x.v(), D.xcur[t * 128:(t + 1) * 128, cs_])
                    P.mmg(p.v(), [(mT[:, kc, t * 128:(t + 1) * 128], w[:, kc, :]) for kc in range(16)])
                    P.tt(tmp.v(), p.v(), g1[s % 2][r].v(), ALU.mult)
                    P.tt(x.v(), x.v(), tmp.v(), ALU.add)
                    P.dma("sp", D.xcur[t * 128:(t + 1) * 128, cs_], x.v())


def stage_norm2_router(P, D, L, need_ctx):
    ntile = NTILE if need_ctx else 16
    with Stage(P) as st:
        h2T = st.sb("h2T", [128, 16, NT], BF16)
        stage_norm(P, D, L, 1, h2T, st)
        P.dma("sp", D.h2Td.v().re("(c p) t -> p c t", p=128), h2T.v())
        rw = st.sb("rtw", [128, 16, 16], BF16)
        P.dma("pool", rw.v(), D.router_w.v().re("(kc p) e -> p kc e", p=128))
        rb = st.sb("rtb", [128, 16], F32)
        P.dma("sp", rb.v(), bc_row(D.router_bias.v().re("(o e) -> o e", o=1)))
        identf = st.sb("rtid", [128, 128], F32)
        P.dma("sp", identf.v(), D.ident_f.v())
        wT = st.sb("rtwT", [16, NT], F32)
        ps = [st.ps(f"rtp{i}", [128, 16], F32) for i in range(2)]
        pst = [st.ps(f"rtq{i}", [16, 128], F32) for i in range(2)]

        def tl(n, shape):
            return [st.sb(f"{n}{i}", shape, F32) for i in range(2)]
        sc, sel, eq, s2, m1, m2, grp, gm, geq, t1, oh1, t2, oh2, den = (tl("rsc", [128, 16]), tl("rsel", [128, 16]), tl("req", [128, 16]), tl("rs2", [128, 16]),
                                                                     tl("rm1", [128, 4]), tl("rm2", [128, 4]), tl("rgrp", [128, 4]), tl("rgm", [128, 1]), tl("rgeq", [128, 4]),
                                                                     tl("rt1", [128, 1]), tl("roh1", [128, 16]), tl("rt2", [128, 1]), tl("roh2", [128, 16]), tl("rden", [128, 1]))
        BIG = 1.0e9

        def g4(v):
            return v.re("p (g e) -> p g e", e=4)

        def b4(v):
            return v.m(lambda a: a.unsqueeze(2).broadcast_to([128, 4, 4]))

        def b16(v):
            return v.m(lambda a: a.broadcast_to([128, 16]))
        for t in range(ntile):
            i = t % 2
            P.mmg(ps[i].v(), [(h2T[:, kc, t * 128:(t + 1) * 128], rw[:, kc, :]) for kc in range(16)])
            P.act(sc[i].v(), ps[i].v(), AF.Sigmoid)
            P.tt(sel[i].v(), sc[i].v(), rb.v(), ALU.add)
            P.reduce(m1[i].v(), g4(sel[i].v()), ALU.max)
            P.tt(g4(eq[i].v()), g4(sel[i].v()), b4(m1[i].v()), ALU.is_equal)
            P.stt(s2[i].v(), eq[i].v(), -BIG, sel[i].v(), ALU.mult, ALU.add)
            P.reduce(m2[i].v(), g4(s2[i].v()), ALU.max)
            P.tt(grp[i].v(), m1[i].v(), m2[i].v(), ALU.add)
            P.reduce(gm[i].v(), grp[i].v(), ALU.max)
            P.ts(geq[i].v(), grp[i].v(), gm[i].v(), None, ALU.is_equal)
            P.ts(geq[i].v(), geq[i].v(), BIG, -BIG, ALU.mult, ALU.add)
            P.tt(g4(s2[i].v()), g4(sel[i].v()), b4(geq[i].v()), ALU.add)
            P.reduce(t1[i].v(), s2[i].v(), ALU.max)
            P.ts(oh1[i].v(), s2[i].v(), t1[i].v(), None, ALU.is_equal)
            P.stt(eq[i].v(), oh1[i].v(), -BIG, s2[i].v(), ALU.mult, ALU.add)
            P.reduce(t2[i].v(), eq[i].v(), ALU.max)
            P.ts(oh2[i].v(), eq[i].v(), t2[i].v(), None, ALU.is_equal)
            P.tt(oh1[i].v(), oh1[i].v(), oh2[i].v(), ALU.add)
            P.tt(oh1[i].v(), oh1[i].v(), sc[i].v(), ALU.mult)
            P.reduce(den[i].v(), oh1[i].v(), ALU.add)
            P.recip(den[i].v(), den[i].v())
            P.ts(oh1[i].v(), oh1[i].v(), den[i].v(), None, ALU.mult)
            P.transpose(pst[i].v(), oh1[i].v(), identf.v())
            P.copy(wT[:, t * 128:(t + 1) * 128], pst[i].v(), e="act")
        P.dma("sp", D.wgtT[:, 0:ntile * 128], wT[:, 0:ntile * 128])


def stage_moe(P, D, L, need_ctx):
    ntok = NT if need_ctx else T
    passes = []
    p0 = 0
    while p0 < ntok:
        pn = min(768, ntok - p0)
        passes.append((p0, pn))
        p0 += pn
    with Stage(P) as st:
        identf = st.sb("moid", [128, 128], F32)
        P.dma("sp", identf.v(), D.ident_f.v())
        selE = st.sb("mosel", [16, 16, 128], F32)
        P.dma("sp", selE.v(), D.selE.v())
        wT = st.sb("mowT", [16, NT], F32)
        P.dma("sp", wT[:, 0:ntok], D.wgtT[:, 0:ntok])
        g2c = [st.sb(f"mog2{r}", [128, 16], F32) for r in range(2)]
        for r in range(2):
            P.dma("sp", g2c[r].v(), D.modv[L][r, 5 * DM:6 * DM].re("(o p) -> p o", p=128), allow_slow_non_contiguous=True)
        h2 = st.sb("moh2", [128, 16, 768], BF16)
        facc = st.sb("mofacc", [128, 16, 768], F32)
        act_ = st.sb("moact", [128, 8, 768], BF16)
        wbc = st.sb("mowbc", [128, 768], F32)
        wgu = [st.sb(f"mowgu{i}", [128, 16, 2, 256], BF16) for i in range(3)]
        wd = [st.sb(f"mowd{i}", [128, 8, 512], BF16) for i in range(2)]
        sgt = [st.sb(f"mosg{i}", [128, 384], F32) for i in range(2)]
        tt_ = [st.sb(f"mott{i}", [128, 384], F32) for i in range(2)]
        xt = [st.sb(f"moxt{i}", [128, DM], F32) for i in range(2)]
        ps = [st.ps(f"mop{i}", [128, 512], F32) for i in range(8)]
        ug = 0
        ud = 0
        pi = 0
        for (p0, pn) in passes:
            chunks = [(c0, min(384, pn - c0)) for c0 in range(0, pn, 384)]
            P.dma("sp", h2[:, :, :pn], D.h2Td[:, p0:p0 + pn].re("(c p) t -> p c t", p=128))
            for e in range(16):
                for (c0, cn) in chunks:
                    p = ps[pi % 8]
                    pi += 1
                    P.mm(p[:, :cn], selE[:, e, :], wT[:, p0 + c0:p0 + c0 + cn])
                    P.copy(wbc[:, c0:c0 + cn], p[:, :cn], e="act")
                for s2_ in range(4):
                    w = wgu[ug % 3]
                    ug += 1
                    for gu in range(2):
                        P.dma("sp", w[:, :, gu, :],
                              D.wgu_bf[L][e][:, gu * 1024 + s2_ * 256:gu * 1024 + (s2_ + 1) * 256].re("(kc p) n -> p kc n", p=128))
                    for b2 in range(2):
                        blk = s2_ * 2 + b2
                        for ci, (c0, cn) in enumerate(chunks):
                            pg = ps[pi % 8]
                            pu = ps[(pi + 1) % 8]
                            pi += 2
                            P.mmg(pg[:, :cn], [(w[:, kc, 0, b2 * 128:(b2 + 1) * 128], h2[:, kc, c0:c0 + cn]) for kc in range(16)])
                            P.mmg(pu[:, :cn], [(w[:, kc, 1, b2 * 128:(b2 + 1) * 128], h2[:, kc, c0:c0 + cn]) for kc in range(16)])
                            sg_ = sgt[ci % 2]
                            t_ = tt_[ci % 2]
                            P.act(sg_[:, :cn], pg[:, :cn], AF.Silu)
                            P.tt(t_[:, :cn], pu[:, :cn], sg_[:, :cn], ALU.mult)
                            P.tt(act_[:, blk, c0:c0 + cn], t_[:, :cn], wbc[:, c0:c0 + cn], ALU.mult)
                for s4 in range(4):
                    w = wd[ud % 2]
                    ud += 1
                    P.dma("sp", w.v(), D.wd_bf[L][e][:, s4 * 512:(s4 + 1) * 512].re("(kc p) n -> p kc n", p=128))
                    for o4 in range(4):
                        ob = s4 * 4 + o4
                        for (c0, cn) in chunks:
                            p = ps[pi % 8]
                            pi += 1
                            P.mmg(p[:, :cn], [(w[:, kc, o4 * 128:(o4 + 1) * 128], act_[:, kc, c0:c0 + cn]) for kc in range(8)])
                            if e == 0:
                                P.copy(facc[:, ob, c0:c0 + cn], p[:, :cn], e="act")
                            else:
                                P.tt(facc[:, ob, c0:c0 + cn], facc[:, ob, c0:c0 + cn], p[:, :cn], ALU.add)
            for ob in range(16):
                lat = max(0, min(pn, T - p0))
                if lat > 0:
                    P.ts(facc[:, ob, 0:lat], facc[:, ob, 0:lat], g2c[0][:, ob:ob + 1], None, ALU.mult)
                if lat < pn:
                    P.ts(facc[:, ob, lat:pn], facc[:, ob, lat:pn], g2c[1][:, ob:ob + 1], None, ALU.mult)
            for ti in range(pn // 128):
                t = p0 // 128 + ti
                x = xt[ti % 2]
                P.dma("sp", x.v(), D.xcur[t * 128:(t + 1) * 128, :])
                for o4 in range(4):
                    p = ps[pi % 8]
                    pi += 1
                    for j in range(4):
                        ob = o4 * 4 + j
                        P.transpose(p[:, j * 128:(j + 1) * 128], facc[:, ob, ti * 128:(ti + 1) * 128], identf.v())
                    P.tt(x[:, o4 * 512:(o4 + 1) * 512], x[:, o4 * 512:(o4 + 1) * 512], p.v(), ALU.add)
                P.dma("sp", D.xcur[t * 128:(t + 1) * 128, :], x.v())


def stage_final(P, D):
    with Stage(P) as st:
        g = st.sb("fng", [128, DM], F32)
        P.dma("sp", g.v(), bc_row(D.final_norm_g.v().re("(o d) -> o d", o=1)))
        xt = [st.sb(f"fnx{i}", [128, DM], F32) for i in range(2)]
        sq = st.sb("fnsq", [128, DM], F32)
        ot = [st.sb(f"fno{i}", [128, DM], F32) for i in range(2)]
        ss = [st.sb(f"fns{i}", [128, 1], F32) for i in range(2)]
        for t in range(16):
            x = xt[t % 2]
            s = ss[t % 2]
            o = ot[t % 2]
            P.dma("sp", x.v(), D.xcur[t * 128:(t + 1) * 128, :])
            P.act(sq.v(), x.v(), AF.Square, accum=s.v())
            P.ts(s.v(), s.v(), 1.0 / DM, 1e-6, ALU.mult, ALU.add)
            P.act(s.v(), s.v(), AF.Sqrt)
            P.recip(s.v(), s.v())
            P.stt(o.v(), x.v(), s.v(), g.v(), ALU.mult, ALU.mult)
            P.dma("sp", D.out[t * 128:(t + 1) * 128, :], o.v())

ORDER = ["mod", "norm1", "win", "na", "mlaprep", "mla", "rw", "merge", "norm2", "moe"]


def build(L_list=(0, 1), upto=None, dbg=(), skip=(), rw_heads=range(16)):
    nc = bass.Bass("TRN2", target_bir_lowering=False)
    P = Prog(nc)
    D = NS()

    def inp(name, shape, dt=F32):
        b = dram(nc, name, shape, dt, kind="ExternalInput")
        setattr(D, name, b)
        return b

    inp("xin", [NT, DM]); inp("cT", [128, 16, 2])
    inp("w_mod", [2, DM, 6 * DM]); inp("b_mod", [2, 6 * DM])
    inp("norm1_g", [2, DM]); inp("norm2_g", [2, DM])
    inp("w_in", [2, DM, IN_W])
    inp("ident_f", [128, 128])
    inp("na_bias", [2, 16, 128, 5, 5, 128])
    inp("rope_c", [32, NT]); inp("rope_s", [32, NT]); inp("perm96", [96, 96])
    inp("mla_q_norm_g", [2, 512]); inp("mla_kv_norm_g", [2, 256])
    inp("mla_w_uq", [2, 512, 1536]); inp("mla_w_ukv", [2, 256, 2048])
    inp("tri_masks", [4, 128, 128])
    inp("rw_shift_mu", [2, 3488]); inp("rw_w0", [2, 2, 1024]); inp("rw_w_up", [2, 2, 64, 1024])
    inp("rw_a0", [2, 2, 1024]); inp("rw_a_up", [2, 2, 64, 1024]); inp("rw_g_up", [2, 160, 1024])
    inp("rw_k_k", [2, 1024]); inp("rw_k_a", [2, 1024]); inp("rw_r_k", [2, 16, 64])
    inp("rw_gn_w", [2, 1024]); inp("rw_gn_b", [2, 1024])
    inp("w_branch", [2, 3, 1024, DM]); inp("w_out", [2, DM, DM])
    inp("router_w", [DM, 16]); inp("router_bias", [16])
    inp("moe_w_gate_up", [2, 16, DM, 2048]); inp("moe_w_down", [2, 16, 1024, DM])
    inp("final_norm_g", [DM]); inp("selE", [16, 16, 128])
    D.out = dram(nc, "out", [T, DM], F32, kind="ExternalOutput")

    def scratch(name, shape, dt):
        kind = "ExternalOutput" if name in dbg else "Internal"
        b = dram(nc, name, shape, dt, kind=kind)
        setattr(D, name, b)
        return b

    scratch("xcur", [NT, DM], F32)
    D.modv = [scratch(f"modv{l}", [2, 6 * DM], F32) for l in range(2)]
    scratch("uT", [IN_WP, NT], BF16)
    scratch("utok", [NT, 1024], BF16)
    scratch("hTd", [DM, NT], BF16)
    D.ys = [scratch(f"ys{z}", [1024, NT], BF16) for z in range(3)]
    scratch("qmT", [16, 96, NT], BF16)
    scratch("kmT", [16, 96, NT], BF16)
    scratch("vmtok", [NT, 1024], BF16)
    scratch("yscan", [1024, NT], F32)
    scratch("h2Td", [DM, NT], BF16)
    D.wgu_bf = [[scratch(f"wgubf{l}_{e}", [DM, 2048], BF16) for e in range(16)] for l in range(2)]
    D.wd_bf = [[scratch(f"wdbf{l}_{e}", [1024, DM], BF16) for e in range(16)] for l in range(2)]
    D.wbr_bf = [[scratch(f"wbrbf{l}_{z}", [1024, DM], BF16) for z in range(3)] for l in range(2)]
    D.wout_bf = [scratch(f"woutbf{l}", [DM, DM], BF16) for l in range(2)]

    def emit_casts(L, h):
        for i in range(4):
            P.dma("pool", D.wgu_bf[L][h].part(i)[i * 512:(i + 1) * 512, :], D.moe_w_gate_up[L, h, i * 512:(i + 1) * 512, :], bg=True)
        for i in range(2):
            P.dma("pool", D.wd_bf[L][h].part(i)[i * 512:(i + 1) * 512, :], D.moe_w_down[L, h, i * 512:(i + 1) * 512, :], bg=True)
        if h < 3:
            for i in range(2):
                P.dma("pool", D.wbr_bf[L][h].part(i)[i * 512:(i + 1) * 512, :], D.w_branch[L, h, i * 512:(i + 1) * 512, :], bg=True)
        elif h < 7:
            i = h - 3
            P.dma("pool", D.wout_bf[L].part(i)[i * 512:(i + 1) * 512, :], D.w_out[L, i * 512:(i + 1) * 512, :], bg=True)
    scratch("wgtT", [16, NT], F32)

    def stop(name):
        return upto is not None and ORDER.index(name) >= ORDER.index(upto)

    with Stage(P) as st0:
        ident_bf = st0.sb("ident_bf", [128, 128], BF16)
        D.ident_bf = ident_bf
        P.dma("pool", ident_bf.v(), D.ident_f.v())
        for i in range(6):
            P.dma("sp", D.xcur[i * 384:(i + 1) * 384, :], D.xin[i * 384:(i + 1) * 384, :])
        for L in L_list:
            need_ctx = (L == 0)
            if "mod" not in skip:
                stage_mod(P, D, L)
                P.mark("stage_mod")
            if stop("mod"):
                break
            if "win" not in skip:
                with Stage(P) as stl:
                    hT = stl.sb("hT", [128, 16, NT], BF16)
                    stage_norm(P, D, L, 0, hT, stl)
                    P.mark("stage_norm")
                    if "hTd" in dbg:
                        P.dma("sp", D.hTd.v().re("(c p) t -> p c t", p=128), hT.v())
                    stage_win(P, D, L, hT)
                    P.mark("stage_win")
            if stop("win"):
                break
            if "na" not in skip:
                stage_na(P, D, L, need_ctx)
                P.mark("stage_na")
            if stop("na"):
                break
            if "mla" not in skip:
                stage_mla_prep(P, D, L, need_ctx)
                P.mark("stage_mla_prep")
                stage_mla_attn(P, D, L, need_ctx)
                P.mark("stage_mla_attn")
            if stop("mla"):
                break
            if "rw" not in skip:
                stage_rw(P, D, L, need_ctx, heads=rw_heads, dbgd=(D.yscan if "yscan" in dbg else None), on_head=(lambda h, L=L: emit_casts(L, h)))
                P.mark("stage_rw")
            if stop("rw"):
                break
            if "merge" not in skip:
                stage_merge(P, D, L, need_ctx)
                P.mark("stage_merge")
            if stop("merge"):
                break
            if "moe" not in skip:
                stage_norm2_router(P, D, L, need_ctx)
                P.mark("stage_norm2_router")
                stage_moe(P, D, L, need_ctx)
                P.mark("stage_moe")
            if stop("moe"):
                break
        else:
            stage_final(P, D)
            P.mark("stage_final")
        P.drain()
    print("ninst", P.ninst, {e: P.etot[e] for e in P.eng})
    build.marks = P.marks
    return nc


def host_consts():
    f32 = np.float32
    c = {}
    c["ident_f"] = np.eye(128, dtype=f32)
    nf = 8
    inv = (10000.0 ** (-np.arange(nf, dtype=np.float32) / nf)).astype(np.float32)
    pos = np.arange(T)
    row = (pos // 64).astype(np.float32)
    col = (pos % 64).astype(np.float32)
    rc = np.ones((32, NT), f32)
    rs = np.zeros((32, NT), f32)
    for d in range(32):
        p = row if d < 16 else col
        dd = d % 16
        f = dd % 8
        ang = (p * inv[f]).astype(np.float32)
        rc[d, :T] = np.cos(ang)
        rs[d, :T] = (-np.sin(ang)) if dd < 8 else np.sin(ang)
    c["rope_c"] = rc
    c["rope_s"] = rs
    pm = np.zeros((96, 96), f32)
    for d in range(32):
        dd = d % 16
        partner = (d + 8) if dd < 8 else (d - 8)
        pm[64 + partner, 64 + d] = 1.0
    c["perm96"] = pm
    i = np.arange(128)
    mu_ = (i[:, None] < i[None, :]).astype(f32)
    ml_ = (i[:, None] > i[None, :]).astype(f32)
    se = np.zeros((16, 16, 128), f32)
    for e in range(16):
        se[e, e, :] = 1.0
    c["selE"] = se
    c["tri_masks"] = np.stack([mu_, ml_, mu_ + np.eye(128, dtype=f32), ml_ + np.eye(128, dtype=f32)], 0)
    return c


def na_bias_table(rpb):
    Lr = rpb.shape[0]
    out = np.full((Lr, 16, 128, 5, 5, 128), -1e30, np.float32)
    jrep = [0, 1, 2, 14, 15]
    for pi, j in enumerate(jrep):
        kr0 = na_kr0(j)
        qtok = np.arange(128)
        qi = 2 * j + qtok // 64
        qj = qtok % 64
        r0 = np.clip(qi - 4, 0, 24)
        cstart = np.clip(qj - 8, 0, 48)
        for c in range(5):
            ktok = np.arange(128)
            ar = kr0 + 2 * c + ktok // 64
            kc = ktok % 64
            rv = (ar[:, None] >= r0[None, :]) & (ar[:, None] < r0[None, :] + 8)
            cv = (kc[:, None] >= cstart[None, :]) & (kc[:, None] < cstart[None, :] + 16)
            ridx = np.clip(ar[:, None] - qi[None, :] + 7, 0, 14)
            cidx = np.clip(kc[:, None] - qj[None, :] + 15, 0, 30)
            val = rpb[:, :, ridx, cidx]
            out[:, :, :, pi, c, :] = np.where((rv & cv)[None, None], val, np.float32(-1e30))
    return out


def make_inputs(inputs, b, consts, nab):
    f32 = np.float32
    x = np.asarray(inputs["x"][b], f32)
    ctx = np.asarray(inputs["ctx"][b], f32)
    d = dict(consts)
    d["xin"] = np.ascontiguousarray(np.concatenate([x, ctx], axis=0))
    cc = np.stack([np.asarray(inputs["c"][b], f32), np.asarray(inputs["c_ctx"], f32)], axis=0)
    d["cT"] = np.ascontiguousarray(cc.reshape(2, 16, 128).transpose(2, 1, 0))
    for k in ["w_mod", "b_mod", "norm1_g", "norm2_g", "w_in", "mla_q_norm_g", "mla_kv_norm_g", "mla_w_uq", "mla_w_ukv",
              "rw_shift_mu", "rw_w0", "rw_w_up", "rw_a0", "rw_a_up", "rw_g_up", "rw_k_k", "rw_k_a", "rw_r_k", "rw_gn_w", "rw_gn_b",
              "w_branch", "w_out", "router_w", "router_bias", "moe_w_gate_up", "moe_w_down", "final_norm_g"]:
        d[k] = np.asarray(inputs[k], f32)
    d["na_bias"] = nab
    return d


def kernel(**inputs):
    nc = build()
    consts = host_consts()
    nab = na_bias_table(np.asarray(inputs["na_rpb"], np.float32))
    in_maps = [make_inputs(inputs, b, consts, nab) for b in range(8)]
    res = run_bass_kernel_spmd(nc, in_maps, core_ids=list(range(8)))
    return np.stack([np.asarray(r["out"], np.float32) for r in res.results], axis=0)
```

```python
import numpy as np
import concourse.bass as bass
import concourse.mybir as mybir
from contextlib import ExitStack

F32 = mybir.dt.float32
BF16 = mybir.dt.bfloat16
AF = mybir.ActivationFunctionType
ALU = mybir.AluOpType
AX = mybir.AxisListType

SEM_ROT = 30000
N_DMA_SEMS = 40


class V:
    __slots__ = ("ap", "buf", "key")

    def __init__(self, ap, buf, key=None):
        self.ap = ap
        self.buf = buf
        self.key = key

    def __getitem__(self, idx):
        return V(self.ap[idx], self.buf, self.key)

    def m(self, fn):
        return V(fn(self.ap), self.buf, self.key)

    def re(self, s, **kw):
        return V(self.ap.rearrange(s, **kw), self.buf, self.key)

    def bc(self, dt):
        return V(self.ap.bitcast(dt), self.buf, self.key)

    def k(self, key):
        return V(self.ap, self.buf, key)


class Buf:
    def __init__(self, name, t):
        self.name = name
        self.t = t
        self.state = {None: [None, []]}
        self.psum = False
        self.bank_ev = None

    def ap(self):
        t = self.t
        return t.ap() if hasattr(t, "ap") and callable(getattr(t, "ap")) and not isinstance(t, bass.AP) else t

    def __getitem__(self, idx):
        return V(self.ap()[idx], self, None)

    def v(self):
        return V(self.ap(), self, None)

    def part(self, key):
        if key not in self.state:
            w, r = self.state[None]
            self.state[key] = [w, list(r)]
        return V(self.ap(), self, key)

    def keys_for(self, key):
        if key is None:
            return list(self.state.keys())
        if isinstance(key, tuple):
            out = []
            for k in key:
                out += self.keys_for(k)
            return out
        if key not in self.state:
            w, r = self.state[None]
            self.state[key] = [w, list(r)]
        return [key]


class Prog:
    def __init__(self, nc):
        self.nc = nc
        self.eng = {"pe": nc.tensor, "act": nc.scalar, "dve": nc.vector, "pool": nc.gpsimd, "sp": nc.sync}
        self.esem = {}
        self.ecnt = {}
        self.etot = {}
        for e in self.eng:
            self.esem[e] = nc.alloc_semaphore(f"es_{e}_0")
            self.ecnt[e] = 0
            self.etot[e] = 0
        self.waited = {e: {} for e in self.eng}
        self.dsems = [nc.alloc_semaphore(f"dma_{i}") for i in range(N_DMA_SEMS)]
        self.dcnt = [0] * N_DMA_SEMS
        self.dnext = 0
        self.bsems = [nc.alloc_semaphore(f"bgdma_{i}") for i in range(8)]
        self.bcnt = [0] * 8
        self.bnext = 0
        self.all_sems = {}
        self.ninst = 0
        self.pe_sems = {id(self.esem["pe"])}
        self.npe = 0
        self.marks = []

    def _wait(self, e, ev):
        if ev is None:
            return
        sem, val = ev
        sid = id(sem)
        self.all_sems[sid] = sem
        if self.waited[e].get(sid, 0) >= val:
            return
        self.eng[e].wait_ge(sem, val)
        self.waited[e][sid] = val

    def _wait2(self, e, ev):
        if ev is not None and e == "pe" and id(ev[0]) in self.pe_sems:
            return
        self._wait(e, ev)

    def _bank(self, e, vs):
        for v in vs:
            if v.buf.psum and v.buf.bank_ev is not None and v.buf.bank_ev[1] != e:
                self._wait(e, v.buf.bank_ev[0])

    def _deps(self, e, reads, writes):
        self._bank(e, list(reads) + list(writes))
        for v in reads:
            for k in v.buf.keys_for(v.key):
                self._wait2(e, v.buf.state[k][0])
        for v in writes:
            for k in v.buf.keys_for(v.key):
                st = v.buf.state[k]
                self._wait2(e, st[0])
                for ev in st[1]:
                    self._wait2(e, ev)

    def _record(self, ev, reads, writes, e=None):
        for v in list(reads) + list(writes):
            if v.buf.psum:
                v.buf.bank_ev = (ev, e)
        for v in reads:
            for k in v.buf.keys_for(v.key):
                v.buf.state[k][1].append(ev)
        for v in writes:
            for k in v.buf.keys_for(v.key):
                v.buf.state[k][0] = ev
                v.buf.state[k][1] = []

    def _newev(self, e, inst):
        if self.ecnt[e] >= SEM_ROT:
            self.esem[e] = self.nc.alloc_semaphore(f"es_{e}_{self.etot[e]}")
            self.ecnt[e] = 0
            if e == "pe":
                self.pe_sems.add(id(self.esem[e]))
        self.ecnt[e] += 1
        self.etot[e] += 1
        inst.then_inc(self.esem[e], 1)
        ev = (self.esem[e], self.ecnt[e])
        self.waited[e][id(self.esem[e])] = 0 if id(self.esem[e]) not in self.waited[e] else self.waited[e][id(self.esem[e])]
        self.all_sems[id(self.esem[e])] = self.esem[e]
        self.last_ev = getattr(self, "last_ev", {})
        self.last_ev[e] = ev
        return ev

    def mark(self, name):
        self.marks.append((name, self.npe))

    def op(self, e, fn, reads, writes, nosync_same=False):
        if e == "pe":
            self.npe += 1
        self._deps(e, reads, writes)
        inst = fn()
        ev = self._newev(e, inst)
        self._record(ev, reads, writes, e)
        self.ninst += 1
        return inst

    def group(self, e, fns, reads, writes):
        self._deps(e, reads, writes)
        if e == "pe":
            self.npe += len(fns)
        inst = None
        for fn in fns:
            inst = fn()
            self.ninst += 1
        ev = self._newev(e, inst)
        self._record(ev, reads, writes, e)
        return inst

    def dma(self, q, out, in_, bg=False, **kw):
        if bg:
            i = self.bnext
            self.bnext = (self.bnext + 1) % len(self.bsems)
            sem = self.bsems[i]
            cnt = self.bcnt
        else:
            i = self.dnext
            self.dnext = (self.dnext + 1) % N_DMA_SEMS
            sem = self.dsems[i]
            cnt = self.dcnt
        if cnt[i] > 0:
            self._wait(q, (sem, cnt[i]))
        self._deps(q, [in_], [out])
        inst = self.eng[q].dma_start(out=out.ap, in_=in_.ap, **kw)
        inst.then_inc(sem, 16)
        cnt[i] += 16
        ev = (sem, cnt[i])
        self.all_sems[id(sem)] = sem
        self._record(ev, [in_], [out])
        self.ninst += 1
        return ev

    def drain(self):
        evs = []
        for e in self.eng:
            if self.ecnt[e] > 0:
                evs.append((self.esem[e], self.ecnt[e]))
        for i in range(N_DMA_SEMS):
            if self.dcnt[i] > 0:
                evs.append((self.dsems[i], self.dcnt[i]))
        for i in range(len(self.bsems)):
            if self.bcnt[i] > 0:
                evs.append((self.bsems[i], self.bcnt[i]))
        for e in self.eng:
            for ev in evs:
                self._wait(e, ev)

    def mm(self, out, lhsT, rhs, start=True, stop=True):
        if lhsT.ap.dtype == F32:
            self.npe += 1
        return self.op("pe", lambda: self.nc.tensor.matmul(out.ap, lhsT.ap, rhs.ap, start=start, stop=stop),
                       [lhsT, rhs] + ([] if start else [out]), [out])

    def mmg(self, out, pairs):
        n = len(pairs)
        if pairs[0][0].ap.dtype == F32:
            self.npe += n
        fns = []
        reads = []
        for j, (l, r) in enumerate(pairs):
            fns.append((lambda l=l, r=r, j=j: self.nc.tensor.matmul(out.ap, l.ap, r.ap, start=(j == 0), stop=(j == n - 1))))
            reads += [l, r]
        return self.group("pe", fns, reads, [out])

    def transpose(self, out, in_, ident):
        return self.op("pe", lambda: self.nc.tensor.transpose(out.ap, in_.ap, ident.ap), [in_, ident], [out])

    def act(self, out, in_, func, bias=None, scale=None, accum=None, e="act"):
        kw = {}
        reads = [in_]
        if bias is not None:
            if isinstance(bias, V):
                kw["bias"] = bias.ap
                reads.append(bias)
            else:
                kw["bias"] = bias
        if scale is not None:
            if isinstance(scale, V):
                kw["scale"] = scale.ap
                reads.append(scale)
            else:
                kw["scale"] = scale
        writes = [out]
        if accum is not None:
            kw["accum_out"] = accum.ap
            writes.append(accum)
        return self.op("act", lambda: self.nc.scalar.activation(out.ap, in_.ap, func, **kw), reads, writes)

    def tt(self, out, a, b, op, e="dve"):
        return self.op(e, lambda: self.eng[e].tensor_tensor(out.ap, a.ap, b.ap, op), [a, b], [out])

    def ts(self, out, a, s1, s2, op0, op1=None, accum=None, e="dve"):
        reads = [a]
        s1a = s1
        s2a = s2
        if isinstance(s1, V):
            reads.append(s1)
            s1a = s1.ap
        if isinstance(s2, V):
            reads.append(s2)
            s2a = s2.ap
        writes = [out]
        kw = {}
        if op1 is not None:
            kw["op1"] = op1
        if accum is not None:
            kw["accum_out"] = accum.ap
            writes.append(accum)
        return self.op(e, lambda: self.eng[e].tensor_scalar(out.ap, a.ap, s1a, s2a, op0, **kw), reads, writes)

    def stt(self, out, a, s, b, op0, op1, accum=None):
        reads = [a, b]
        sa = s
        if isinstance(s, V):
            reads.append(s)
            sa = s.ap
        writes = [out]
        kw = {}
        if accum is not None:
            kw["accum_out"] = accum.ap
            writes.append(accum)
        return self.op("dve", lambda: self.nc.vector.scalar_tensor_tensor(out.ap, a.ap, sa, b.ap, op0, op1, **kw), reads, writes)

    def copy(self, out, in_, e="dve"):
        if e == "act":
            return self.op("act", lambda: self.nc.scalar.copy(out.ap, in_.ap), [in_], [out])
        return self.op(e, lambda: self.eng[e].tensor_copy(out.ap, in_.ap), [in_], [out])

    def memset(self, out, val, e="dve"):
        return self.op(e, lambda: self.eng[e].memset(out.ap, val), [], [out])

    def reduce(self, out, in_, op, axis=None, e="dve"):
        axis = axis or AX.X
        return self.op(e, lambda: self.eng[e].tensor_reduce(out.ap, in_.ap, axis, op), [in_], [out])

    def recip(self, out, in_):
        return self.op("dve", lambda: self.nc.vector.reciprocal(out.ap, in_.ap), [in_], [out])


class Stage:
    uid = 0

    def __init__(self, P):
        self.P = P
        self.es = ExitStack()

    def __enter__(self):
        self.es.__enter__()
        return self

    def sb(self, name, shape, dtype=F32):
        Stage.uid += 1
        name = f"{name}_s{Stage.uid}"
        t = self.es.enter_context(self.P.nc.sbuf_tensor(name, list(shape), dtype))
        return Buf(name, t)

    def ps(self, name, shape, dtype=F32):
        Stage.uid += 1
        name = f"{name}_p{Stage.uid}"
        t = self.es.enter_context(self.P.nc.psum_tensor(name, list(shape), dtype))
        b = Buf(name, t)
        b.psum = True
        return b

    def __exit__(self, *a):
        self.P.drain()
        return self.es.__exit__(*a)

from concourse.bass_utils import run_bass_kernel_spmd
import ml_dtypes
import math

DM = 2048
T = 2048
TC = 256
NT = T + TC
NTILE = NT // 128
IN_W = 13504
IN_WP = 13568
OFF_NA = 0
OFF_RW = 3072
OFF_CQ = 6560
OFF_CKV = 7072
OFF_KR = 7328
OFF_G = 7360
TOKCH = [(0, 512), (512, 512), (1024, 512), (1536, 512), (2048, 256)]


class NS:
    pass


def dram(nc, name, shape, dtype, kind="Internal"):
    return Buf(name, nc.dram_tensor(name, list(shape), dtype, kind=kind))


def stage_mod(P, D, L):
    with Stage(P) as st:
        cT = st.sb("cT", [128, 16, 2], F32)
        cs = st.sb("cs", [128, 16, 2], F32)
        P.dma("sp", cT.v(), D.cT.v())
        P.act(cs.v(), cT.v(), AF.Silu)
        wb = [st.sb(f"wm{i}", [128, 16, 512], F32) for i in range(3)]
        bt = [st.sb(f"bm{i}", [2, 512], F32) for i in range(2)]
        ot = [st.sb(f"om{i}", [2, 512], F32) for i in range(2)]
        ps = [st.ps(f"pm{i}", [2, 512], F32) for i in range(2)]
        for j in range(24):
            w = wb[j % 3]
            for q4 in range(4):
                P.dma(("sp" if q4 % 2 == 0 else "act"), w[:, q4 * 4:(q4 + 1) * 4, :],
                      D.w_mod[L, q4 * 512:(q4 + 1) * 512, j * 512:(j + 1) * 512].re("(kc p) n -> p kc n", p=128))
            P.dma("sp", bt[j % 2].v(), D.b_mod[L:L + 1, j * 512:(j + 1) * 512].m(lambda a: a.broadcast_to([2, 512])))
            P.mmg(ps[j % 2].v(), [(cs[:, kc, :], w[:, kc, :]) for kc in range(16)])
            P.tt(ot[j % 2].v(), ps[j % 2].v(), bt[j % 2].v(), ALU.add)
            P.dma("sp", D.modv[L][:, j * 512:(j + 1) * 512], ot[j % 2].v())


def bc_row(v, n=128):
    return v.m(lambda a: a.broadcast_to([n, a.shape[-1]]))


def stage_norm(P, D, L, which, hT, st_outer):
    gsrc = D.norm1_g if which == 0 else D.norm2_g
    sh_i, sc_i = (0, 1) if which == 0 else (3, 4)
    with Stage(P) as st:
        g = st.sb("ng", [128, DM], F32)
        P.dma("sp", g.v(), bc_row(gsrc[L:L + 1, :]))
        A = []
        Bs = []
        for r in range(2):
            a = st.sb(f"nA{r}", [128, DM], F32)
            b = st.sb(f"nB{r}", [128, DM], F32)
            P.dma("sp", a.v(), bc_row(D.modv[L][r:r + 1, sc_i * DM:(sc_i + 1) * DM]))
            P.dma("sp", b.v(), bc_row(D.modv[L][r:r + 1, sh_i * DM:(sh_i + 1) * DM]))
            P.stt(a.v(), a.v(), 1.0, g.v(), ALU.add, ALU.mult)
            A.append(a)
            Bs.append(b)
        xt = [st.sb(f"nx{i}", [128, DM], F32) for i in range(2)]
        sq = st.sb("nsq", [128, DM], F32)
        hb = [st.sb(f"nh{i}", [128, DM], BF16) for i in range(2)]
        ss = [st.sb(f"nss{i}", [128, 1], F32) for i in range(2)]
        pt = [st.ps(f"npt{i}", [128, 16, 128], BF16) for i in range(2)]
        for t in range(NTILE):
            r = 0 if t < 16 else 1
            x = xt[t % 2]
            s = ss[t % 2]
            h = hb[t % 2]
            p = pt[t % 2]
            P.dma("sp", x.v(), D.xcur[t * 128:(t + 1) * 128, :])
            P.act(sq.v(), x.v(), AF.Square, accum=s.v())
            P.ts(s.v(), s.v(), 1.0 / DM, 1e-6, ALU.mult, ALU.add)
            P.act(s.v(), s.v(), AF.Sqrt)
            P.recip(s.v(), s.v())
            P.stt(sq.v(), x.v(), s.v(), A[r].v(), ALU.mult, ALU.mult)
            P.tt(h.v(), sq.v(), Bs[r].v(), ALU.add)
            for c in range(16):
                P.transpose(p[:, c, :], h[:, c * 128:(c + 1) * 128], D.ident_bf.v())
            P.copy(hT[:, :, t * 128:(t + 1) * 128], p.v(), e=("act" if t % 2 else "dve"))


def stage_win(P, D, L, hT):
    with Stage(P) as st:
        wb = [st.sb(f"ww{i}", [128, 16, 512], BF16) for i in range(2)]
        ob = [st.sb(f"wo{i}", [128, NT], BF16) for i in range(2)]
        ps = [st.ps(f"wp{i}", [128, 512], F32) for i in range(4)]
        nslab = (IN_W + 511) // 512
        pi = 0
        oi = 0
        for s in range(nslab):
            c0 = s * 512
            cw = min(512, IN_W - c0)
            w = wb[s % 2]
            for q4 in range(4):
                P.dma("pool", w[:, q4 * 4:(q4 + 1) * 4, :cw],
                      D.w_in[L, q4 * 512:(q4 + 1) * 512, c0:c0 + cw].re("(kc p) n -> p kc n", p=128))
            tokmajor = (2048 <= c0 < 3072)
            if tokmajor:
                for t in range(NTILE):
                    p = ps[pi % 4]
                    pi += 1
                    P.mmg(p.v(), [(hT[:, kc, t * 128:(t + 1) * 128], w[:, kc, :]) for kc in range(16)])
                    o = ob[oi % 2]
                    oi += 1
                    P.copy(o[:, :512], p.v(), e=("act" if t % 2 else "dve"))
                    P.dma("sp", D.utok[t * 128:(t + 1) * 128, c0 - 2048:c0 - 2048 + 512], o[:, :512])
                continue
            for blk in range((cw + 127) // 128):
                m = min(128, cw - blk * 128)
                o = ob[oi % 2]
                oi += 1
                for ci, (t0, tn) in enumerate(TOKCH):
                    p = ps[pi % 4]
                    pi += 1
                    P.mmg(p[:m, :tn], [(w[:, kc, blk * 128:blk * 128 + m], hT[:, kc, t0:t0 + tn]) for kc in range(16)])
                    P.copy(o[:m, t0:t0 + tn], p[:m, :tn], e=("act" if ci % 2 else "dve"))
                r0 = c0 + blk * 128
                P.dma("sp", D.uT[r0:r0 + m, :], o[:m, :])


def na_kr0(j):
    return min(max(2 * j - 4, 0), 22)


def na_pat(j):
    return 0 if j == 0 else 1 if j == 1 else 2 if j <= 13 else 3 if j == 14 else 4


def stage_na(P, D, L, need_ctx):
    with Stage(P) as st:
        qT = [st.sb(f"naq{i}", [64, NT], BF16) for i in range(2)]
        kT = [st.sb(f"nak{i}", [64, NT], BF16) for i in range(2)]
        va = [st.sb(f"nav{i}", [128, NTILE, 65], BF16) for i in range(2)]
        bias = [st.sb(f"nab{i}", [128, 5, 5, 128], F32) for i in range(2)]
        eloc = [st.sb(f"nae{i}", [128, 5, 128], F32) for i in range(2)]
        pT = [st.sb(f"nap{i}", [128, 7, 128], BF16) for i in range(2)]
        rec = [st.sb(f"nar{i}", [128, 1], F32) for i in range(2)]
        ytok = st.sb("nay", [128, NTILE, 64], BF16)
        yT = [st.sb(f"nayT{i}", [64, NT], BF16) for i in range(2)]
        ps_s = [st.ps(f"nps{i}", [128, 8, 128], F32) for i in range(2)]
        ps_o = [st.ps(f"npo{i}", [128, 65], F32) for i in range(2)]
        ps_t = st.ps("npt", [64, 8, 128], BF16)
        for i in range(2):
            P.memset(va[i][:, :, 64:65], 1.0)
        ntq = NTILE if need_ctx else 16
        u = 0
        for h in range(16):
            b = h % 2
            P.dma("sp", qT[b].v(), D.uT[h * 64:(h + 1) * 64, :])
            P.dma("sp", kT[b].v(), D.uT[1024 + h * 64:1024 + (h + 1) * 64, :])
            P.dma("sp", va[b][:, :, 0:64], D.utok[:, h * 64:(h + 1) * 64].re("(c p) d -> p c d", p=128))
            P.dma("sp", bias[b].v(), D.na_bias[L, h])
            def na_tiles(j):
                if j < 16:
                    t0 = na_kr0(j) // 2
                    return [t0 + c for c in range(5)] + [16, 17]
                return [16, 17]

            def na_S(j, uu):
                s = ps_s[uu % 2]
                q = qT[b][:, j * 128:(j + 1) * 128]
                P.group("pe", [(lambda c=c, tt_=tt_: P.nc.tensor.matmul(s[:, c, :].ap, kT[b][:, tt_ * 128:(tt_ + 1) * 128].ap, q.ap, start=True, stop=True))
                               for c, tt_ in enumerate(na_tiles(j))], [kT[b].v(), qT[b].v()], [s.v()])
            def na_soft(j, uu):
                s = ps_s[uu % 2]
                e_ = eloc[uu % 2]
                p_ = pT[uu % 2]
                if j < 16:
                    P.stt(e_.v(), s[:, 0:5, :], 0.125, bias[b][:, na_pat(j), :, :], ALU.mult, ALU.add)
                    P.act(p_[:, 0:5, :], e_.v(), AF.Exp)
                    P.act(p_[:, 5:7, :], s[:, 5:7, :], AF.Exp, scale=0.125)
                else:
                    P.act(p_[:, 0:2, :], s[:, 0:2, :], AF.Exp, scale=0.125)
            na_S(0, u)
            na_soft(0, u)
            for j in range(ntq):
                o = ps_o[u % 2]
                p_ = pT[u % 2]
                r_ = rec[u % 2]
                if j + 1 < ntq:
                    na_S(j + 1, u + 1)
                    na_soft(j + 1, u + 1)
                u += 1
                tiles = na_tiles(j)
                P.mmg(o.v(), [(p_[:, c, :], va[b][:, tt_, :]) for c, tt_ in enumerate(tiles)])
                P.recip(r_.v(), o[:, 64:65])
                P.ts(ytok[:, j, :], o[:, 0:64], r_.v(), None, ALU.mult)
            for g0 in range(0, ntq, 8):
                gn = min(8, ntq - g0)
                for jj in range(gn):
                    P.transpose(ps_t[:, jj, :], ytok[:, g0 + jj, :], D.ident_bf.v())
                P.copy(yT[b][:, g0 * 128:(g0 + gn) * 128], ps_t[:, 0:gn, :], e="act")
            P.dma("sp", D.ys[0][h * 64:(h + 1) * 64, 0:ntq * 128], yT[b][:, 0:ntq * 128])


def stage_mla_prep(P, D, L, need_ctx):
    with Stage(P) as st:
        cq = st.sb("mcq", [128, 4, NT], BF16)
        ckv = st.sb("mckv", [128, 2, NT], BF16)
        kr = st.sb("mkr", [96, NT], BF16)
        krr = st.sb("mkrr", [96, NT], BF16)
        cs = st.sb("mcs", [96, NT], F32)
        sg = st.sb("msg", [96, NT], F32)
        perm = st.sb("mperm", [96, 96], BF16)
        ones = st.sb("mones", [128, 128], BF16)
        gq = st.sb("mgq", [128, 4], F32)
        gkv = st.sb("mgkv", [128, 2], F32)
        wq = st.sb("mwq", [128, 4, 1536], BF16)
        wkv = st.sb("mwkv", [128, 2, 2048], BF16)
        P.dma("sp", cq.v(), D.uT[OFF_CQ:OFF_CQ + 512, :].re("(c p) t -> p c t", p=128))
        P.dma("sp", ckv.v(), D.uT[OFF_CKV:OFF_CKV + 256, :].re("(c p) t -> p c t", p=128))
        P.memset(kr.v(), 0.0)
        P.dma("sp", kr[64:96, :], D.uT[OFF_KR:OFF_KR + 32, :])
        P.dma("sp", cs[64:96, :], D.rope_c.v())
        P.dma("sp", sg[64:96, :], D.rope_s.v())
        P.dma("pool", perm.v(), D.perm96.v())
        P.memset(ones.v(), 1.0)
        P.dma("sp", gq.v(), D.mla_q_norm_g[L].re("(c p) -> p c", p=128), allow_slow_non_contiguous=True)
        P.dma("sp", gkv.v(), D.mla_kv_norm_g[L].re("(c p) -> p c", p=128), allow_slow_non_contiguous=True)
        for kc in range(4):
            P.dma("pool", wq[:, kc, :], D.mla_w_uq[L, kc * 128:(kc + 1) * 128, :])
        for kc in range(2):
            P.dma("pool", wkv[:, kc, :], D.mla_w_ukv[L, kc * 128:(kc + 1) * 128, :])
        sq = st.sb("msq", [128, 4, 512], BF16)
        rs = st.sb("mrs", [128, 512], F32)
        ps = [st.ps(f"mps{i}", [128, 512], F32) for i in range(2)]
        psb = [st.ps(f"mpb{i}", [96, 512], F32) for i in range(2)]
        for (src, g, nblk) in ((cq, gq, 4), (ckv, gkv, 2)):
            for ci, (t0, tn) in enumerate(TOKCH):
                p = ps[ci % 2]
                P.act(sq[:, 0:nblk, :tn], src[:, :, t0:t0 + tn], AF.Square)
                P.mmg(p[:, :tn], [(ones.v(), sq[:, c, :tn]) for c in range(nblk)])
                P.ts(rs[:, :tn], p[:, :tn], 1.0 / (128 * nblk), 1e-6, ALU.mult, ALU.add)
                P.act(rs[:, :tn], rs[:, :tn], AF.Sqrt)
                P.recip(rs[:, :tn], rs[:, :tn])
                for c in range(nblk):
                    P.stt(src[:, c, t0:t0 + tn], src[:, c, t0:t0 + tn], g[:, c:c + 1], rs[:, :tn], ALU.mult, ALU.mult)
        tmp = st.sb("mtmp", [96, 512], F32)
        tmp2 = st.sb("mtmp2", [96, 512], F32)
        for ci, (t0, tn) in enumerate(TOKCH):
            p = psb[ci % 2]
            P.mm(p[:, :tn], perm.v(), kr[:, t0:t0 + tn])
            P.tt(tmp[64:96, :tn], p[64:96, :tn], sg[64:96, t0:t0 + tn], ALU.mult)
            P.tt(tmp2[64:96, :tn], kr[64:96, t0:t0 + tn], cs[64:96, t0:t0 + tn], ALU.mult)
            P.tt(krr[64:96, t0:t0 + tn], tmp[64:96, :tn], tmp2[64:96, :tn], ALU.add)
        for h in range(16):
            P.dma("sp", D.kmT[h, 64:96, :], krr[64:96, :])
        qh = [st.sb(f"mqh{i}", [96, NT], BF16) for i in range(2)]
        kh = [st.sb(f"mkh{i}", [64, NT], BF16) for i in range(2)]
        for h in range(16):
            q_ = qh[h % 2]
            k_ = kh[h % 2]
            for ci, (t0, tn) in enumerate(TOKCH):
                if t0 >= T and not need_ctx:
                    continue
                pa = ps[ci % 2]
                pb = psb[ci % 2]
                P.mmg(pa[:96, :tn], [(wq[:, kc, h * 96:(h + 1) * 96], cq[:, kc, t0:t0 + tn]) for kc in range(4)])
                P.copy(q_[:, t0:t0 + tn], pa[:96, :tn], e="act")
                P.mm(pb[:, :tn], perm.v(), q_[:, t0:t0 + tn])
                P.tt(tmp[64:96, :tn], pb[64:96, :tn], sg[64:96, t0:t0 + tn], ALU.mult)
                P.tt(tmp2[64:96, :tn], pa[64:96, :tn], cs[64:96, t0:t0 + tn], ALU.mult)
                P.tt(q_[64:96, t0:t0 + tn], tmp[64:96, :tn], tmp2[64:96, :tn], ALU.add)
            nq = NT if need_ctx else T
            P.dma("sp", D.qmT[h, :, 0:nq], q_[:, 0:nq])
            for ci, (t0, tn) in enumerate(TOKCH):
                pa = ps[ci % 2]
                P.mmg(pa[:64, :tn], [(wkv[:, kc, h * 128:h * 128 + 64], ckv[:, kc, t0:t0 + tn]) for kc in range(2)])
                P.copy(k_[:, t0:t0 + tn], pa[:64, :tn], e="act")
            P.dma("sp", D.kmT[h, 0:64, :], k_.v())
        vo = [st.sb(f"mvo{i}", [128, 1024], BF16) for i in range(2)]
        wv = wkv.v().re("p kc (h two d) -> p kc h two d", two=2, d=64)
        for t in range(NTILE):
            o = vo[t % 2]
            for half in range(2):
                pa = ps[half]
                P.mmg(pa.v().re("p (h d) -> p h d", d=64),
                      [(ckv[:, kc, t * 128:(t + 1) * 128], wv[:, kc, half * 8:(half + 1) * 8, 1, :]) for kc in range(2)])
                P.copy(o[:, half * 512:(half + 1) * 512], pa.v(), e=("act" if half else "dve"))
            P.dma("sp", D.vmtok[t * 128:(t + 1) * 128, :], o.v())


def stage_mla_attn(P, D, L, need_ctx):
    sc = 1.0 / math.sqrt(96.0)
    with Stage(P) as st:
        qT = [st.sb(f"aq{i}", [96, NT], BF16) for i in range(2)]
        kT = [st.sb(f"ak{i}", [96, NT], BF16) for i in range(2)]
        va = [st.sb(f"av{i}", [128, NTILE, 65], BF16) for i in range(2)]
        pT = [st.sb(f"ap{i}", [128, 512], BF16) for i in range(3)]
        rec = [st.sb(f"ar{i}", [128, 1], F32) for i in range(4)]
        ytok = st.sb("ay", [128, NTILE, 64], BF16)
        yT = [st.sb(f"ayT{i}", [64, NT], BF16) for i in range(2)]
        ps_s = [st.ps(f"aps{i}", [128, 512], F32) for i in range(2)]
        ps_o = [st.ps(f"apo{i}", [128, 65], F32) for i in range(4)]
        ps_t = st.ps("apt", [64, 8, 128], BF16)
        for i in range(2):
            P.memset(va[i][:, :, 64:65], 1.0)
        ntq = NTILE if need_ctx else 16
        u = 0
        for h in range(16):
            b = h % 2
            nq = NT if need_ctx else T
            P.dma("sp", qT[b][:, 0:nq], D.qmT[h, :, 0:nq])
            P.dma("sp", kT[b].v(), D.kmT[h])
            P.dma("sp", va[b][:, :, 0:64], D.vmtok[:, h * 64:(h + 1) * 64].re("(c p) d -> p c d", p=128))
            blocks = [(0, 512, list(range(18))), (512, 512, list(range(18))), (1024, 512, list(range(18))), (1536, 512, list(range(18)))]
            if need_ctx:
                blocks.append((2048, 256, [16, 17]))
            items = []
            for (q0, qn, tiles) in blocks:
                for ci, tt_ in enumerate(tiles):
                    items.append((q0, qn, tt_, ci, len(tiles)))

            def mla_S(it, uu):
                q0, qn, tt_, ci, nt_ = it
                P.mm(ps_s[uu % 2][:, :qn], kT[b][:, tt_ * 128:(tt_ + 1) * 128], qT[b][:, q0:q0 + qn])
            mla_S(items[0], u)
            for ii, it in enumerate(items):
                q0, qn, tt_, ci, nt_ = it
                nqi = qn // 128
                s = ps_s[u % 2]
                p_ = pT[u % 3]
                if ii + 1 < len(items):
                    mla_S(items[ii + 1], u + 1)
                u += 1
                P.act(p_[:, :qn], s[:, :qn], AF.Exp, scale=sc)
                for qi in range(nqi):
                    P.mm(ps_o[qi].v(), p_[:, qi * 128:(qi + 1) * 128], va[b][:, tt_, :], start=(ci == 0), stop=(ci == nt_ - 1))
                if ci == nt_ - 1:
                    for qi in range(nqi):
                        P.recip(rec[qi].v(), ps_o[qi][:, 64:65])
                    for qi in range(nqi):
                        P.ts(ytok[:, q0 // 128 + qi, :], ps_o[qi][:, 0:64], rec[qi].v(), None, ALU.mult)
            for g0 in range(0, ntq, 8):
                gn = min(8, ntq - g0)
                for jj in range(gn):
                    P.transpose(ps_t[:, jj, :], ytok[:, g0 + jj, :], D.ident_bf.v())
                P.copy(yT[b][:, g0 * 128:(g0 + gn) * 128], ps_t[:, 0:gn, :], e="dve")
            P.dma("sp", D.ys[2][h * 64:(h + 1) * 64, 0:ntq * 128], yT[b][:, 0:ntq * 128])


def ek(a, b):
    return tuple(f"e{i}" for i in range(a, b))


def rw_shift(P, X, om, hm, out, tmp, tmp2, n_):
    for (a, n) in ((0, T), (T, TC)):
        P.tt(tmp[:n_, a + 1:a + n - 1], X[:n_, a:a + n - 2], X[:n_, a + 2:a + n], ALU.add)
        P.copy(tmp[:n_, a:a + 1], X[:n_, a + 1:a + 2], e="pool")
        P.copy(tmp[:n_, a + n - 1:a + n], X[:n_, a + n - 2:a + n - 1], e="pool")
    P.ts(tmp2[:n_], X[:n_], om, None, ALU.mult)
    P.stt(out[:n_], tmp[:n_], hm, tmp2[:n_], ALU.mult, ALU.add)


def stage_rw(P, D, L, need_ctx, heads=range(16), dbgd=None, on_head=None):
    C0 = math.exp(-0.5)
    nc = P.nc
    NS_ = 4
    with Stage(P) as st:
        masks = [st.sb(f"rwm{i}", [128, 128], F32) for i in range(4)]
        for i in range(4):
            P.dma("sp", masks[i].v(), D.tri_masks[i])
        identf = st.sb("rwid", [128, 128], F32)
        P.dma("sp", identf.v(), D.ident_f.v())
        ones_f = st.sb("rwof", [64, 64], F32)
        P.memset(ones_f.v(), 1.0)
        rmask = st.sb("rwrm", [64, NTILE, 128], BF16)
        P.memset(rmask.v(), 1.0)
        P.memset(rmask[:, :, 0:1], 0.0)

        def col16(name, src):
            t = st.sb(name, [64, 16], F32)
            P.dma("sp", t.v(), src.re("(h p) -> p h", p=64), allow_slow_non_contiguous=True)
            return t

        def col1(name, src, n):
            t = st.sb(name, [n, 1], F32)
            P.dma("sp", t.v(), src.re("(p o) -> p o", o=1), allow_slow_non_contiguous=True)
            return t

        kk_c = col16("ckk", D.rw_k_k[L])
        ka_c = col16("cka", D.rw_k_a[L])
        rk_c = col16("crk", D.rw_r_k[L].re("h d -> (h d)"))
        gnw_c = col16("cgw", D.rw_gn_w[L])
        gnb_c = col16("cgb", D.rw_gn_b[L])
        w0_c = [col16(f"cw0{z}", D.rw_w0[L, z]) for z in range(2)]
        a0_c = [col16(f"ca0{z}", D.rw_a0[L, z]) for z in range(2)]
        omka = st.sb("comka", [64, 16], F32)
        P.ts(omka.v(), ka_c.v(), -1.0, 1.0, ALU.mult, ALU.add)
        mu = D.rw_shift_mu[L]
        mus = {}
        for nm, src, kind in (("r", mu[0:1024], 16), ("k", mu[1024:2048], 16), ("v", mu[2048:3072], 16),
                              ("wd", mu[3072:3200], 128), ("ad", mu[3200:3328], 128), ("gd", mu[3328:3456], 128), ("gd2", mu[3456:3488], 32)):
            m_ = col16("mu" + nm, src) if kind == 16 else col1("mu" + nm, src, kind)
            shp = [64, 16] if kind == 16 else [kind, 1]
            om = st.sb("om" + nm, shp, F32)
            hm = st.sb("hm" + nm, shp, F32)
            P.ts(om.v(), m_.v(), -1.0, 1.0, ALU.mult, ALU.add)
            P.ts(hm.v(), m_.v(), 0.5, None, ALU.mult)
            mus[nm] = (om, hm)

        wup = st.sb("rwwup", [128, 1024], BF16)
        aup = st.sb("rwaup", [128, 1024], BF16)
        gup = st.sb("rwgup", [128, 1024], BF16)
        gup2 = st.sb("rwgup2", [32, 1024], BF16)
        P.dma("pool", wup.v(), D.rw_w_up[L].re("z l c -> (z l) c"))
        P.dma("pool", aup.v(), D.rw_a_up[L].re("z l c -> (z l) c"))
        P.dma("pool", gup.v(), D.rw_g_up[L, 0:128, :])
        P.dma("pool", gup2.v(), D.rw_g_up[L, 128:160, :])

        tA = st.sb("rwtA", [128, NT], BF16)
        tB = st.sb("rwtB", [128, NT], BF16)
        xin = st.sb("rwxin", [128, NT], BF16)
        twd = st.sb("rwtwd", [128, NT], BF16)
        ads = st.sb("rwads", [128, NT], BF16)
        sgd = st.sb("rwsgd", [128, NT], BF16)
        sgd2 = st.sb("rwsgd2", [32, NT], BF16)
        base = OFF_RW
        for (dst, r0, n_, key, fn) in ((twd, 3072, 128, "wd", AF.Tanh), (ads, 3200, 128, "ad", AF.Copy),
                                       (sgd, 3328, 128, "gd", AF.Sigmoid), (sgd2, 3456, 32, "gd2", AF.Sigmoid)):
            P.dma("sp", xin[:n_, :], D.uT[base + r0:base + r0 + n_, :])
            rw_shift(P, xin, mus[key][0].v(), mus[key][1].v(), tA, tA, tB, n_)
            P.act(dst[:n_, :], tA[:n_, :], fn)

        bk = [st.ps(f"rwbk{i}", [128, 512], F32) for i in range(8)]
        r_s = st.sb("rwr", [64, NT], BF16)
        k_s = st.sb("rwk", [64, NT], BF16)
        v_s = st.sb("rwv", [64, NT], BF16)
        g_s = st.sb("rwg", [64, NT], BF16)
        kk = st.sb("rwkk", [64, NT], F32)
        f_sg = st.sb("rwsg", [64, NT], F32)
        f_a = st.sb("rwa", [64, NT], F32)
        f_t = st.sb("rwt", [64, NT], F32)
        f_L = st.sb("rwL", [64, NTILE, 128], F32)
        f_E = st.sb("rwE", [64, NTILE, 128], F32)
        l63 = st.sb("rwl63", [64, NTILE, 1], F32)
        eA = st.sb("rweA", [64, NTILE, 1], F32)
        eB = st.sb("rweB", [64, NTILE, 1], F32)
        scl = st.sb("rwscl", [64, NTILE], F32)
        rT = st.sb("rwrT", [64, NT], BF16)
        kapT = st.sb("rwkapT", [64, NT], BF16)
        ktT = st.sb("rwktT", [64, NT], BF16)
        bT = st.sb("rwbT", [64, NT], BF16)
        tokz = st.sb("rwtokz", [128, NTILE, 3, 64], BF16)
        vtok = st.sb("rwvtok", [128, NTILE, 64], BF16)
        MA = st.sb("rwMA", [64, NTILE, 192], BF16)
        MB = st.sb("rwMB", [128, NTILE, 192], BF16)
        yacc = st.sb("rwy", [64, NT], F32)
        Nsb = [st.sb(f"rwN{i}", [128, 128], F32) for i in range(NS_)]
        NTsb = [st.sb(f"rwNT{i}", [128, 128], F32) for i in range(NS_)]
        PP = [[st.sb(f"rwPP{i}_{j}", [128, 2, 128], F32) for j in range(2)] for i in range(NS_)]
        ZZ = [[st.sb(f"rwZ{i}_{j}", [128, 192], F32) for j in range(2)] for i in range(NS_)]
        rc = [st.sb(f"rwrc{i}", [128, 192], F32) for i in range(NS_)]
        akr = [st.sb(f"rwakr{i}", [128, 128], F32) for i in range(NS_)]
        ST = [st.sb(f"rwST{i}", [64, 64], BF16) for i in range(2)]
        idb = D.ident_bf

        for h in heads:
            hc = slice(h, h + 1)
            if on_head is not None:
                on_head(h)
            for (dst, r0, key) in ((r_s, 0, "r"), (k_s, 1024, "k"), (v_s, 2048, "v")):
                P.dma("sp", xin[:64, :], D.uT[base + r0 + h * 64:base + r0 + (h + 1) * 64, :])
                rw_shift(P, xin, mus[key][0][:, hc], mus[key][1][:, hc], dst, tA, tB, 64)
            for ci, (t0, tn) in enumerate(TOKCH):
                ps = bk[ci % 8]
                P.mmg(ps[:64, :tn], [(gup[:, h * 64:(h + 1) * 64], sgd[:, t0:t0 + tn]), (gup2[:32, h * 64:(h + 1) * 64], sgd2[:32, t0:t0 + tn])])
                P.copy(g_s[:, t0:t0 + tn], ps[:64, :tn], e="act")
            P.ts(f_t.v(), k_s.v(), kk_c[:, hc], None, ALU.mult)
            P.act(f_a.v(), f_t.v(), AF.Square)
            for ci, (t0, tn) in enumerate(TOKCH):
                ps = bk[(ci + 5) % 8]
                P.mm(ps[:64, :tn], ones_f.v(), f_a[:, t0:t0 + tn])
                P.act(f_sg[:, t0:t0 + tn], ps[:64, :tn], AF.Sqrt)
            P.ts(f_sg.v(), f_sg.v(), 1e-12, None, ALU.max)
            P.recip(f_sg.v(), f_sg.v())
            P.tt(kk.v(), f_t.v(), f_sg.v(), ALU.mult)
            for c in range(NTILE):
                pb = bk[c % 8].v().bc(BF16)
                P.transpose(pb[:, 0:64], v_s[:, c * 128:(c + 1) * 128], idb[0:64, 0:64])
                P.copy(vtok[:, c, :], pb[:, 0:64], e="act")

            for z in range(2):
                zs = slice(z * 64, (z + 1) * 64)
                mS, mST, mD = (masks[0], masks[1], masks[2]) if z == 0 else (masks[1], masks[0], masks[3])
                for ci, (t0, tn) in enumerate(TOKCH):
                    ps = bk[ci % 8]
                    P.mm(ps[:64, :tn], wup[zs, h * 64:(h + 1) * 64], twd[zs, t0:t0 + tn])
                    P.act(f_sg[:, t0:t0 + tn], ps[:64, :tn], AF.Sigmoid, bias=w0_c[z][:, hc])
                    ps2 = bk[(ci + 4) % 8]
                    P.mm(ps2[:64, :tn], aup[zs, h * 64:(h + 1) * 64], ads[zs, t0:t0 + tn])
                    P.act(f_a[:, t0:t0 + tn], ps2[:64, :tn], AF.Sigmoid, bias=a0_c[z][:, hc])
                P.ts(f_t.v(), f_a.v(), ka_c[:, hc], omka[:, hc], ALU.mult, ALU.add)
                P.tt(f_t.v(), f_t.v(), k_s.v(), ALU.mult)
                P.tt(f_a.v(), f_a.v(), kk.v(), ALU.mult)
                L2 = f_L.v().re("p c t -> p (c t)")
                P.op("dve", lambda: nc.vector.tensor_tensor_scan(L2.ap, rmask.v().re("p c t -> p (c t)").ap, f_sg.ap() , 0.0, ALU.mult, ALU.add),
                     [rmask.v(), f_sg.v()], [f_L.v()])
                P.copy(l63.v(), f_L[:, :, 63:64])
                P.tt(f_L.v(), f_L.v(), l63.v().m(lambda a: a.broadcast_to([64, NTILE, 128])), ALU.subtract)
                P.tt(f_sg.v(), L2, f_sg.v(), ALU.subtract)
                E2d = f_E.v().re("p c t -> p (c t)")
                P.act(E2d, L2, AF.Exp, scale=-C0)
                P.copy(eA.v(), f_E[:, :, 127:128])
                if z == 0:
                    P.tt(rT.v(), r_s.v(), E2d, ALU.mult)
                P.act(E2d, L2, AF.Exp, scale=C0)
                if z == 0:
                    P.tt(ktT.v(), f_t.v(), E2d, ALU.mult)
                    P.tt(bT.v(), f_a.v(), E2d, ALU.mult)
                else:
                    P.tt(kapT.v(), kk.v(), E2d, ALU.mult)
                P.act(E2d, f_sg.v(), AF.Exp, scale=-C0)
                if z == 0:
                    P.tt(kapT.v(), kk.v(), E2d, ALU.mult)
                else:
                    P.tt(ktT.v(), f_t.v(), E2d, ALU.mult)
                    P.tt(bT.v(), f_a.v(), E2d, ALU.mult)
                P.act(E2d, f_sg.v(), AF.Exp, scale=C0)
                P.copy(eB.v(), f_E[:, :, 0:1])
                if z == 1:
                    P.tt(rT.v(), r_s.v(), E2d, ALU.mult)
                eA2 = eA.v().re("p c o -> p (c o)")
                eB2 = eB.v().re("p c o -> p (c o)")
                P.memset(scl.v(), 1.0)
                if z == 0:
                    P.tt(scl[:, 0:15], eA2[:, 0:15], eB2[:, 1:16], ALU.mult)
                    P.tt(scl[:, 16:17], eA2[:, 16:17], eB2[:, 17:18], ALU.mult)
                    P.tt(scl[:, 17:18], eA2[:, 17:18], eB2[:, 0:1], ALU.mult)
                else:
                    P.tt(scl[:, 1:18], eB2[:, 1:18], eA2[:, 0:17], ALU.mult)
                for c in range(NTILE):
                    cs_ = slice(c * 128, (c + 1) * 128)
                    pb = bk[c % 8].v().bc(BF16)
                    P.transpose(pb[:, 0:64], kapT[:, cs_], idb[0:64, 0:64])
                    P.transpose(pb[:, 64:128], ktT[:, cs_], idb[0:64, 0:64])
                    P.transpose(pb[:, 128:192], bT[:, cs_], idb[0:64, 0:64])
                    P.copy(tokz[:, c, :, :], pb[:, 0:192].re("p (a d) -> p a d", a=3), e=("act" if c % 2 else "dve"))
                for c0 in range(0, NTILE, NS_):
                    units = list(range(c0, min(NTILE, c0 + NS_)))
                    for s, c in enumerate(units):
                        cs_ = slice(c * 128, (c + 1) * 128)
                        b = bk[s]
                        P.mm(b[:, 0:128].k(ek(0, 2)), bT[:, cs_], kapT[:, cs_])
                        P.mm(b[:, 128:256].k(ek(2, 4)), kapT[:, cs_], bT[:, cs_])
                        P.mm(b[:, 256:384].k(ek(4, 6)), kapT[:, cs_], ktT[:, cs_])
                        P.mm(b[:, 384:512].k(ek(6, 8)), bT[:, cs_], rT[:, cs_])
                        P.tt(Nsb[s].v(), b[:, 0:128].k(ek(0, 2)), mS.v(), ALU.mult)
                        P.tt(NTsb[s].v(), b[:, 128:256].k(ek(2, 4)), mST.v(), ALU.mult)
                        P.tt(ZZ[s][0][:, 64:192], b[:, 256:384].k(ek(4, 6)), mST.v(), ALU.mult)
                        P.tt(rc[s][:, 0:128], b[:, 384:512].k(ek(6, 8)), mD.v(), ALU.mult)
                        P.copy(ZZ[s][0][:, 0:64], tokz[:, c, 0, :], e="pool")
                        P.copy(rc[s][:, 128:192], tokz[:, c, 2, :], e="pool")
                    for s, c in enumerate(units):
                        b = bk[s]
                        P.mm(b[:, 256:448].k(ek(4, 7)), Nsb[s].v(), ZZ[s][0].v())
                        P.mm(b[:, 0:128].k(ek(0, 2)), NTsb[s].v(), Nsb[s].v())
                        P.mm(b[:, 128:256].k(ek(2, 4)), Nsb[s].v(), NTsb[s].v())
                        P.tt(ZZ[s][1].v(), ZZ[s][0].v(), b[:, 256:448].k(ek(4, 7)), ALU.subtract)
                        P.copy(PP[s][0].v(), b[:, 0:256].k(ek(0, 4)), e="act")
                    for j in range(1, 7):
                        for s, c in enumerate(units):
                            b = bk[s]
                            pc = PP[s][(j - 1) % 2]
                            pn = PP[s][j % 2]
                            zc = ZZ[s][j % 2]
                            zn = ZZ[s][(j + 1) % 2]
                            P.mm(b[:, 256:448].k(ek(4, 7)), pc[:, 0, :], zc.v())
                            if j < 6:
                                P.mm(b[:, 0:128].k(ek(0, 2)), pc[:, 1, :], pc[:, 0, :])
                                if j < 5:
                                    P.mm(b[:, 128:256].k(ek(2, 4)), pc[:, 0, :], pc[:, 1, :])
                            P.tt(zn.v(), zc.v(), b[:, 256:448].k(ek(4, 7)), ALU.add)
                            if j < 5:
                                P.copy(pn.v(), b[:, 0:256].k(ek(0, 4)), e="act")
                            elif j == 5:
                                P.copy(pn[:, 0, :], b[:, 0:128].k(ek(0, 2)), e="act")
                    for s, c in enumerate(units):
                        cs_ = slice(c * 128, (c + 1) * 128)
                        b = bk[s]
                        zf = ZZ[s][1]
                        P.mm(b[:64, 0:192].k(ek(0, 3)), zf[:, 0:64], rc[s].v())
                        P.mm(b[:, 192:384].k(ek(3, 6)), zf[:, 64:192], rc[s].v())
                        P.mm(b[:, 384:512].k(ek(6, 8)), ktT[:, cs_], rT[:, cs_])
                        P.tt(akr[s].v(), b[:, 384:512].k(ek(6, 8)), mD.v(), ALU.mult)
                        P.tt(MA[:, c, 0:128], rT[:, cs_], b[:64, 0:128].k(ek(0, 2)), ALU.subtract)
                        P.tt(MA[:, c, 128:192], identf[0:64, 0:64], b[:64, 128:192].k(ek(2, 3)), ALU.subtract)
                        P.tt(MB[:, c, 0:128], akr[s].v(), b[:, 192:320].k(ek(3, 5)), ALU.subtract)
                        P.tt(MB[:, c, 128:192], tokz[:, c, 1, :], b[:, 320:384].k(ek(5, 6)), ALU.subtract)
                order = ([16, 17] + list(range(16))) if z == 0 else ([17, 16] + list(range(15, -1, -1)))
                P.memset(ST[0].v(), 0.0)
                for i, c in enumerate(order):
                    cur = ST[i % 2]
                    nxt = ST[(i + 1) % 2]
                    bY = bk[(2 * i) % 8]
                    bS = bk[(2 * i + 1) % 8]
                    P.mmg(bY[:64, 0:128].k(ek(0, 2)), [(cur.v(), MA[:, c, 0:128]), (vtok[:, c, :], MB[:, c, 0:128])])
                    P.mmg(bS[:64, 0:64].k(ek(0, 1)), [(MA[:, c, 128:192], cur.v()), (MB[:, c, 128:192], vtok[:, c, :])])
                    if i < NTILE - 1:
                        P.ts(nxt.v(), bS[:64, 0:64].k(ek(0, 1)), scl[:, c:c + 1], None, ALU.mult)
                    ysl = yacc[:, c * 128:(c + 1) * 128]
                    if z == 0:
                        P.copy(ysl, bY[:64, 0:128].k(ek(0, 2)), e="act")
                    else:
                        P.tt(ysl, ysl, bY[:64, 0:128].k(ek(0, 2)), ALU.add)
            if dbgd is not None:
                P.dma("sp", dbgd[h * 64:(h + 1) * 64, :], yacc.v())
            E2o = f_E.v().re("p c t -> p (c t)")
            yo = rT
            L2o = f_L.v().re("p c t -> p (c t)")
            for ci, (t0, tn) in enumerate(TOKCH):
                ts_ = slice(t0, t0 + tn)
                b1, b2, b3 = bk[(3 * ci) % 8], bk[(3 * ci + 1) % 8], bk[(3 * ci + 2) % 8]
                P.act(L2o[:, 0:tn], yacc[:, ts_], AF.Square)
                P.mm(b1[:64, :tn], ones_f.v(), yacc[:, ts_])
                P.mm(b2[:64, :tn], ones_f.v(), L2o[:, 0:tn])
                P.stt(L2o[:, 512:512 + tn], r_s[:, ts_], rk_c[:, hc], k_s[:, ts_], ALU.mult, ALU.mult)
                P.mm(b3[:64, :tn], ones_f.v(), L2o[:, 512:512 + tn])
                mean = E2o[:, 0:tn]
                var = E2o[:, 512:512 + tn]
                dd = E2o[:, 1024:1024 + tn]
                P.ts(mean, b1[:64, :tn], 1.0 / 64, None, ALU.mult)
                P.tt(var, mean, mean, ALU.mult)
                P.stt(var, b2[:64, :tn], 1.0 / 64, var, ALU.mult, ALU.subtract)
                P.ts(var, var, 64e-5, None, ALU.add)
                P.act(var, var, AF.Sqrt)
                P.recip(var, var)
                P.tt(dd, yacc[:, ts_], mean, ALU.subtract)
                P.tt(dd, dd, var, ALU.mult)
                P.ts(dd, dd, gnw_c[:, hc], gnb_c[:, hc], ALU.mult, ALU.add)
                P.tt(var, b3[:64, :tn], v_s[:, ts_], ALU.mult)
                P.tt(dd, dd, var, ALU.add)
                P.tt(yo[:, ts_], dd, g_s[:, ts_], ALU.mult)
            P.dma("sp", D.ys[1][h * 64:(h + 1) * 64, :], yo.v())

def stage_merge(P, D, L, need_ctx):
    chunks = TOKCH if need_ctx else TOKCH[:4]
    ntile = NTILE if need_ctx else 16
    with Stage(P) as st:
        mT = st.sb("mgT", [128, 16, NT], BF16)
        with Stage(P) as sa:
            Y = [sa.sb(f"mgY{i}", [128, 24, 512], BF16) for i in range(2)]
            wb = [sa.sb(f"mgw{i}", [128, 3, 8, 512], BF16) for i in range(2)]
            gt = [sa.sb(f"mgg{i}", [128, 3, 512], BF16) for i in range(2)]
            sg = [sa.sb(f"mgs{i}", [128, 3, 512], F32) for i in range(2)]
            t0_ = sa.sb("mgt0", [128, 512], F32)
            t1_ = sa.sb("mgt1", [128, 512], F32)
            ps = [sa.ps(f"mgp{i}", [128, 512], F32) for i in range(6)]
            u = 0
            uw = 0
            for ci, (t0, tn) in enumerate(chunks):
                y = Y[ci % 2]
                for z in range(3):
                    P.dma("sp", y[:, z * 8:(z + 1) * 8, :tn], D.ys[z][:, t0:t0 + tn].re("(kc p) t -> p kc t", p=128))
                for ob4 in range(4):
                    w = wb[uw % 2]
                    uw += 1
                    for z in range(3):
                        P.dma("act", w[:, z, :, :], D.wbr_bf[L][z][:, ob4 * 512:(ob4 + 1) * 512].re("(kc p) n -> p kc n", p=128))
                    for o4 in range(4):
                        ob = ob4 * 4 + o4
                        g = gt[u % 2]
                        s_ = sg[u % 2]
                        pp = ps[(u % 2) * 3:(u % 2) * 3 + 3]
                        u += 1
                        for z in range(3):
                            r0 = OFF_G + z * DM + ob * 128
                            P.dma("sp", g[:, z, :tn], D.uT[r0:r0 + 128, t0:t0 + tn])
                        P.act(s_[:, :, :tn], g[:, :, :tn], AF.Sigmoid)
                        for z in range(3):
                            P.mmg(pp[z][:, :tn], [(w[:, z, kc, o4 * 128:(o4 + 1) * 128], y[:, z * 8 + kc, :tn]) for kc in range(8)])
                        P.tt(t0_[:, :tn], pp[0][:, :tn], s_[:, 0, :tn], ALU.mult)
                        P.tt(t1_[:, :tn], pp[1][:, :tn], s_[:, 1, :tn], ALU.mult)
                        P.tt(t0_[:, :tn], t0_[:, :tn], t1_[:, :tn], ALU.add)
                        P.tt(t1_[:, :tn], pp[2][:, :tn], s_[:, 2, :tn], ALU.mult)
                        P.tt(mT[:, ob, t0:t0 + tn], t0_[:, :tn], t1_[:, :tn], ALU.add)
        with Stage(P) as sb_:
            wo = [sb_.sb(f"mow{i}", [128, 16, 512], BF16) for i in range(2)]
            g1 = [[sb_.sb(f"mog{i}_{r}", [128, 512], F32) for r in range(2)] for i in range(2)]
            xt = [sb_.sb(f"mox{i}", [128, 512], F32) for i in range(3)]
            tm = [sb_.sb(f"mot{i}", [128, 512], F32) for i in range(2)]
            ps = [sb_.ps(f"mop{i}", [128, 512], F32) for i in range(4)]
            u = 0
            for s in range(4):
                cs_ = slice(s * 512, (s + 1) * 512)
                w = wo[s % 2]
                P.dma("sp", w.v(), D.wout_bf[L][:, cs_].re("(kc p) n -> p kc n", p=128))
                for r in range(2):
                    P.dma("sp", g1[s % 2][r].v(), bc_row(D.modv[L][r:r + 1, 2 * DM + s * 512:2 * DM + (s + 1) * 512]))
                for t in range(ntile):
                    r = 0 if t < 16 else 1
                    x = xt[u % 3]
                    tmp = tm[u % 2]
                    p = ps[u % 4]
                    u += 1
                    P.dma("sp", x.v(), D.xcur[t * 128:(t + 1) * 128, cs_])
                    P.mmg(p.v(), [(mT[:, kc, t * 128:(t + 1) * 128], w[:, kc, :]) for kc in range(16)])
                    P.tt(tmp.v(), p.v(), g1[s % 2][r].v(), ALU.mult)
                    P.tt(x.v(), x.v(), tmp.v(), ALU.add)
                    P.dma("sp", D.xcur[t * 128:(t + 1) * 128, cs_], x.v())


def stage_norm2_router(P, D, L, need_ctx):
    ntile = NTILE if need_ctx else 16
    with Stage(P) as st:
        h2T = st.sb("h2T", [128, 16, NT], BF16)
        stage_norm(P, D, L, 1, h2T, st)
        P.dma("sp", D.h2Td.v().re("(c p) t -> p c t", p=128), h2T.v())
        rw = st.sb("rtw", [128, 16, 16], BF16)
        P.dma("pool", rw.v(), D.router_w.v().re("(kc p) e -> p kc e", p=128))
        rb = st.sb("rtb", [128, 16], F32)
        P.dma("sp", rb.v(), bc_row(D.router_bias.v().re("(o e) -> o e", o=1)))
        identf = st.sb("rtid", [128, 128], F32)
        P.dma("sp", identf.v(), D.ident_f.v())
        wT = st.sb("rtwT", [16, NT], F32)
        ps = [st.ps(f"rtp{i}", [128, 16], F32) for i in range(2)]
        pst = [st.ps(f"rtq{i}", [16, 128], F32) for i in range(2)]

        def tl(n, shape):
            return [st.sb(f"{n}{i}", shape, F32) for i in range(2)]
        sc, sel, eq, s2, m1, m2, grp, gm, geq, t1, oh1, t2, oh2, den = (tl("rsc", [128, 16]), tl("rsel", [128, 16]), tl("req", [128, 16]), tl("rs2", [128, 16]),
                                                                     tl("rm1", [128, 4]), tl("rm2", [128, 4]), tl("rgrp", [128, 4]), tl("rgm", [128, 1]), tl("rgeq", [128, 4]),
                                                                     tl("rt1", [128, 1]), tl("roh1", [128, 16]), tl("rt2", [128, 1]), tl("roh2", [128, 16]), tl("rden", [128, 1]))
        BIG = 1.0e9

        def g4(v):
            return v.re("p (g e) -> p g e", e=4)

        def b4(v):
            return v.m(lambda a: a.unsqueeze(2).broadcast_to([128, 4, 4]))

        def b16(v):
            return v.m(lambda a: a.broadcast_to([128, 16]))
        for t in range(ntile):
            i = t % 2
            P.mmg(ps[i].v(), [(h2T[:, kc, t * 128:(t + 1) * 128], rw[:, kc, :]) for kc in range(16)])
            P.act(sc[i].v(), ps[i].v(), AF.Sigmoid)
            P.tt(sel[i].v(), sc[i].v(), rb.v(), ALU.add)
            P.reduce(m1[i].v(), g4(sel[i].v()), ALU.max)
            P.tt(g4(eq[i].v()), g4(sel[i].v()), b4(m1[i].v()), ALU.is_equal)
            P.stt(s2[i].v(), eq[i].v(), -BIG, sel[i].v(), ALU.mult, ALU.add)
            P.reduce(m2[i].v(), g4(s2[i].v()), ALU.max)
            P.tt(grp[i].v(), m1[i].v(), m2[i].v(), ALU.add)
            P.reduce(gm[i].v(), grp[i].v(), ALU.max)
            P.ts(geq[i].v(), grp[i].v(), gm[i].v(), None, ALU.is_equal)
            P.ts(geq[i].v(), geq[i].v(), BIG, -BIG, ALU.mult, ALU.add)
            P.tt(g4(s2[i].v()), g4(sel[i].v()), b4(geq[i].v()), ALU.add)
            P.reduce(t1[i].v(), s2[i].v(), ALU.max)
            P.ts(oh1[i].v(), s2[i].v(), t1[i].v(), None, ALU.is_equal)
            P.stt(eq[i].v(), oh1[i].v(), -BIG, s2[i].v(), ALU.mult, ALU.add)
            P.reduce(t2[i].v(), eq[i].v(), ALU.max)
            P.ts(oh2[i].v(), eq[i].v(), t2[i].v(), None, ALU.is_equal)
            P.tt(oh1[i].v(), oh1[i].v(), oh2[i].v(), ALU.add)
            P.tt(oh1[i].v(), oh1[i].v(), sc[i].v(), ALU.mult)
            P.reduce(den[i].v(), oh1[i].v(), ALU.add)
            P.recip(den[i].v(), den[i].v())
            P.ts(oh1[i].v(), oh1[i].v(), den[i].v(), None, ALU.mult)
            P.transpose(pst[i].v(), oh1[i].v(), identf.v())
            P.copy(wT[:, t * 128:(t + 1) * 128], pst[i].v(), e="act")
        P.dma("sp", D.wgtT[:, 0:ntile * 128], wT[:, 0:ntile * 128])


def stage_moe(P, D, L, need_ctx):
    ntok = NT if need_ctx else T
    passes = []
    p0 = 0
    while p0 < ntok:
        pn = min(768, ntok - p0)
        passes.append((p0, pn))
        p0 += pn
    with Stage(P) as st:
        identf = st.sb("moid", [128, 128], F32)
        P.dma("sp", identf.v(), D.ident_f.v())
        selE = st.sb("mosel", [16, 16, 128], F32)
        P.dma("sp", selE.v(), D.selE.v())
        wT = st.sb("mowT", [16, NT], F32)
        P.dma("sp", wT[:, 0:ntok], D.wgtT[:, 0:ntok])
        g2c = [st.sb(f"mog2{r}", [128, 16], F32) for r in range(2)]
        for r in range(2):
            P.dma("sp", g2c[r].v(), D.modv[L][r, 5 * DM:6 * DM].re("(o p) -> p o", p=128), allow_slow_non_contiguous=True)
        h2 = st.sb("moh2", [128, 16, 768], BF16)
        facc = st.sb("mofacc", [128, 16, 768], F32)
        act_ = st.sb("moact", [128, 8, 768], BF16)
        wbc = st.sb("mowbc", [128, 768], F32)
        wgu = [st.sb(f"mowgu{i}", [128, 16, 2, 256], BF16) for i in range(3)]
        wd = [st.sb(f"mowd{i}", [128, 8, 512], BF16) for i in range(2)]
        sgt = [st.sb(f"mosg{i}", [128, 384], F32) for i in range(2)]
        tt_ = [st.sb(f"mott{i}", [128, 384], F32) for i in range(2)]
        xt = [st.sb(f"moxt{i}", [128, DM], F32) for i in range(2)]
        ps = [st.ps(f"mop{i}", [128, 512], F32) for i in range(8)]
        ug = 0
        ud = 0
        pi = 0
        for (p0, pn) in passes:
            chunks = [(c0, min(384, pn - c0)) for c0 in range(0, pn, 384)]
            P.dma("sp", h2[:, :, :pn], D.h2Td[:, p0:p0 + pn].re("(c p) t -> p c t", p=128))
            for e in range(16):
                for (c0, cn) in chunks:
                    p = ps[pi % 8]
                    pi += 1
                    P.mm(p[:, :cn], selE[:, e, :], wT[:, p0 + c0:p0 + c0 + cn])
                    P.copy(wbc[:, c0:c0 + cn], p[:, :cn], e="act")
                for s2_ in range(4):
                    w = wgu[ug % 3]
                    ug += 1
                    for gu in range(2):
                        P.dma("sp", w[:, :, gu, :],
                              D.wgu_bf[L][e][:, gu * 1024 + s2_ * 256:gu * 1024 + (s2_ + 1) * 256].re("(kc p) n -> p kc n", p=128))
                    for b2 in range(2):
                        blk = s2_ * 2 + b2
                        for ci, (c0, cn) in enumerate(chunks):
                            pg = ps[pi % 8]
                            pu = ps[(pi + 1) % 8]
                            pi += 2
                            P.mmg(pg[:, :cn], [(w[:, kc, 0, b2 * 128:(b2 + 1) * 128], h2[:, kc, c0:c0 + cn]) for kc in range(16)])
                            P.mmg(pu[:, :cn], [(w[:, kc, 1, b2 * 128:(b2 + 1) * 128], h2[:, kc, c0:c0 + cn]) for kc in range(16)])
                            sg_ = sgt[ci % 2]
                            t_ = tt_[ci % 2]
                            P.act(sg_[:, :cn], pg[:, :cn], AF.Silu)
                            P.tt(t_[:, :cn], pu[:, :cn], sg_[:, :cn], ALU.mult)
                            P.tt(act_[:, blk, c0:c0 + cn], t_[:, :cn], wbc[:, c0:c0 + cn], ALU.mult)
                for s4 in range(4):
                    w = wd[ud % 2]
                    ud += 1
                    P.dma("sp", w.v(), D.wd_bf[L][e][:, s4 * 512:(s4 + 1) * 512].re("(kc p) n -> p kc n", p=128))
                    for o4 in range(4):
                        ob = s4 * 4 + o4
                        for (c0, cn) in chunks:
                            p = ps[pi % 8]
                            pi += 1
                            P.mmg(p[:, :cn], [(w[:, kc, o4 * 128:(o4 + 1) * 128], act_[:, kc, c0:c0 + cn]) for kc in range(8)])
                            if e == 0:
                                P.copy(facc[:, ob, c0:c0 + cn], p[:, :cn], e="act")
                            else:
                                P.tt(facc[:, ob, c0:c0 + cn], facc[:, ob, c0:c0 + cn], p[:, :cn], ALU.add)
            for ob in range(16):
                lat = max(0, min(pn, T - p0))
                if lat > 0:
                    P.ts(facc[:, ob, 0:lat], facc[:, ob, 0:lat], g2c[0][:, ob:ob + 1], None, ALU.mult)
                if lat < pn:
                    P.ts(facc[:, ob, lat:pn], facc[:, ob, lat:pn], g2c[1][:, ob:ob + 1], None, ALU.mult)
            for ti in range(pn // 128):
                t = p0 // 128 + ti
                x = xt[ti % 2]
                P.dma("sp", x.v(), D.xcur[t * 128:(t + 1) * 128, :])
                for o4 in range(4):
                    p = ps[pi % 8]
                    pi += 1
                    for j in range(4):
                        ob = o4 * 4 + j
                        P.transpose(p[:, j * 128:(j + 1) * 128], facc[:, ob, ti * 128:(ti + 1) * 128], identf.v())
                    P.tt(x[:, o4 * 512:(o4 + 1) * 512], x[:, o4 * 512:(o4 + 1) * 512], p.v(), ALU.add)
                P.dma("sp", D.xcur[t * 128:(t + 1) * 128, :], x.v())


def stage_final(P, D):
    with Stage(P) as st:
        g = st.sb("fng", [128, DM], F32)
        P.dma("sp", g.v(), bc_row(D.final_norm_g.v().re("(o d) -> o d", o=1)))
        xt = [st.sb(f"fnx{i}", [128, DM], F32) for i in range(2)]
        sq = st.sb("fnsq", [128, DM], F32)
        ot = [st.sb(f"fno{i}", [128, DM], F32) for i in range(2)]
        ss = [st.sb(f"fns{i}", [128, 1], F32) for i in range(2)]
        for t in range(16):
            x = xt[t % 2]
            s = ss[t % 2]
            o = ot[t % 2]
            P.dma("sp", x.v(), D.xcur[t * 128:(t + 1) * 128, :])
            P.act(sq.v(), x.v(), AF.Square, accum=s.v())
            P.ts(s.v(), s.v(), 1.0 / DM, 1e-6, ALU.mult, ALU.add)
            P.act(s.v(), s.v(), AF.Sqrt)
            P.recip(s.v(), s.v())
            P.stt(o.v(), x.v(), s.v(), g.v(), ALU.mult, ALU.mult)
            P.dma("sp", D.out[t * 128:(t + 1) * 128, :], o.v())

ORDER = ["mod", "norm1", "win", "na", "mlaprep", "mla", "rw", "merge", "norm2", "moe"]


def build(L_list=(0, 1), upto=None, dbg=(), skip=(), rw_heads=range(16)):
    nc = bass.Bass("TRN2", target_bir_lowering=False)
    P = Prog(nc)
    D = NS()

    def inp(name, shape, dt=F32):
        b = dram(nc, name, shape, dt, kind="ExternalInput")
        setattr(D, name, b)
        return b

    inp("xin", [NT, DM]); inp("cT", [128, 16, 2])
    inp("w_mod", [2, DM, 6 * DM]); inp("b_mod", [2, 6 * DM])
    inp("norm1_g", [2, DM]); inp("norm2_g", [2, DM])
    inp("w_in", [2, DM, IN_W])
    inp("ident_f", [128, 128])
    inp("na_bias", [2, 16, 128, 5, 5, 128])
    inp("rope_c", [32, NT]); inp("rope_s", [32, NT]); inp("perm96", [96, 96])
    inp("mla_q_norm_g", [2, 512]); inp("mla_kv_norm_g", [2, 256])
    inp("mla_w_uq", [2, 512, 1536]); inp("mla_w_ukv", [2, 256, 2048])
    inp("tri_masks", [4, 128, 128])
    inp("rw_shift_mu", [2, 3488]); inp("rw_w0", [2, 2, 1024]); inp("rw_w_up", [2, 2, 64, 1024])
    inp("rw_a0", [2, 2, 1024]); inp("rw_a_up", [2, 2, 64, 1024]); inp("rw_g_up", [2, 160, 1024])
    inp("rw_k_k", [2, 1024]); inp("rw_k_a", [2, 1024]); inp("rw_r_k", [2, 16, 64])
    inp("rw_gn_w", [2, 1024]); inp("rw_gn_b", [2, 1024])
    inp("w_branch", [2, 3, 1024, DM]); inp("w_out", [2, DM, DM])
    inp("router_w", [DM, 16]); inp("router_bias", [16])
    inp("moe_w_gate_up", [2, 16, DM, 2048]); inp("moe_w_down", [2, 16, 1024, DM])
    inp("final_norm_g", [DM]); inp("selE", [16, 16, 128])
    D.out = dram(nc, "out", [T, DM], F32, kind="ExternalOutput")

    def scratch(name, shape, dt):
        kind = "ExternalOutput" if name in dbg else "Internal"
        b = dram(nc, name, shape, dt, kind=kind)
        setattr(D, name, b)
        return b

    scratch("xcur", [NT, DM], F32)
    D.modv = [scratch(f"modv{l}", [2, 6 * DM], F32) for l in range(2)]
    scratch("uT", [IN_WP, NT], BF16)
    scratch("utok", [NT, 1024], BF16)
    scratch("hTd", [DM, NT], BF16)
    D.ys = [scratch(f"ys{z}", [1024, NT], BF16) for z in range(3)]
    scratch("qmT", [16, 96, NT], BF16)
    scratch("kmT", [16, 96, NT], BF16)
    scratch("vmtok", [NT, 1024], BF16)
    scratch("yscan", [1024, NT], F32)
    scratch("h2Td", [DM, NT], BF16)
    D.wgu_bf = [[scratch(f"wgubf{l}_{e}", [DM, 2048], BF16) for e in range(16)] for l in range(2)]
    D.wd_bf = [[scratch(f"wdbf{l}_{e}", [1024, DM], BF16) for e in range(16)] for l in range(2)]
    D.wbr_bf = [[scratch(f"wbrbf{l}_{z}", [1024, DM], BF16) for z in range(3)] for l in range(2)]
    D.wout_bf = [scratch(f"woutbf{l}", [DM, DM], BF16) for l in range(2)]

    def emit_casts(L, h):
        for i in range(4):
            P.dma("pool", D.wgu_bf[L][h].part(i)[i * 512:(i + 1) * 512, :], D.moe_w_gate_up[L, h, i * 512:(i + 1) * 512, :], bg=True)
        for i in range(2):
            P.dma("pool", D.wd_bf[L][h].part(i)[i * 512:(i + 1) * 512, :], D.moe_w_down[L, h, i * 512:(i + 1) * 512, :], bg=True)
        if h < 3:
            for i in range(2):
                P.dma("pool", D.wbr_bf[L][h].part(i)[i * 512:(i + 1) * 512, :], D.w_branch[L, h, i * 512:(i + 1) * 512, :], bg=True)
        elif h < 7:
            i = h - 3
            P.dma("pool", D.wout_bf[L].part(i)[i * 512:(i + 1) * 512, :], D.w_out[L, i * 512:(i + 1) * 512, :], bg=True)
    scratch("wgtT", [16, NT], F32)

    def stop(name):
        return upto is not None and ORDER.index(name) >= ORDER.index(upto)

    with Stage(P) as st0:
        ident_bf = st0.sb("ident_bf", [128, 128], BF16)
        D.ident_bf = ident_bf
        P.dma("pool", ident_bf.v(), D.ident_f.v())
        for i in range(6):
            P.dma("sp", D.xcur[i * 384:(i + 1) * 384, :], D.xin[i * 384:(i + 1) * 384, :])
        for L in L_list:
            need_ctx = (L == 0)
            if "mod" not in skip:
                stage_mod(P, D, L)
                P.mark("stage_mod")
            if stop("mod"):
                break
            if "win" not in skip:
                with Stage(P) as stl:
                    hT = stl.sb("hT", [128, 16, NT], BF16)
                    stage_norm(P, D, L, 0, hT, stl)
                    P.mark("stage_norm")
                    if "hTd" in dbg:
                        P.dma("sp", D.hTd.v().re("(c p) t -> p c t", p=128), hT.v())
                    stage_win(P, D, L, hT)
                    P.mark("stage_win")
            if stop("win"):
                break
            if "na" not in skip:
                stage_na(P, D, L, need_ctx)
                P.mark("stage_na")
            if stop("na"):
                break
            if "mla" not in skip:
                stage_mla_prep(P, D, L, need_ctx)
                P.mark("stage_mla_prep")
                stage_mla_attn(P, D, L, need_ctx)
                P.mark("stage_mla_attn")
            if stop("mla"):
                break
            if "rw" not in skip:
                stage_rw(P, D, L, need_ctx, heads=rw_heads, dbgd=(D.yscan if "yscan" in dbg else None), on_head=(lambda h, L=L: emit_casts(L, h)))
                P.mark("stage_rw")
            if stop("rw"):
                break
            if "merge" not in skip:
                stage_merge(P, D, L, need_ctx)
                P.mark("stage_merge")
            if stop("merge"):
                break
            if "moe" not in skip:
                stage_norm2_router(P, D, L, need_ctx)
                P.mark("stage_norm2_router")
                stage_moe(P, D, L, need_ctx)
                P.mark("stage_moe")
            if stop("moe"):
                break
        else:
            stage_final(P, D)
            P.mark("stage_final")
        P.drain()
    print("ninst", P.ninst, {e: P.etot[e] for e in P.eng})
    build.marks = P.marks
    return nc


def host_consts():
    f32 = np.float32
    c = {}
    c["ident_f"] = np.eye(128, dtype=f32)
    nf = 8
    inv = (10000.0 ** (-np.arange(nf, dtype=np.float32) / nf)).astype(np.float32)
    pos = np.arange(T)
    row = (pos // 64).astype(np.float32)
    col = (pos % 64).astype(np.float32)
    rc = np.ones((32, NT), f32)
    rs = np.zeros((32, NT), f32)
    for d in range(32):
        p = row if d < 16 else col
        dd = d % 16
        f = dd % 8
        ang = (p * inv[f]).astype(np.float32)
        rc[d, :T] = np.cos(ang)
        rs[d, :T] = (-np.sin(ang)) if dd < 8 else np.sin(ang)
    c["rope_c"] = rc
    c["rope_s"] = rs
    pm = np.zeros((96, 96), f32)
    for d in range(32):
        dd = d % 16
        partner = (d + 8) if dd < 8 else (d - 8)
        pm[64 + partner, 64 + d] = 1.0
    c["perm96"] = pm
    i = np.arange(128)
    mu_ = (i[:, None] < i[None, :]).astype(f32)
    ml_ = (i[:, None] > i[None, :]).astype(f32)
    se = np.zeros((16, 16, 128), f32)
    for e in range(16):
        se[e, e, :] = 1.0
    c["selE"] = se
    c["tri_masks"] = np.stack([mu_, ml_, mu_ + np.eye(128, dtype=f32), ml_ + np.eye(128, dtype=f32)], 0)
    return c


def na_bias_table(rpb):
    Lr = rpb.shape[0]
    out = np.full((Lr, 16, 128, 5, 5, 128), -1e30, np.float32)
    jrep = [0, 1, 2, 14, 15]
    for pi, j in enumerate(jrep):
        kr0 = na_kr0(j)
        qtok = np.arange(128)
        qi = 2 * j + qtok // 64
        qj = qtok % 64
        r0 = np.clip(qi - 4, 0, 24)
        cstart = np.clip(qj - 8, 0, 48)
        for c in range(5):
            ktok = np.arange(128)
            ar = kr0 + 2 * c + ktok // 64
            kc = ktok % 64
            rv = (ar[:, None] >= r0[None, :]) & (ar[:, None] < r0[None, :] + 8)
            cv = (kc[:, None] >= cstart[None, :]) & (kc[:, None] < cstart[None, :] + 16)
            ridx = np.clip(ar[:, None] - qi[None, :] + 7, 0, 14)
            cidx = np.clip(kc[:, None] - qj[None, :] + 15, 0, 30)
            val = rpb[:, :, ridx, cidx]
            out[:, :, :, pi, c, :] = np.where((rv & cv)[None, None], val, np.float32(-1e30))
    return out


def make_inputs(inputs, b, consts, nab):
    f32 = np.float32
    x = np.asarray(inputs["x"][b], f32)
    ctx = np.asarray(inputs["ctx"][b], f32)
    d = dict(consts)
    d["xin"] = np.ascontiguousarray(np.concatenate([x, ctx], axis=0))
    cc = np.stack([np.asarray(inputs["c"][b], f32), np.asarray(inputs["c_ctx"], f32)], axis=0)
    d["cT"] = np.ascontiguousarray(cc.reshape(2, 16, 128).transpose(2, 1, 0))
    for k in ["w_mod", "b_mod", "norm1_g", "norm2_g", "w_in", "mla_q_norm_g", "mla_kv_norm_g", "mla_w_uq", "mla_w_ukv",
              "rw_shift_mu", "rw_w0", "rw_w_up", "rw_a0", "rw_a_up", "rw_g_up", "rw_k_k", "rw_k_a", "rw_r_k", "rw_gn_w", "rw_gn_b",
              "w_branch", "w_out", "router_w", "router_bias", "moe_w_gate_up", "moe_w_down", "final_norm_g"]:
        d[k] = np.asarray(inputs[k], f32)
    d["na_bias"] = nab
    return d


def kernel(**inputs):
    nc = build()
    consts = host_consts()
    nab = na_bias_table(np.asarray(inputs["na_rpb"], np.float32))
    in_maps = [make_inputs(inputs, b, consts, nab) for b in range(8)]
    res = run_bass_kernel_spmd(nc, in_maps, core_ids=list(range(8)))
    return np.stack([np.asarray(r["out"], np.float32) for r in res.results], axis=0)
```
